# Optimizing a Trainium2 kernel written in Bass

```python
import math
import jax
import jax.numpy as jnp
from jax import lax
import numpy as np


D_MODEL = 1024
BATCH = 1
SEQ = 16384
DEPTH = 4

GRID_W = 64
CTX_LEN = 256
BLOCK = 128
WINDOW = 128
ROPE_THETA = 10000.0
A_HQ = 8
A_HKV = 2
A_HD = 64
B_H = 4
B_HD = 64
B_VD = 2 * B_HD
C_HQ = 8
C_HKV = 2
C_HD = 128
N_EXPERTS = 16
N_GROUPS = 4
EXPERTS_PER_GROUP = N_EXPERTS // N_GROUPS
TOP_K = 2
D_EXPERT = 512
N_EVEN = (DEPTH + 1) // 2
N_ODD = DEPTH // 2
ALPHA = (2 * DEPTH) ** 0.25
BETA = (8 * DEPTH) ** -0.25
LN_EPS = 1e-5
QK_EPS = 1e-6
SUBLN_EPS = 1e-5
EVEN_SPLITS = (A_HQ * A_HD, A_HKV * A_HD, A_HKV * A_HD, B_H * 2 * B_HD, B_H * 2 * B_HD, B_H * B_VD)
EVEN_IN = sum(EVEN_SPLITS)
EVEN_OUT = A_HQ * A_HD + B_H * B_VD
ODD_SPLITS = (C_HQ * C_HD, C_HKV * C_HD, C_HKV * C_HD)
ODD_IN = sum(ODD_SPLITS)
ODD_OUT = C_HQ * C_HD

kernel_name = 'hybrid_diffusion_window_diff_axial_moe'


def _split(z, sizes):
    idx, acc = [], 0
    for s in sizes[:-1]:
        acc += s
        idx.append(acc)
    return jnp.split(z, idx, axis=-1)


def layer_norm(x, g, b):
    xf = x.astype(jnp.float32)
    mu = jnp.mean(xf, -1, keepdims=True)
    var = jnp.mean(jnp.square(xf - mu), -1, keepdims=True)
    y = (xf - mu) * lax.rsqrt(var + LN_EPS)
    return (y * g.astype(jnp.float32) + b.astype(jnp.float32)).astype(x.dtype)


def rms_norm(x, g, eps):
    xf = x.astype(jnp.float32)
    y = xf * lax.rsqrt(jnp.mean(jnp.square(xf), -1, keepdims=True) + eps)
    return (y * g.astype(jnp.float32)).astype(x.dtype)


def axial_rope_tables(n_tokens, head_dim):
    rows = n_tokens // GRID_W
    rr, cc = jnp.meshgrid(jnp.arange(rows), jnp.arange(GRID_W), indexing='ij')
    quarter = head_dim // 4
    inv_freq = ROPE_THETA ** (-jnp.arange(quarter, dtype=jnp.float32) / quarter)
    ang_r = rr.reshape(-1)[:, None].astype(jnp.float32) * inv_freq
    ang_c = cc.reshape(-1)[:, None].astype(jnp.float32) * inv_freq
    return jnp.cos(ang_r), jnp.sin(ang_r), jnp.cos(ang_c), jnp.sin(ang_c)


def _rotate(x, cos, sin):
    x1, x2 = jnp.split(x, 2, axis=-1)
    return jnp.concatenate([x1 * cos - x2 * sin, x1 * sin + x2 * cos], axis=-1)


def apply_axial_rope(x, tabs):
    cr, sr, cc, sc = [t[:, None, :].astype(x.dtype) for t in tabs]
    xr, xc = jnp.split(x, 2, axis=-1)
    return jnp.concatenate([_rotate(xr, cr, sr), _rotate(xc, cc, sc)], axis=-1)


def gqa_softmax(q, k, v, sink=None):
    hkv, grp = q.shape[2], q.shape[3]
    s = jnp.einsum('bqhgd,bkhd->bhgqk', q, k).astype(jnp.float32) * (q.shape[-1] ** -0.5)
    if sink is not None:
        s_sink = jnp.broadcast_to(sink.astype(jnp.float32).reshape(1, hkv, grp, 1, 1), s.shape[:-1] + (1,))
        p = jax.nn.softmax(jnp.concatenate([s_sink, s], axis=-1), axis=-1)[..., 1:]
    else:
        p = jax.nn.softmax(s, axis=-1)
    return jnp.einsum('bhgqk,bkhd->bqhgd', p.astype(v.dtype), v)


def window_attention_sink(q, k, v, kc, vc, sink):
    bsz, seq = q.shape[0], q.shape[1]
    nb = seq // BLOCK
    grp = A_HQ // A_HKV
    n_ctx = kc.shape[1]
    scale = A_HD ** -0.5
    qb = q.reshape(bsz, nb, BLOCK, A_HKV, grp, A_HD)

    def band(t):
        tp = jnp.pad(t, ((0, 0), (BLOCK, BLOCK), (0, 0), (0, 0))).reshape(bsz, nb + 2, BLOCK, A_HKV, A_HD)
        return jnp.concatenate([tp[:, :-2], tp[:, 1:-1], tp[:, 2:]], axis=2)

    kb, vb = band(k), band(v)
    blk = jnp.arange(nb)[:, None, None] * BLOCK
    qpos = blk + jnp.arange(BLOCK)[None, :, None]
    kpos = blk - BLOCK + jnp.arange(3 * BLOCK)[None, None, :]
    ok = (jnp.abs(qpos - kpos) <= WINDOW) & (kpos >= 0) & (kpos < seq)
    s_loc = jnp.einsum('bnqhgd,bnkhd->bnhgqk', qb, kb).astype(jnp.float32) * scale
    s_loc = jnp.where(ok[None, :, None, None], s_loc, -jnp.inf)
    s_ctx = jnp.einsum('bnqhgd,bchd->bnhgqc', qb, kc).astype(jnp.float32) * scale
    s_sink = jnp.broadcast_to(sink.astype(jnp.float32).reshape(1, 1, A_HKV, grp, 1, 1), s_ctx.shape[:-1] + (1,))
    p = jax.nn.softmax(jnp.concatenate([s_sink, s_ctx, s_loc], axis=-1), axis=-1)
    p_ctx = p[..., 1:1 + n_ctx].astype(v.dtype)
    p_loc = p[..., 1 + n_ctx:].astype(v.dtype)
    o = jnp.einsum('bnhgqc,bchd->bnqhgd', p_ctx, vc) + jnp.einsum('bnhgqk,bnkhd->bnqhgd', p_loc, vb)
    return o.reshape(bsz, seq, A_HQ * A_HD)


def diff_core(q, k, v, lam):
    s = jnp.einsum('bqhmd,bkhmd->bhmqk', q, k).astype(jnp.float32) * (B_HD ** -0.5)
    p = jax.nn.softmax(s, axis=-1)
    a = p[:, :, 0] - lam * p[:, :, 1]
    return jnp.einsum('bhqk,bkhe->bqhe', a.astype(v.dtype), v)


def diff_post(o, subln_g, lam_init):
    o = rms_norm(o, subln_g, SUBLN_EPS) * (1.0 - lam_init)
    return o.reshape(o.shape[0], o.shape[1], B_H * B_VD)


def even_mixer(h, hc, w_in, w_out, sink, lq1, lk1, lq2, lk2, subln_g, lam_init, ctx_out):
    bsz, seq = h.shape[0], h.shape[1]
    n_ctx = hc.shape[1]
    nb = seq // BLOCK
    grp = A_HQ // A_HKV
    tabs = axial_rope_tables(seq, A_HD)

    def proj(t):
        aq, ak, av, bq, bk, bv = _split(t @ w_in, EVEN_SPLITS)
        lead = t.shape[:2]
        return (aq.reshape(lead + (A_HQ, A_HD)), ak.reshape(lead + (A_HKV, A_HD)),
                av.reshape(lead + (A_HKV, A_HD)), bq.reshape(lead + (B_H * 2, B_HD)),
                bk.reshape(lead + (B_H * 2, B_HD)), bv.reshape(lead + (B_H, B_VD)))

    aq, ak, av, bq, bk, bv = proj(h)
    aq, ak, bq, bk = [apply_axial_rope(t, tabs) for t in (aq, ak, bq, bk)]
    aqc, akc, avc, bqc, bkc, bvc = proj(hc)
    lam = (jnp.exp(jnp.sum(lq1 * lk1)) - jnp.exp(jnp.sum(lq2 * lk2))).astype(jnp.float32) + lam_init

    oa = window_attention_sink(aq, ak, av, akc, avc, sink)
    kb_all = jnp.concatenate([bkc, bk], axis=1).reshape(bsz, n_ctx + seq, B_H, 2, B_HD)
    vb_all = jnp.concatenate([bvc, bv], axis=1)
    qblocks = bq.reshape(bsz, nb, BLOCK, B_H, 2, B_HD).swapaxes(0, 1)
    ob = lax.map(lambda qblk: diff_core(qblk, kb_all, vb_all, lam), qblocks)
    ob = diff_post(ob.swapaxes(0, 1).reshape(bsz, seq, B_H, B_VD), subln_g, lam_init)
    y = jnp.concatenate([oa, ob], axis=-1) @ w_out
    if not ctx_out:
        return y, None
    oac = gqa_softmax(aqc.reshape(bsz, n_ctx, A_HKV, grp, A_HD), akc, avc, sink).reshape(bsz, n_ctx, A_HQ * A_HD)
    obc = diff_post(diff_core(bqc.reshape(bsz, n_ctx, B_H, 2, B_HD), bkc.reshape(bsz, n_ctx, B_H, 2, B_HD), bvc, lam), subln_g, lam_init)
    yc = jnp.concatenate([oac, obc], axis=-1) @ w_out
    return y, yc


def odd_mixer(h, hc, w_in, w_out, qn_g, kn_g, ctx_out):
    bsz, seq = h.shape[0], h.shape[1]
    n_ctx = hc.shape[1]
    nb = seq // BLOCK
    grp = C_HQ // C_HKV
    tabs = axial_rope_tables(seq, C_HD)

    def proj(t):
        q, k, v = _split(t @ w_in, ODD_SPLITS)
        lead = t.shape[:2]
        q = rms_norm(q.reshape(lead + (C_HQ, C_HD)), qn_g, QK_EPS)
        k = rms_norm(k.reshape(lead + (C_HKV, C_HD)), kn_g, QK_EPS)
        return q, k, v.reshape(lead + (C_HKV, C_HD))

    q, k, v = proj(h)
    q, k = apply_axial_rope(q, tabs), apply_axial_rope(k, tabs)
    qc, kc, vc = proj(hc)
    k_all = jnp.concatenate([kc, k], axis=1)
    v_all = jnp.concatenate([vc, v], axis=1)
    qblocks = q.reshape(bsz, nb, BLOCK, C_HKV, grp, C_HD).swapaxes(0, 1)
    o = lax.map(lambda qblk: gqa_softmax(qblk, k_all, v_all), qblocks)
    y = o.swapaxes(0, 1).reshape(bsz, seq, C_HQ * C_HD) @ w_out
    if not ctx_out:
        return y, None
    oc = gqa_softmax(qc.reshape(bsz, n_ctx, C_HKV, grp, C_HD), kc, vc).reshape(bsz, n_ctx, C_HQ * C_HD)
    return y, oc @ w_out


def moe(h, w_router, b_router, w_gate, w_up, w_down):
    logits = jnp.einsum('...d,de->...e', h, w_router).astype(jnp.float32)
    scores = jax.nn.sigmoid(logits)
    sel = scores + b_router.astype(jnp.float32)
    grouped = sel.reshape(sel.shape[:-1] + (N_GROUPS, EXPERTS_PER_GROUP))
    group_score = jnp.sum(lax.top_k(grouped, TOP_K)[0], axis=-1)
    best = jnp.argmax(group_score, axis=-1)
    in_group = (jnp.arange(N_EXPERTS) // EXPERTS_PER_GROUP) == best[..., None]
    _, idx = lax.top_k(jnp.where(in_group, sel, -jnp.inf), TOP_K)
    wts = jnp.take_along_axis(scores, idx, axis=-1)
    wts = wts / jnp.sum(wts, axis=-1, keepdims=True)
    gates = jnp.sum(jax.nn.one_hot(idx, N_EXPERTS, dtype=jnp.float32) * wts[..., None], axis=-2)
    g = jnp.einsum('...d,edf->...ef', h, w_gate)
    u = jnp.einsum('...d,edf->...ef', h, w_up)
    a = jax.nn.silu(g) * u * gates[..., None].astype(h.dtype)
    return jnp.einsum('...ef,efd->...d', a, w_down)


def setup_inputs(seed: int = 0) -> dict:
    key = jax.random.key(seed)
    ks = jax.random.split(key, 25)

    def nrm(k, shape, s):
        return jax.random.normal(k, shape, jnp.float32) * s

    return {
        'x': nrm(ks[0], (BATCH, SEQ, D_MODEL), 1.0),
        'c': nrm(ks[1], (BATCH, D_MODEL), 1.0),
        'ctx': nrm(ks[2], (BATCH, CTX_LEN, D_MODEL), 1.0),
        'c_ctx': nrm(ks[3], (D_MODEL,), 1.0),
        'w_mod': nrm(ks[4], (DEPTH, D_MODEL, 6 * D_MODEL), 0.5 * D_MODEL ** -0.5),
        'b_mod': nrm(ks[5], (DEPTH, 6 * D_MODEL), 0.02),
        'ln_g': 1.0 + nrm(ks[6], (DEPTH, 2, D_MODEL), 0.02),
        'ln_b': nrm(ks[7], (DEPTH, 2, D_MODEL), 0.02),
        'w_in_even': nrm(ks[8], (N_EVEN, D_MODEL, EVEN_IN), D_MODEL ** -0.5),
        'w_out_even': nrm(ks[9], (N_EVEN, EVEN_OUT, D_MODEL), BETA * EVEN_OUT ** -0.5),
        'sink_logits': nrm(ks[10], (N_EVEN, A_HQ), 0.5),
        'lam_q1': nrm(ks[11], (N_EVEN, B_HD), 0.1),
        'lam_k1': nrm(ks[12], (N_EVEN, B_HD), 0.1),
        'lam_q2': nrm(ks[13], (N_EVEN, B_HD), 0.1),
        'lam_k2': nrm(ks[14], (N_EVEN, B_HD), 0.1),
        'subln_g': 1.0 + nrm(ks[15], (N_EVEN, B_VD), 0.02),
        'w_in_odd': nrm(ks[16], (N_ODD, D_MODEL, ODD_IN), D_MODEL ** -0.5),
        'w_out_odd': nrm(ks[17], (N_ODD, ODD_OUT, D_MODEL), BETA * ODD_OUT ** -0.5),
        'q_norm_g': 1.0 + nrm(ks[18], (N_ODD, C_HD), 0.02),
        'k_norm_g': 1.0 + nrm(ks[19], (N_ODD, C_HD), 0.02),
        'w_router': nrm(ks[20], (D_MODEL, N_EXPERTS), D_MODEL ** -0.5),
        'b_router': nrm(ks[21], (N_EXPERTS,), 0.01),
        'w_gate': nrm(ks[22], (DEPTH, N_EXPERTS, D_MODEL, D_EXPERT), D_MODEL ** -0.5),
        'w_up': nrm(ks[23], (DEPTH, N_EXPERTS, D_MODEL, D_EXPERT), D_MODEL ** -0.5),
        'w_down': nrm(ks[24], (DEPTH, N_EXPERTS, D_EXPERT, D_MODEL), BETA * D_EXPERT ** -0.5),
    }


def reference(x, c, ctx, c_ctx, w_mod, b_mod, ln_g, ln_b, w_in_even, w_out_even, sink_logits,
              lam_q1, lam_k1, lam_q2, lam_k2, subln_g, w_in_odd, w_out_odd, q_norm_g, k_norm_g,
              w_router, b_router, w_gate, w_up, w_down):
    xc = ctx
    cond = jax.nn.silu(c)
    cond_ctx = jax.nn.silu(c_ctx)
    for l in range(DEPTH):
        ctx_out = l < DEPTH - 1
        sh1, sc1, g1, sh2, sc2, g2 = [m[:, None, :] for m in jnp.split(cond @ w_mod[l] + b_mod[l], 6, axis=-1)]
        shc1, scc1, gc1, shc2, scc2, gc2 = jnp.split(cond_ctx @ w_mod[l] + b_mod[l], 6, axis=-1)
        h = x * (1.0 + sc1) + sh1
        hc = xc * (1.0 + scc1) + shc1
        i = l // 2
        if l % 2 == 0:
            lam_init = 0.8 - 0.6 * math.exp(-0.3 * l)
            y, yc = even_mixer(h, hc, w_in_even[i], w_out_even[i], sink_logits[i], lam_q1[i], lam_k1[i],
                               lam_q2[i], lam_k2[i], subln_g[i], lam_init, ctx_out)
        else:
            y, yc = odd_mixer(h, hc, w_in_odd[i], w_out_odd[i], q_norm_g[i], k_norm_g[i], ctx_out)
        x = layer_norm(ALPHA * x + g1 * y, ln_g[l, 0], ln_b[l, 0])
        h2 = x * (1.0 + sc2) + sh2
        x = layer_norm(ALPHA * x + g2 * moe(h2, w_router, b_router, w_gate[l], w_up[l], w_down[l]), ln_g[l, 1], ln_b[l, 1])
        if ctx_out:
            xc = layer_norm(ALPHA * xc + gc1 * yc, ln_g[l, 0], ln_b[l, 0])
            hc2 = xc * (1.0 + scc2) + shc2
            xc = layer_norm(ALPHA * xc + gc2 * moe(hc2, w_router, b_router, w_gate[l], w_up[l], w_down[l]), ln_g[l, 1], ln_b[l, 1])
    return x
```

```python
import os
import numpy as np
import ml_dtypes
from contextlib import ExitStack
import concourse.bass as bass
import concourse.mybir as mybir
from concourse.bass_utils import run_bass_kernel_spmd

F32 = mybir.dt.float32
BF16 = mybir.dt.bfloat16
AF = mybir.ActivationFunctionType
ALU = mybir.AluOpType
AX = mybir.AxisListType
NPBF = ml_dtypes.bfloat16

NCORES = 8
D = 1024
SEQ = 16384
CTX = 256
NT = 17
TOK = NT * 128
NKB = (SEQ + CTX) // 128
DEPTH = 4
ALPHA = (2 * DEPTH) ** 0.25
LN_EPS = 1e-5
QK_EPS = 1e-6
SUBLN_EPS = 1e-5
E_COLS = 2304
O_COLS = 1536
NEXP = 16
DEXP = 512
CHUNKS = [(0, 512), (512, 512), (1024, 512), (1536, 512), (2048, 128)]


class Buf:
    __slots__ = ("name", "w", "rs", "dsem", "dcnt", "excl")

    def __init__(self, name, excl=False):
        self.name = name
        self.w = None
        self.rs = {}
        self.dsem = None
        self.dcnt = 0
        self.excl = excl


class Trk:
    def __init__(self, nc, es):
        self.nc = nc
        self.es = es
        self.eng = {"pe": nc.tensor, "act": nc.scalar, "dve": nc.vector, "pool": nc.gpsimd, "sp": nc.sync}
        self.sem = {}
        self.cnt = {}
        self.waited = {k: {} for k in self.eng}
        self.nsem = 0
        self.dbufs = []
        for k in self.eng:
            self.sem[k] = es.enter_context(nc.semaphore("e_" + k))
            self.cnt[k] = 0
        self.n_ins = 0
        self.n_wait = 0

    def _deps(self, reads, writes, e=None):
        deps = {}
        own = self.sem.get(e)

        def add(t):
            if t is None:
                return
            k = id(t[0])
            if k not in deps or deps[k][1] < t[1]:
                deps[k] = t
        for b in reads:
            add(b.w)
            if b.excl:
                for t in b.rs.values():
                    if t[0] is not own:
                        add(t)
        for b in writes:
            add(b.w)
            for t in b.rs.values():
                add(t)
        return deps

    def _emit_waits(self, e, deps):
        own = self.sem.get(e)
        eo = self.eng[e]
        wd = self.waited[e]
        for k, (s, v) in deps.items():
            if s is own and e in ("pe", "sp"):
                continue
            if wd.get(k, 0) >= v:
                continue
            eo.wait_ge(s, v)
            wd[k] = v
            self.n_wait += 1

    def _commit(self, t, reads, writes):
        k = id(t[0])
        for b in reads:
            b.rs[k] = t
        for b in writes:
            b.w = t
            b.rs = {}

    def op(self, e, fn, reads=(), writes=()):
        self._emit_waits(e, self._deps(reads, writes, e))
        ins = fn()
        self.cnt[e] += 1
        ins.then_inc(self.sem[e], 1)
        self._commit((self.sem[e], self.cnt[e]), reads, writes)
        self.n_ins += 1
        return ins

    def dma(self, q, out, in_, reads=(), writes=(), **kw):
        self._emit_waits(q, self._deps(reads, writes, q))
        owner = writes[0] if len(writes) else reads[0]
        if owner.dsem is None:
            owner.dsem = self.es.enter_context(self.nc.semaphore("d_%d" % self.nsem))
            self.nsem += 1
            self.dbufs.append(owner)
        ins = self.eng[q].dma_start(out=out, in_=in_, **kw)
        owner.dcnt += 16
        ins.then_inc(owner.dsem, 16)
        self._commit((owner.dsem, owner.dcnt), reads, writes)
        self.n_ins += 1
        return ins

    def barrier(self):
        deps = {}
        for k in self.eng:
            if self.cnt[k]:
                deps[id(self.sem[k])] = (self.sem[k], self.cnt[k])
        for b in self.dbufs:
            deps[id(b.dsem)] = (b.dsem, b.dcnt)
        for e in self.eng:
            own = self.sem[e]
            eo = self.eng[e]
            wd = self.waited[e]
            for k, (s, v) in deps.items():
                if s is own or wd.get(k, 0) >= v:
                    continue
                eo.wait_ge(s, v)
                wd[k] = v

    def finish(self, e="pool"):
        deps = {}
        for b in self.dbufs:
            deps[id(b.dsem)] = (b.dsem, b.dcnt)
        for k in self.eng:
            if self.cnt[k]:
                deps[id(self.sem[k])] = (self.sem[k], self.cnt[k])
        self._emit_waits(e, deps)


class Prog:
    def __init__(self):
        self.nc = bass.Bass("TRN2", target_bir_lowering=False)
        self.es = ExitStack()
        self.T = Trk(self.nc, self.es)
        self.banks = [self.es.enter_context(self.nc.psum_tensor("bank%d" % i, [128, 512], F32)) for i in range(8)]
        self.bb = [Buf("bank%d" % i, excl=True) for i in range(8)]
        self.uid = 0
        nc, T = self.nc, self.T
        self.ident_d = self.inp("ident", [128, 128], F32)
        self.ident = self.sb(self.es, [128, 128], F32)
        self.b_ident = Buf("ident")
        T.dma("sp", self.ident[:], self.ident_d[:, :], writes=[self.b_ident])
        self.ones = self.sb(self.es, [128, 128], BF16)
        self.b_ones = Buf("ones")
        T.op("pool", lambda: nc.gpsimd.memset(self.ones[:], 1.0), writes=[self.b_ones])
        self.onesd = self.sb(self.es, [128, 128], F32)
        self.b_onesd = Buf("onesd")
        T.op("pool", lambda: nc.gpsimd.memset(self.onesd[:], 1.0 / 128.0), writes=[self.b_onesd])

    def inp(self, name, shape, dt):
        return self.nc.dram_tensor(name, list(shape), dt, kind="ExternalInput").ap()

    def outp(self, name, shape, dt):
        return self.nc.dram_tensor(name, list(shape), dt, kind="ExternalOutput").ap()

    def sb(self, es, shape, dt, name=None):
        self.uid += 1
        return es.enter_context(self.nc.sbuf_tensor("%s_%d" % (name or "t", self.uid), list(shape), dt))

    def close(self):
        self.T.finish("pool")
        self.es.close()
        return self.nc


def bcast_rows(ap_row, n):
    return ap_row.partition_broadcast(n)


def phase_mod(P, cc_d, wm_d, bm_d, out_d, ncols):
    nc, T = P.nc, P.T
    with ExitStack() as es:
        cc = P.sb(es, [128, 8, 2], F32)
        b_cc = Buf("cc")
        T.dma("sp", cc[:], cc_d[:, :, :], writes=[b_cc])
        ccs = P.sb(es, [128, 8, 2], F32)
        b_ccs = Buf("ccs")
        T.op("act", lambda: nc.scalar.activation(out=ccs[:], in_=cc[:], func=AF.Silu), reads=[b_cc], writes=[b_ccs])
        CW = 384
        stg = [P.sb(es, [128, 8, CW], F32) for _ in range(2)]
        b_stg = [Buf("stg%d" % i) for i in range(2)]
        bt = [P.sb(es, [2, CW], F32) for _ in range(2)]
        b_bt = [Buf("bt%d" % i) for i in range(2)]
        rs = [P.sb(es, [2, CW], F32) for _ in range(2)]
        b_rs = [Buf("rs%d" % i) for i in range(2)]
        it = 0
        for l in range(DEPTH):
            for c0 in range(0, ncols, CW):
                i = it % 2
                it += 1
                T.dma("sp", stg[i][:], wm_d[l, :, c0:c0 + CW].rearrange("(p k) n -> p k n", k=8), writes=[b_stg[i]])
                T.dma("sp", bt[i][:], bm_d[l, :, c0:c0 + CW], writes=[b_bt[i]])
                ps = P.banks[i]
                for k in range(8):
                    T.op("pe", lambda: nc.tensor.matmul(ps[0:2, 0:CW], lhsT=ccs[:, k, :], rhs=stg[i][:, k, :], start=(k == 0), stop=(k == 7)),
                         reads=[b_ccs, b_stg[i]], writes=[P.bb[i]])
                T.op("dve", lambda: nc.vector.tensor_tensor(out=rs[i][:], in0=ps[0:2, 0:CW], in1=bt[i][:], op=ALU.add),
                     reads=[P.bb[i], b_bt[i]], writes=[b_rs[i]])
                T.dma("pool", out_d[l, :, c0:c0 + CW], rs[i][:], reads=[b_rs[i]])
    T.barrier()


def load_weight_bf16(P, es, src, K, ncols, name):
    nc, T = P.nc, P.T
    dst = P.sb(es, [128, K, ncols], BF16, name)
    CW = 4096 // K
    stg = [P.sb(es, [128, K, CW], F32, name + "s") for _ in range(2)]
    b_stg = [Buf(name + "s%d" % i) for i in range(2)]
    bufs = {}
    it = 0
    for c0 in range(0, ncols, CW):
        cw = min(CW, ncols - c0)
        i = it % 2
        it += 1
        T.dma("sp", stg[i][:, :, 0:cw], src[:, c0:c0 + cw].rearrange("(k p) n -> p k n", p=128), writes=[b_stg[i]])
        b = Buf(name + "_c%d" % c0)
        T.op("act", lambda: nc.scalar.copy(out=dst[:, :, c0:c0 + cw], in_=stg[i][:, :, 0:cw]), reads=[b_stg[i]], writes=[b])
        for c in range(c0, c0 + cw, 128):
            bufs[c // 128] = b
    return dst, bufs


def rope_tok(P, src, dst, nh, hd, cos, sin, b_src, b_dst, b_tab, tmp, b_tmp):
    nc, T = P.nc, P.T
    q = hd // 4
    sv = src.rearrange("p (h a j i) -> p h a j i", h=nh, a=2, j=2, i=q)
    dv = dst.rearrange("p (h a j i) -> p h a j i", h=nh, a=2, j=2, i=q)
    x1 = sv[:, :, :, 0, :]
    x2 = sv[:, :, :, 1, :]
    cb = cos.rearrange("p (a i) -> p a i", a=2).unsqueeze(1).to_broadcast([128, nh, 2, q])
    sbc = sin.rearrange("p (a i) -> p a i", a=2).unsqueeze(1).to_broadcast([128, nh, 2, q])
    n = nh * 2 * q
    t = [tmp[j][:, 0:n].rearrange("p (h a i) -> p h a i", h=nh, a=2, i=q) for j in range(4)]
    T.op("dve", lambda: nc.vector.tensor_tensor(out=t[0], in0=x1, in1=cb, op=ALU.mult), reads=[b_src, b_tab], writes=[b_tmp[0]])
    T.op("dve", lambda: nc.vector.tensor_tensor(out=t[1], in0=x2, in1=sbc, op=ALU.mult), reads=[b_src, b_tab], writes=[b_tmp[1]])
    T.op("dve", lambda: nc.vector.tensor_tensor(out=t[2], in0=x1, in1=sbc, op=ALU.mult), reads=[b_src, b_tab], writes=[b_tmp[2]])
    T.op("dve", lambda: nc.vector.tensor_tensor(out=t[3], in0=x2, in1=cb, op=ALU.mult), reads=[b_src, b_tab], writes=[b_tmp[3]])
    T.op("pool", lambda: nc.gpsimd.tensor_tensor(out=dv[:, :, :, 0, :], in0=t[0], in1=t[1], op=ALU.subtract),
         reads=[b_tmp[0], b_tmp[1]], writes=[b_dst])
    T.op("pool", lambda: nc.gpsimd.tensor_tensor(out=dv[:, :, :, 1, :], in0=t[2], in1=t[3], op=ALU.add),
         reads=[b_tmp[2], b_tmp[3]], writes=[b_dst])


def transpose_mod(P, xt, b_xt, scT, shT, b_mod, hT, b_hT, tb, hTf=None, b_hTf=None):
    nc, T = P.nc, P.T
    for k in range(8):
        bk = tb[k // 4]
        T.op("pe", lambda: nc.tensor.transpose(P.banks[bk][:, (k % 4) * 128:(k % 4 + 1) * 128], xt[:, k * 128:(k + 1) * 128], P.ident[:]),
             reads=[b_xt, P.b_ident], writes=[P.bb[bk]])
    for k in range(8):
        bk = tb[k // 4]
        src = P.banks[bk][:, (k % 4) * 128:(k % 4 + 1) * 128]
        if hTf is None:
            T.op("act", lambda: nc.scalar.activation(out=hT[:, k, :], in_=src, func=AF.Identity, bias=shT[:, k:k + 1], scale=scT[:, k:k + 1]),
                 reads=[P.bb[bk], b_mod], writes=[b_hT])
        else:
            T.op("act", lambda: nc.scalar.activation(out=hTf[:, k, :], in_=src, func=AF.Identity, bias=shT[:, k:k + 1], scale=scT[:, k:k + 1]),
                 reads=[P.bb[bk], b_mod], writes=[b_hTf])
    if hTf is not None:
        T.op("dve", lambda: nc.vector.tensor_copy(out=hT, in_=hTf[:]), reads=[b_hTf], writes=[b_hT])


def load_modT(P, es, m_d, name):
    nc, T = P.nc, P.T
    m = P.sb(es, [128, 2, 2, 8], F32, name)
    b = Buf(name)
    T.dma("sp", m[:], m_d[:, :, :, :], writes=[b])
    T.op("dve", lambda: nc.vector.tensor_scalar(out=m[:, :, 0, :], in0=m[:, :, 0, :], scalar1=1.0, scalar2=None, op0=ALU.add),
         reads=[b], writes=[b])
    return m, b


def phase_A(P, even, x_d, x_bufs, w_d, mA_d, cos_d, sin_d, qng_d, qkv_d):
    nc, T = P.nc, P.T
    ncols = E_COLS if even else O_COLS
    hd = 64 if even else 128
    hq = hd // 2
    with ExitStack() as es:
        W, wb = load_weight_bf16(P, es, w_d, 8, ncols, "win")
        m, b_m = load_modT(P, es, mA_d, "mA")
        xt = [P.sb(es, [128, D], F32, "xt") for _ in range(2)]
        b_xt = [Buf("xt%d" % i) for i in range(2)]
        hT = [P.sb(es, [128, 8, 128], BF16, "hT") for _ in range(2)]
        b_hT = [Buf("hT%d" % i) for i in range(2)]
        ot = [P.sb(es, [128, ncols], BF16, "ot") for _ in range(2)]
        b_ot = [Buf("ot%d" % i) for i in range(2)]
        cs = [P.sb(es, [128, 2, hq], F32, "cs") for _ in range(2)]
        b_cs = [Buf("cs%d" % i) for i in range(2)]
        tmp = [P.sb(es, [128, 512], F32, "rt") for _ in range(4)]
        b_tmp = [Buf("rt%d" % i) for i in range(4)]
        if not even:
            gq = P.sb(es, [128, 2, 128], F32, "gq")
            b_gq = Buf("gq")
            T.dma("sp", gq[:, 0, :], qng_d[0:1, :].partition_broadcast(128), writes=[b_gq])
            T.dma("sp", gq[:, 1, :], qng_d[1:2, :].partition_broadcast(128), writes=[b_gq])
            epsq = P.sb(es, [128, 1], F32, "epsq")
            b_epsq = Buf("epsq")
            T.op("pool", lambda: nc.gpsimd.memset(epsq[:], QK_EPS), writes=[b_epsq])
            sq = P.sb(es, [128, 512], F32, "sq")
            b_sq = Buf("sq")
            ssq = P.sb(es, [128, 4], F32, "ssq")
            b_ssq = Buf("ssq")
            xn = [P.sb(es, [128, 512], F32, "xn") for _ in range(2)]
            b_xn = [Buf("xn%d" % i) for i in range(2)]
        nbk = (ncols + 511) // 512
        for t in range(NT):
            i = t % 2
            who = 1 if t == NT - 1 else 0
            rd = [x_bufs[t]] if x_bufs is not None else []
            T.dma("sp", xt[i][:], x_d[t * 128:(t + 1) * 128, :], reads=rd, writes=[b_xt[i]])
            T.dma("sp", cs[i][:, 0, :], cos_d[t * 128:(t + 1) * 128, :], writes=[b_cs[i]])
            T.dma("sp", cs[i][:, 1, :], sin_d[t * 128:(t + 1) * 128, :], writes=[b_cs[i]])
            transpose_mod(P, xt[i], b_xt[i], m[:, who, 0, :], m[:, who, 1, :], b_m, hT[i], b_hT[i], (6, 7))
            for c in range(nbk):
                cw = min(512, ncols - c * 512)
                for k in range(8):
                    T.op("pe", lambda: nc.tensor.matmul(P.banks[c][:, 0:cw], lhsT=hT[i][:, k, :], rhs=W[:, k, c * 512:c * 512 + cw],
                                                        start=(k == 0), stop=(k == 7)),
                         reads=[b_hT[i], wb[c * 4]] + ([wb[c * 4 + 3]] if cw == 512 else []), writes=[P.bb[c]])
            o = ot[i]
            cosv, sinv = cs[i][:, 0, :], cs[i][:, 1, :]
            if even:
                def rp(bank, c0, c1):
                    rope_tok(P, P.banks[bank][:, c0 - bank * 512:c1 - bank * 512], o[:, c0:c1], (c1 - c0) // 64, 64, cosv, sinv,
                             P.bb[bank], b_ot[i], b_cs[i], tmp, b_tmp)
                rp(0, 0, 512)
                rp(1, 512, 640)
                rp(1, 768, 1024)
                rp(2, 1024, 1536)
                rp(3, 1536, 1792)
                T.op("act", lambda: nc.scalar.copy(out=o[:, 640:768], in_=P.banks[1][:, 128:256]), reads=[P.bb[1]], writes=[b_ot[i]])
                T.op("act", lambda: nc.scalar.copy(out=o[:, 1792:2048], in_=P.banks[3][:, 256:512]), reads=[P.bb[3]], writes=[b_ot[i]])
                T.op("act", lambda: nc.scalar.copy(out=o[:, 2048:2304], in_=P.banks[4][:, 0:256]), reads=[P.bb[4]], writes=[b_ot[i]])
            else:
                for bank, nh, gi in ((0, 4, 0), (1, 4, 0), (2, 2, 1)):
                    n = nh * 128
                    src = P.banks[bank][:, 0:n]
                    T.op("act", lambda: nc.scalar.activation(out=sq[:, 0:n], in_=src, func=AF.Square), reads=[P.bb[bank]], writes=[b_sq])
                    T.op("dve", lambda: nc.vector.tensor_reduce(out=ssq[:, 0:nh], in_=sq[:, 0:n].rearrange("p (h d) -> p h d", h=nh),
                                                                axis=AX.X, op=ALU.add), reads=[b_sq], writes=[b_ssq])
                    T.op("act", lambda: nc.scalar.activation(out=ssq[:, 0:nh], in_=ssq[:, 0:nh], func=AF.Sqrt, bias=epsq[:, 0:1], scale=1.0 / 128.0),
                         reads=[b_ssq, b_epsq], writes=[b_ssq])
                    T.op("dve", lambda: nc.vector.reciprocal(out=ssq[:, 0:nh], in_=ssq[:, 0:nh]), reads=[b_ssq], writes=[b_ssq])
                    j = bank % 2
                    T.op("dve", lambda: nc.vector.tensor_tensor(out=xn[j][:, 0:n].rearrange("p (h d) -> p h d", h=nh),
                                                                in0=src.rearrange("p (h d) -> p h d", h=nh),
                                                                in1=ssq[:, 0:nh].unsqueeze(2).to_broadcast([128, nh, 128]), op=ALU.mult),
                         reads=[P.bb[bank], b_ssq], writes=[b_xn[j]])
                    T.op("pool", lambda: nc.gpsimd.tensor_tensor(out=xn[j][:, 0:n].rearrange("p (h d) -> p h d", h=nh),
                                                                 in0=xn[j][:, 0:n].rearrange("p (h d) -> p h d", h=nh),
                                                                 in1=gq[:, gi, :].unsqueeze(1).to_broadcast([128, nh, 128]), op=ALU.mult),
                         reads=[b_xn[j], b_gq], writes=[b_xn[j]])
                    rope_tok(P, xn[j][:, 0:n], o[:, bank * 512:bank * 512 + n], nh, 128, cosv, sinv, b_xn[j], b_ot[i], b_cs[i], tmp, b_tmp)
                T.op("act", lambda: nc.scalar.copy(out=o[:, 1280:1536], in_=P.banks[2][:, 256:512]), reads=[P.bb[2]], writes=[b_ot[i]])
            T.dma("pool", qkv_d[t * 128:(t + 1) * 128, :], o[:], reads=[b_ot[i]])
    T.barrier()


class SweepCtx:
    def __init__(self, P, es, npt=4):
        self.P = P
        self.pT = [P.sb(es, [128, 512], BF16, "pT") for _ in range(npt)]
        self.b_pT = [Buf("pT%d" % i) for i in range(npt)]
        self.n = 0
        self.osel = 0


def sweep(P, S, N, blocks, qT, b_q, kT_of, v_of, b_kv, M, scale, ones_ap, b_ones):
    nc, T = P.nc, P.T
    ob, sb_ = (2, 3) if S.osel == 0 else (4, 5)
    S.osel ^= 1
    nb = len(blocks)
    idx = []
    for i in range(nb + 1):
        if i < nb:
            n = S.n
            S.n += 1
            idx.append(n)
            s = n % 2
            p = n % len(S.pT)
            T.op("pe", lambda: nc.tensor.matmul(P.banks[s][:, 0:N], lhsT=kT_of(blocks[i]), rhs=qT, start=True, stop=True),
                 reads=[b_kv, b_q], writes=[P.bb[s]])
            T.op("act", lambda: nc.scalar.activation(out=S.pT[p][:, 0:N], in_=P.banks[s][:, 0:N], func=AF.Exp, scale=scale),
                 reads=[P.bb[s]], writes=[S.b_pT[p]])
        if i >= 1:
            j = i - 1
            p = idx[j] % len(S.pT)
            T.op("pe", lambda: nc.tensor.matmul(P.banks[ob][0:M, 0:N], lhsT=v_of(blocks[j]), rhs=S.pT[p][:, 0:N], start=(j == 0), stop=(j == nb - 1)),
                 reads=[b_kv, S.b_pT[p]], writes=[P.bb[ob]])
            T.op("pe", lambda: nc.tensor.matmul(P.banks[sb_][0:M, 0:N], lhsT=ones_ap, rhs=S.pT[p][:, 0:N], start=(j == 0), stop=(j == nb - 1)),
                 reads=[b_ones, S.b_pT[p]], writes=[P.bb[sb_]])
    return ob, sb_


ALL_BLOCKS = list(range(NKB))
CTX_BLOCKS = [0, 1]


def phase_B_odd(P, OT, b_OT, QT_d, KT_d, V_d):
    nc, T = P.nc, P.T
    scale = 128.0 ** -0.5
    with ExitStack() as es:
        S = SweepCtx(P, es)
        kt = P.sb(es, [128, NKB * 128], BF16, "kt")
        vt = P.sb(es, [128, NKB, 128], BF16, "vt")
        b_kv = Buf("kv")
        qt = [P.sb(es, [128, TOK], BF16, "qt") for _ in range(2)]
        b_qt = [Buf("qt%d" % i) for i in range(2)]
        rec = [P.sb(es, [128, 512], F32, "rec") for _ in range(2)]
        b_rec = [Buf("rec%d" % i) for i in range(2)]
        fi = 0
        for kvh in range(2):
            T.dma("sp", kt[:], KT_d[kvh, :, :], writes=[b_kv])
            T.dma("sp", vt[:], V_d[kvh, :, :, :], writes=[b_kv])
            for g in range(4):
                h = kvh * 4 + g
                qi = h % 2
                T.dma("sp", qt[qi][:], QT_d[h, :, :], writes=[b_qt[qi]])
                for (c0, N) in CHUNKS:
                    blocks = ALL_BLOCKS if N == 512 else CTX_BLOCKS
                    ob, sbk = sweep(P, S, N, blocks, qt[qi][:, c0:c0 + N], b_qt[qi],
                                    lambda b: kt[:, b * 128:(b + 1) * 128], lambda b: vt[:, b, :], b_kv, 128, scale, P.ones[:], P.b_ones)
                    r = fi % 2
                    fi += 1
                    T.op("dve", lambda: nc.vector.reciprocal(out=rec[r][:, 0:N], in_=P.banks[sbk][:, 0:N]), reads=[P.bb[sbk]], writes=[b_rec[r]])
                    T.op("dve", lambda: nc.vector.tensor_tensor(out=OT[:, h, c0:c0 + N], in0=P.banks[ob][:, 0:N], in1=rec[r][:, 0:N], op=ALU.mult),
                         reads=[P.bb[ob], b_rec[r]], writes=[b_OT])
    T.barrier()


def phase_B_even(P, OT, b_OT, QTa_d, KTa_d, Va_d, mask_d, sinkp_d, QTb_d, KTb_d, Vb_d, lamv_d, lami_d, subg_d):
    nc, T = P.nc, P.T
    sc = 64.0 ** -0.5
    with ExitStack() as es:
        qta = P.sb(es, [128, 4, TOK], BF16, "qta")
        b_qta = Buf("qta")
        T.dma("sp", qta[:], QTa_d[:, :, :], writes=[b_qta])
        ktaw = P.sb(es, [128, 20 * 128], BF16, "ktaw")
        b_kta = Buf("ktaw")
        T.dma("sp", ktaw[:], KTa_d[:, :], writes=[b_kta])
        vaw = P.sb(es, [128, 4, 20, 128], BF16, "vaw")
        b_vaw = Buf("vaw")
        T.dma("sp", vaw[:], Va_d[:, :, :, :], writes=[b_vaw])
        msk = P.sb(es, [128, NT, 384], BF16, "msk")
        b_msk = Buf("msk")
        T.dma("sp", msk[:], mask_d[:, :, :], writes=[b_msk])
        esink = P.sb(es, [128, 4], F32, "esink")
        b_es = Buf("esink")
        T.dma("sp", esink[:], sinkp_d[:, :], writes=[b_es])
        T.op("act", lambda: nc.scalar.activation(out=esink[:], in_=esink[:], func=AF.Exp), reads=[b_es], writes=[b_es])
        oneslh = P.sb(es, [128, 2, 128], BF16, "oneslh")
        b_olh = Buf("oneslh")
        T.op("pool", lambda: nc.gpsimd.memset(oneslh[:], 0.0), writes=[b_olh])
        T.op("pool", lambda: nc.gpsimd.memset(oneslh[:, 0, 0:64], 1.0), reads=[b_olh], writes=[b_olh])
        T.op("pool", lambda: nc.gpsimd.memset(oneslh[:, 1, 64:128], 1.0), reads=[b_olh], writes=[b_olh])
        pTa = [P.sb(es, [128, 640], BF16, "pTa") for _ in range(4)]
        b_pTa = [Buf("pTa%d" % i) for i in range(4)]
        den = [P.sb(es, [128, 128], F32, "den") for _ in range(2)]
        b_den = [Buf("den%d" % i) for i in range(2)]
        n = 0
        fi = 0
        for c in range(4):
            kvh = c // 2
            ksl = slice(kvh * 64, kvh * 64 + 64)
            for t in range(NT):
                wl = (t + 2) if t < NT - 1 else 2
                blocks = [0, 1, wl, wl + 1, wl + 2]
                ob, sbk = (4, 5) if fi % 2 == 0 else (6, 7)
                for hh in range(2):
                    j = (2 * c + hh) % 4
                    p = n % 4
                    s0, s1 = (0, 1) if n % 2 == 0 else (2, 3)
                    n += 1
                    q_ap = qta[ksl, j, t * 128:(t + 1) * 128]
                    for bi, w in enumerate(blocks):
                        bk, col = (s0, bi * 128) if bi < 2 else (s1, (bi - 2) * 128)
                        T.op("pe", lambda: nc.tensor.matmul(P.banks[bk][:, col:col + 128], lhsT=ktaw[ksl, w * 128:(w + 1) * 128], rhs=q_ap,
                                                            start=True, stop=True), reads=[b_kta, b_qta], writes=[P.bb[bk]])
                    T.op("act", lambda: nc.scalar.activation(out=pTa[p][:, 0:256], in_=P.banks[s0][:, 0:256], func=AF.Exp, scale=sc),
                         reads=[P.bb[s0]], writes=[b_pTa[p]])
                    T.op("act", lambda: nc.scalar.activation(out=pTa[p][:, 256:640], in_=P.banks[s1][:, 0:384], func=AF.Exp, scale=sc),
                         reads=[P.bb[s1]], writes=[b_pTa[p]])
                    T.op("pool", lambda: nc.gpsimd.tensor_tensor(out=pTa[p][:, 256:640], in0=pTa[p][:, 256:640], in1=msk[:, t, :], op=ALU.mult),
                         reads=[b_pTa[p], b_msk], writes=[b_pTa[p]])
                    for bi, w in enumerate(blocks):
                        first = (hh == 0 and bi == 0)
                        last = (hh == 1 and bi == 4)
                        T.op("pe", lambda: nc.tensor.matmul(P.banks[ob][:, 0:128], lhsT=vaw[:, kvh * 2 + hh, w, :], rhs=pTa[p][:, bi * 128:(bi + 1) * 128],
                                                            start=first, stop=last), reads=[b_vaw, b_pTa[p]], writes=[P.bb[ob]])
                        T.op("pe", lambda: nc.tensor.matmul(P.banks[sbk][:, 0:128], lhsT=oneslh[:, hh, :], rhs=pTa[p][:, bi * 128:(bi + 1) * 128],
                                                            start=first, stop=last), reads=[b_olh, b_pTa[p]], writes=[P.bb[sbk]])
                r = fi % 2
                fi += 1
                T.op("dve", lambda: nc.vector.tensor_scalar(out=den[r][:], in0=P.banks[sbk][:, 0:128], scalar1=esink[:, c:c + 1], scalar2=None, op0=ALU.add),
                     reads=[P.bb[sbk], b_es], writes=[b_den[r]])
                T.op("dve", lambda: nc.vector.reciprocal(out=den[r][:], in_=den[r][:]), reads=[b_den[r]], writes=[b_den[r]])
                T.op("dve", lambda: nc.vector.tensor_tensor(out=OT[:, c, t * 128:(t + 1) * 128], in0=P.banks[ob][:, 0:128], in1=den[r][:], op=ALU.mult),
                     reads=[P.bb[ob], b_den[r]], writes=[b_OT])
    T.barrier()
    with ExitStack() as es:
        S = SweepCtx(P, es)
        ktb = P.sb(es, [128, NKB * 128], BF16, "ktb")
        vtb = P.sb(es, [128, NKB, 128], BF16, "vtb")
        b_kv = Buf("kvb")
        qtb = [P.sb(es, [128, TOK], BF16, "qtb") for _ in range(2)]
        b_qtb = [Buf("qtb%d" % i) for i in range(2)]
        lamv = P.sb(es, [128, 4, 64], F32, "lamv")
        b_lamv = Buf("lamv")
        for i in range(4):
            T.dma("sp", lamv[:, i, :], lamv_d[i:i + 1, :].partition_broadcast(128), writes=[b_lamv])
        lami = P.sb(es, [128, 2], F32, "lami")
        b_lami = Buf("lami")
        T.dma("sp", lami[:], lami_d[:, :], writes=[b_lami])
        lp = P.sb(es, [128, 2, 64], F32, "lp")
        b_lp = Buf("lp")
        ls = P.sb(es, [128, 4], F32, "ls")
        b_ls = Buf("ls")
        T.op("dve", lambda: nc.vector.tensor_tensor(out=lp[:, 0, :], in0=lamv[:, 0, :], in1=lamv[:, 1, :], op=ALU.mult), reads=[b_lamv], writes=[b_lp])
        T.op("dve", lambda: nc.vector.tensor_tensor(out=lp[:, 1, :], in0=lamv[:, 2, :], in1=lamv[:, 3, :], op=ALU.mult), reads=[b_lamv, b_lp], writes=[b_lp])
        T.op("dve", lambda: nc.vector.tensor_reduce(out=ls[:, 0:2], in_=lp[:], axis=AX.X, op=ALU.add), reads=[b_lp], writes=[b_ls])
        T.op("act", lambda: nc.scalar.activation(out=ls[:, 0:2], in_=ls[:, 0:2], func=AF.Exp), reads=[b_ls], writes=[b_ls])
        T.op("dve", lambda: nc.vector.tensor_tensor(out=ls[:, 2:3], in0=ls[:, 1:2], in1=ls[:, 0:1], op=ALU.subtract), reads=[b_ls], writes=[b_ls])
        T.op("dve", lambda: nc.vector.tensor_tensor(out=ls[:, 2:3], in0=ls[:, 2:3], in1=lami[:, 0:1], op=ALU.subtract), reads=[b_ls, b_lami], writes=[b_ls])
        gsc = P.sb(es, [128, 1], F32, "gsc")
        b_gsc = Buf("gsc")
        T.dma("sp", gsc[:], subg_d[:, :], writes=[b_gsc])
        T.op("dve", lambda: nc.vector.tensor_tensor(out=gsc[:], in0=gsc[:], in1=lami[:, 1:2], op=ALU.mult), reads=[b_gsc, b_lami], writes=[b_gsc])
        epss = P.sb(es, [128, 1], F32, "epss")
        b_epss = Buf("epss")
        T.op("pool", lambda: nc.gpsimd.memset(epss[:], SUBLN_EPS), writes=[b_epss])
        rec = P.sb(es, [128, 512], F32, "recb")
        b_rec = Buf("recb")
        am = [P.sb(es, [128, 512], F32, "am") for _ in range(2)]
        b_am = [Buf("am%d" % i) for i in range(2)]
        dm = P.sb(es, [128, 512], F32, "dm")
        b_dm = Buf("dm")
        sq = P.sb(es, [128, 512], F32, "sqb")
        b_sq = Buf("sqb")
        rstd = P.sb(es, [128, 512], F32, "rstd")
        b_rstd = Buf("rstd")
        for h in range(4):
            T.dma("sp", ktb[:], KTb_d[h, :, :], writes=[b_kv])
            T.dma("sp", vtb[:], Vb_d[h, :, :, :], writes=[b_kv])
            qi = h % 2
            T.dma("sp", qtb[qi][:], QTb_d[h, :, :], writes=[b_qtb[qi]])
            for (c0, N) in CHUNKS:
                blocks = ALL_BLOCKS if N == 512 else CTX_BLOCKS
                for mm in range(2):
                    msl = slice(mm * 64, mm * 64 + 64)
                    ob, sbk = sweep(P, S, N, blocks, qtb[qi][msl, c0:c0 + N], b_qtb[qi],
                                    lambda b: ktb[msl, b * 128:(b + 1) * 128], lambda b: vtb[:, b, :], b_kv, 128, sc, P.ones[:], P.b_ones)
                    T.op("dve", lambda: nc.vector.reciprocal(out=rec[:, 0:N], in_=P.banks[sbk][:, 0:N]), reads=[P.bb[sbk]], writes=[b_rec])
                    T.op("dve", lambda: nc.vector.tensor_tensor(out=am[mm][:, 0:N], in0=P.banks[ob][:, 0:N], in1=rec[:, 0:N], op=ALU.mult),
                         reads=[P.bb[ob], b_rec], writes=[b_am[mm]])
                T.op("dve", lambda: nc.vector.scalar_tensor_tensor(out=dm[:, 0:N], in0=am[1][:, 0:N], scalar=ls[:, 2:3], in1=am[0][:, 0:N],
                                                                   op0=ALU.mult, op1=ALU.add), reads=[b_am[0], b_am[1], b_ls], writes=[b_dm])
                T.op("act", lambda: nc.scalar.activation(out=sq[:, 0:N], in_=dm[:, 0:N], func=AF.Square), reads=[b_dm], writes=[b_sq])
                T.op("pe", lambda: nc.tensor.matmul(P.banks[6][:, 0:N], lhsT=P.onesd[:], rhs=sq[:, 0:N], start=True, stop=True),
                     reads=[P.b_onesd, b_sq], writes=[P.bb[6]])
                T.op("act", lambda: nc.scalar.activation(out=rstd[:, 0:N], in_=P.banks[6][:, 0:N], func=AF.Sqrt, bias=epss[:, 0:1], scale=1.0),
                     reads=[P.bb[6], b_epss], writes=[b_rstd])
                T.op("dve", lambda: nc.vector.reciprocal(out=rstd[:, 0:N], in_=rstd[:, 0:N]), reads=[b_rstd], writes=[b_rstd])
                T.op("dve", lambda: nc.vector.scalar_tensor_tensor(out=OT[:, 4 + h, c0:c0 + N], in0=dm[:, 0:N], scalar=gsc[:, 0:1], in1=rstd[:, 0:N],
                                                                   op0=ALU.mult, op1=ALU.mult), reads=[b_dm, b_gsc, b_rstd], writes=[b_OT])
    T.barrier()


def layer_norm_tile(P, L, src, b_src, dst, b_dst, gam, bet, b_gb):
    nc, T = P.nc, P.T
    st, b_st, mv, b_mv, xn, b_xn = L
    T.op("dve", lambda: nc.vector.bn_stats(out=st[:, 0, :], in_=src[:, 0:512]), reads=[b_src], writes=[b_st])
    T.op("dve", lambda: nc.vector.bn_stats(out=st[:, 1, :], in_=src[:, 512:1024]), reads=[b_src, b_st], writes=[b_st])
    T.op("dve", lambda: nc.vector.bn_aggr(out=mv[:, 0:2], in_=st[:].rearrange("p a s -> p (a s)")), reads=[b_st], writes=[b_mv])
    T.op("act", lambda: nc.scalar.activation(out=mv[:, 2:3], in_=mv[:, 1:2], func=AF.Sqrt, bias=mv[:, 3:4], scale=1.0), reads=[b_mv], writes=[b_mv])
    T.op("dve", lambda: nc.vector.reciprocal(out=mv[:, 2:3], in_=mv[:, 2:3]), reads=[b_mv], writes=[b_mv])
    T.op("dve", lambda: nc.vector.tensor_scalar(out=xn[:], in0=src, scalar1=mv[:, 0:1], scalar2=mv[:, 2:3], op0=ALU.subtract, op1=ALU.mult),
         reads=[b_src, b_mv], writes=[b_xn])
    T.op("pool", lambda: nc.gpsimd.tensor_tensor(out=xn[:], in0=xn[:], in1=gam, op=ALU.mult), reads=[b_xn, b_gb], writes=[b_xn])
    T.op("pool", lambda: nc.gpsimd.tensor_tensor(out=dst, in0=xn[:], in1=bet, op=ALU.add), reads=[b_xn, b_gb], writes=[b_dst])


def ln_scratch(P, es, tag):
    nc, T = P.nc, P.T
    st = P.sb(es, [128, 2, 6], F32, "lnst")
    mv = P.sb(es, [128, 4], F32, "lnmv")
    xn = P.sb(es, [128, D], F32, "lnxn")
    b_mv = Buf("lnmv" + tag)
    T.op("pool", lambda: nc.gpsimd.memset(mv[:, 3:4], LN_EPS), writes=[b_mv])
    return (st, Buf("lnst" + tag), mv, b_mv, xn, Buf("lnxn" + tag))


def phase_C1(P, OT, b_OT, x_d, wo_d, gbc_d, lng_d, lnb_d, x1_d, x1b):
    nc, T = P.nc, P.T
    with ExitStack() as es:
        wo, wob = load_weight_bf16(P, es, wo_d, 8, D, "wo")
        gb = P.sb(es, [128, 2, D], F32, "g1bc")
        b_gb = Buf("g1bc")
        for who in range(2):
            T.dma("sp", gb[:, who, :], gbc_d[who, 0:1, :].partition_broadcast(128), writes=[b_gb])
        ln = P.sb(es, [128, 2, D], F32, "ln1")
        b_ln = Buf("ln1")
        T.dma("sp", ln[:, 0, :], lng_d[0:1, :].partition_broadcast(128), writes=[b_ln])
        T.dma("sp", ln[:, 1, :], lnb_d[0:1, :].partition_broadcast(128), writes=[b_ln])
        L = ln_scratch(P, es, "1")
        xt = [P.sb(es, [128, D], F32, "cxt") for _ in range(2)]
        b_xt = [Buf("cxt%d" % i) for i in range(2)]
        rr = [P.sb(es, [128, D], F32, "crr") for _ in range(2)]
        b_rr = [Buf("crr%d" % i) for i in range(2)]
        x1t = [P.sb(es, [128, D], F32, "x1t") for _ in range(2)]
        b_x1t = [Buf("x1t%d" % i) for i in range(2)]
        for t in range(NT):
            i = t % 2
            who = 1 if t == NT - 1 else 0
            yb = (0, 1) if i == 0 else (2, 3)
            T.dma("sp", xt[i][:], x_d[t * 128:(t + 1) * 128, :], writes=[b_xt[i]])
            for half in range(2):
                for k in range(8):
                    T.op("pe", lambda: nc.tensor.matmul(P.banks[yb[half]][:, :], lhsT=OT[:, k, t * 128:(t + 1) * 128],
                                                        rhs=wo[:, k, half * 512:(half + 1) * 512], start=(k == 0), stop=(k == 7)),
                         reads=[b_OT, wob[half * 4]], writes=[P.bb[yb[half]]])
                T.op("dve", lambda: nc.vector.tensor_tensor(out=rr[i][:, half * 512:(half + 1) * 512], in0=P.banks[yb[half]][:, :],
                                                            in1=gb[:, who, half * 512:(half + 1) * 512], op=ALU.mult),
                     reads=[P.bb[yb[half]], b_gb], writes=[b_rr[i]])
            T.op("dve", lambda: nc.vector.scalar_tensor_tensor(out=rr[i][:], in0=xt[i][:], scalar=ALPHA, in1=rr[i][:], op0=ALU.mult, op1=ALU.add),
                 reads=[b_xt[i], b_rr[i]], writes=[b_rr[i]])
            layer_norm_tile(P, L, rr[i][:], b_rr[i], x1t[i][:], b_x1t[i], ln[:, 0, :], ln[:, 1, :], b_ln)
            T.dma("pool", x1_d[t * 128:(t + 1) * 128, :], x1t[i][:], reads=[b_x1t[i]], writes=[x1b[t]])
    T.barrier()


def phase_C2(P, x1_d, x1b, mC_d, gbc_d, lng_d, lnb_d, wr_d, br_d, wg_d, wu_d, wd_d, xo_d, xob):
    nc, T = P.nc, P.T
    with ExitStack() as es:
        acc = P.sb(es, [128, NT, D], F32, "acc")
        accb = [Buf("acc%d" % t) for t in range(NT)]
        h2T = P.sb(es, [128, 8, TOK], BF16, "h2T")
        h2b = [Buf("h2T%d" % c) for c in range(len(CHUNKS))]
        m, b_m = load_modT(P, es, mC_d, "mC")
        gb = P.sb(es, [128, 2, D], F32, "g2bc")
        b_gb = Buf("g2bc")
        for who in range(2):
            T.dma("sp", gb[:, who, :], gbc_d[who, 1:2, :].partition_broadcast(128), writes=[b_gb])
        wr = P.sb(es, [128, 8, NEXP], F32, "wr")
        b_wr = Buf("wr")
        T.dma("sp", wr[:], wr_d.rearrange("(k p) e -> p k e", p=128), writes=[b_wr])
        brt = P.sb(es, [128, NEXP], F32, "brt")
        b_brt = Buf("brt")
        T.dma("sp", brt[:], br_d[0:1, :].partition_broadcast(128), writes=[b_brt])
        scs = P.sb(es, [128, NT, NEXP], F32, "scs")
        b_scs = Buf("scs")
        gates = P.sb(es, [128, NT, NEXP], F32, "gates")
        b_gates = Buf("gates")
        with ExitStack() as es2:
            hTf = [P.sb(es2, [128, 8, 128], F32, "hTf") for _ in range(2)]
            b_hTf = [Buf("hTf%d" % i) for i in range(2)]
            for t in range(int(os.environ.get("K_NT", NT))):
                i = t % 2
                who = 1 if t == NT - 1 else 0
                ch = min(t // 4, 4)
                T.dma("sp", acc[:, t, :], x1_d[t * 128:(t + 1) * 128, :], reads=[x1b[t]], writes=[accb[t]])
                transpose_mod(P, acc[:, t, :], accb[t], m[:, who, 0, :], m[:, who, 1, :], b_m, h2T[:, :, t * 128:(t + 1) * 128], h2b[ch],
                              (0, 1) if i == 0 else (2, 3), None if "nohtf" in os.environ.get("K_DBG", "") else hTf[i], b_hTf[i])
                lb = 4 + i
                DBG = os.environ.get("K_DBG", "")
                if "nologit" not in DBG:
                    for k in range(8):
                        T.op("pe", lambda: nc.tensor.matmul(P.banks[lb][:, 0:NEXP], lhsT=hTf[i][:, k, :], rhs=wr[:, k, :], start=(k == 0), stop=(k == 7)),
                             reads=[b_hTf[i], b_wr], writes=[P.bb[lb]])
                    if "nosig" not in DBG:
                        T.op("act", lambda: nc.scalar.activation(out=scs[:, t, :], in_=P.banks[lb][:, 0:NEXP], func=AF.Sigmoid), reads=[P.bb[lb]], writes=[b_scs])
                if "nopool" not in DBG:
                    T.op("pool", lambda: nc.gpsimd.tensor_scalar(out=acc[:, t, :], in0=acc[:, t, :], scalar1=ALPHA, scalar2=None, op0=ALU.mult),
                         reads=[accb[t]], writes=[accb[t]])
            if "noroute" in os.environ.get("K_DBG", ""):
                return
            G = NT * 4
            sel = P.sb(es2, [128, NT, NEXP], F32, "sel")
            sel2 = P.sb(es2, [128, NT, NEXP], F32, "sel2")
            eq = P.sb(es2, [128, NT, NEXP], F32, "eq")
            m1 = P.sb(es2, [128, G], F32, "m1")
            m2 = P.sb(es2, [128, G], F32, "m2")
            gs = P.sb(es2, [128, G], F32, "gs")
            gmx = P.sb(es2, [128, NT], F32, "gmx")
            b_r = Buf("route")
            g4 = lambda a: a[:].rearrange("p t (g j) -> p (t g) j", j=4)
            bc4 = lambda a: a[:].unsqueeze(2).to_broadcast([128, G, 4])
            R = dict(reads=[b_r, b_scs, b_brt], writes=[b_r])
            T.op("dve", lambda: nc.vector.tensor_tensor(out=sel[:], in0=scs[:], in1=brt[:].unsqueeze(1).to_broadcast([128, NT, NEXP]), op=ALU.add), **R)
            T.op("dve", lambda: nc.vector.tensor_reduce(out=m1[:], in_=g4(sel), axis=AX.X, op=ALU.max), **R)
            T.op("dve", lambda: nc.vector.tensor_tensor(out=g4(eq), in0=g4(sel), in1=bc4(m1), op=ALU.is_equal), **R)
            T.op("dve", lambda: nc.vector.scalar_tensor_tensor(out=sel2[:], in0=eq[:], scalar=-1.0e9, in1=sel[:], op0=ALU.mult, op1=ALU.add), **R)
            T.op("dve", lambda: nc.vector.tensor_reduce(out=m2[:], in_=g4(sel2), axis=AX.X, op=ALU.max), **R)
            T.op("dve", lambda: nc.vector.tensor_tensor(out=gs[:], in0=m1[:], in1=m2[:], op=ALU.add), **R)
            T.op("dve", lambda: nc.vector.tensor_reduce(out=gmx[:], in_=gs[:].rearrange("p (t g) -> p t g", g=4), axis=AX.X, op=ALU.max), **R)
            T.op("dve", lambda: nc.vector.tensor_tensor(out=gs[:].rearrange("p (t g) -> p t g", g=4), in0=gs[:].rearrange("p (t g) -> p t g", g=4),
                                                        in1=gmx[:].unsqueeze(2).to_broadcast([128, NT, 4]), op=ALU.is_equal), **R)
            T.op("dve", lambda: nc.vector.tensor_tensor(out=g4(eq), in0=g4(sel), in1=bc4(m2), op=ALU.is_ge), **R)
            T.op("dve", lambda: nc.vector.tensor_tensor(out=g4(eq), in0=g4(eq), in1=bc4(gs), op=ALU.mult), **R)
            T.op("dve", lambda: nc.vector.tensor_tensor(out=sel[:], in0=scs[:], in1=eq[:], op=ALU.mult), **R)
            T.op("dve", lambda: nc.vector.tensor_reduce(out=gmx[:], in_=sel[:], axis=AX.X, op=ALU.add), **R)
            T.op("dve", lambda: nc.vector.reciprocal(out=gmx[:], in_=gmx[:]), **R)
            T.op("dve", lambda: nc.vector.tensor_tensor(out=gates[:], in0=sel[:], in1=gmx[:].unsqueeze(2).to_broadcast([128, NT, NEXP]), op=ALU.mult),
                 reads=[b_r], writes=[b_gates])
        T.barrier()
        DBG = os.environ.get("K_DBG", "")
        if "noexp" in DBG:
            return
        with ExitStack() as es3:
            wg = [P.sb(es3, [128, 8, DEXP], BF16, "wg") for _ in range(2)]
            wu = [P.sb(es3, [128, 8, DEXP], BF16, "wu") for _ in range(2)]
            wd = [P.sb(es3, [128, 4, D], BF16, "wd") for _ in range(2)]
            wbuf = [[[Buf("w%d_%d_%d" % (s_, m_, h_)) for h_ in range(2)] for m_ in range(3)] for s_ in range(2)]
            NSTG = 2
            stg = [P.sb(es3, [128, 2048], F32, "wstg") for _ in range(NSTG)]
            b_stg = [Buf("wstg%d" % i) for i in range(NSTG)]
            sg = [P.sb(es3, [128, 512], F32, "sg") for _ in range(2)]
            b_sg = [Buf("sg%d" % i) for i in range(2)]
            aT = [P.sb(es3, [128, 4, 512], BF16, "aT") for _ in range(2)]
            b_aT = [Buf("aT%d" % i) for i in range(2)]
            tmp = [P.sb(es3, [128, D], F32, "mtmp") for _ in range(2)]
            b_tmp = [Buf("mtmp%d" % i) for i in range(2)]
            si = 0
            ci = 0
            fi = 0
            ti = 0
            for e in range(NEXP if "exp1" not in DBG else 1):
                s_ = e % 2
                for m_, (src, dst, K) in enumerate(((wg_d, wg[s_], 8), (wu_d, wu[s_], 8), (wd_d, wd[s_], 4))):
                    for h_ in range(2):
                        st = stg[si % NSTG]
                        bs = b_stg[si % NSTG]
                        si += 1
                        kh = K // 2
                        ncol = 2048 // kh
                        T.dma("sp", st[:].rearrange("p (k n) -> p k n", k=kh),
                              src[e, h_ * kh * 128:(h_ + 1) * kh * 128, :].rearrange("(k p) n -> p k n", p=128), writes=[bs])
                        T.op("act", lambda: nc.scalar.copy(out=dst[:, h_ * kh:(h_ + 1) * kh, :], in_=st[:].rearrange("p (k n) -> p k n", k=kh)),
                             reads=[bs], writes=[wbuf[s_][m_][h_]])
                for chn, (c0, N) in enumerate(CHUNKS):
                    a = ci % 2
                    ci += 1
                    for f in range(4):
                        gbk = fi % 2
                        ubk = 2 + fi % 2
                        fi += 1
                        for k in range(8):
                            T.op("pe", lambda: nc.tensor.matmul(P.banks[gbk][:, 0:N], lhsT=wg[s_][:, k, f * 128:(f + 1) * 128], rhs=h2T[:, k, c0:c0 + N],
                                                                start=(k == 0), stop=(k == 7)), reads=[wbuf[s_][0][k // 4], h2b[chn]], writes=[P.bb[gbk]])
                        for k in range(8):
                            T.op("pe", lambda: nc.tensor.matmul(P.banks[ubk][:, 0:N], lhsT=wu[s_][:, k, f * 128:(f + 1) * 128], rhs=h2T[:, k, c0:c0 + N],
                                                                start=(k == 0), stop=(k == 7)), reads=[wbuf[s_][1][k // 4], h2b[chn]], writes=[P.bb[ubk]])
                        T.op("act", lambda: nc.scalar.activation(out=sg[gbk][:, 0:N], in_=P.banks[gbk][:, 0:N], func=AF.Silu),
                             reads=[P.bb[gbk]], writes=[b_sg[gbk]])
                        T.op("dve", lambda: nc.vector.tensor_tensor(out=aT[a][:, f, 0:N], in0=sg[gbk][:, 0:N], in1=P.banks[ubk][:, 0:N], op=ALU.mult),
                             reads=[b_sg[gbk], P.bb[ubk]], writes=[b_aT[a]])
                    for tt in range(N // 128):
                        t = c0 // 128 + tt
                        who = 1 if t == NT - 1 else 0
                        yb = (4, 5) if ti % 2 == 0 else (6, 7)
                        tm = ti % 2
                        ti += 1
                        for half in range(2):
                            for f in range(4):
                                T.op("pe", lambda: nc.tensor.matmul(P.banks[yb[half]][:, :], lhsT=aT[a][:, f, tt * 128:(tt + 1) * 128],
                                                                    rhs=wd[s_][:, f, half * 512:(half + 1) * 512], start=(f == 0), stop=(f == 3)),
                                     reads=[b_aT[a], wbuf[s_][2][f // 2]], writes=[P.bb[yb[half]]])
                            T.op("dve", lambda: nc.vector.scalar_tensor_tensor(out=tmp[tm][:, half * 512:(half + 1) * 512], in0=P.banks[yb[half]][:, :],
                                                                               scalar=gates[:, t, e:e + 1], in1=gb[:, who, half * 512:(half + 1) * 512],
                                                                               op0=ALU.mult, op1=ALU.mult),
                                 reads=[P.bb[yb[half]], b_gates, b_gb], writes=[b_tmp[tm]])
                        T.op("pool", lambda: nc.gpsimd.tensor_tensor(out=acc[:, t, :], in0=acc[:, t, :], in1=tmp[tm][:], op=ALU.add),
                             reads=[accb[t], b_tmp[tm]], writes=[accb[t]])
        T.barrier()
        with ExitStack() as es4:
            L = ln_scratch(P, es4, "2")
            ln = P.sb(es4, [128, 2, D], F32, "ln2")
            b_ln = Buf("ln2")
            T.dma("sp", ln[:, 0, :], lng_d[1:2, :].partition_broadcast(128), writes=[b_ln])
            T.dma("sp", ln[:, 1, :], lnb_d[1:2, :].partition_broadcast(128), writes=[b_ln])
            xo = [P.sb(es4, [128, D], F32, "xo") for _ in range(2)]
            b_xo = [Buf("xo%d" % i) for i in range(2)]
            for t in range(NT):
                i = t % 2
                layer_norm_tile(P, L, acc[:, t, :], accb[t], xo[i][:], b_xo[i], ln[:, 0, :], ln[:, 1, :], b_ln)
                T.dma("pool", xo_d[t * 128:(t + 1) * 128, :], xo[i][:], reads=[b_xo[i]], writes=[xob[t]])
    T.barrier()


def rope_tables(hd):
    q = hd // 4
    inv = (10000.0 ** (-np.arange(q, dtype=np.float32) / np.float32(q))).astype(np.float32)
    tpos = np.arange(SEQ)
    ang_r = (tpos // 64).astype(np.float32)[:, None] * inv[None, :]
    ang_c = (tpos % 64).astype(np.float32)[:, None] * inv[None, :]
    cos = np.concatenate([np.cos(ang_r), np.cos(ang_c)], axis=1).astype(np.float32)
    sin = np.concatenate([np.sin(ang_r), np.sin(ang_c)], axis=1).astype(np.float32)
    cos_c = np.ones((NCORES, TOK, 2 * q), np.float32)
    sin_c = np.zeros((NCORES, TOK, 2 * q), np.float32)
    for r in range(NCORES):
        cos_c[r, :2048] = cos[r * 2048:(r + 1) * 2048]
        sin_c[r, :2048] = sin[r * 2048:(r + 1) * 2048]
    return cos_c, sin_c


IDENT = np.eye(128, dtype=np.float32)
_PROG_CACHE = {}


def run(nc, in_maps):
    return run_bass_kernel_spmd(nc, in_maps, core_ids=list(range(NCORES))).results


def modT_layout(modv, l, cols):
    out = np.empty((128, 2, len(cols), 8), np.float32)
    for who in range(2):
        for j, c in enumerate(cols):
            out[:, who, j, :] = modv[l, who, c * 1024:(c + 1) * 1024].reshape(8, 128).T
    return out


def build_mod():
    if "mod" not in _PROG_CACHE:
        P = Prog()
        cc = P.inp("cc", [128, 8, 2], F32)
        wm = P.inp("wm", [DEPTH, D, 768], F32)
        bm = P.inp("bm", [DEPTH, 2, 768], F32)
        out = P.outp("modp", [DEPTH, 2, 768], F32)
        phase_mod(P, cc, wm, bm, out, 768)
        _PROG_CACHE["mod"] = P.close()
    return _PROG_CACHE["mod"]


def run_mod(c, c_ctx, w_mod, b_mod):
    nc = build_mod()
    cc = np.stack([c.reshape(128, 8), c_ctx.reshape(128, 8)], axis=-1).astype(np.float32)
    maps = []
    for r in range(NCORES):
        sl = slice(r * 768, (r + 1) * 768)
        maps.append({"ident": IDENT, "cc": cc, "wm": np.ascontiguousarray(w_mod[:, :, sl]),
                     "bm": np.ascontiguousarray(np.repeat(b_mod[:, None, sl], 2, axis=1))})
    res = run(nc, maps)
    return np.concatenate([res[r]["modp"] for r in range(NCORES)], axis=2)


def build_A(even):
    key = "A%d" % even
    if key not in _PROG_CACHE:
        P = Prog()
        ncols = E_COLS if even else O_COLS
        hq = 32 if even else 64
        x = P.inp("x", [TOK, D], F32)
        w = P.inp("w_in", [D, ncols], F32)
        mA = P.inp("mA", [128, 2, 2, 8], F32)
        cos = P.inp("cos", [TOK, hq], F32)
        sin = P.inp("sin", [TOK, hq], F32)
        qng = None if even else P.inp("qng", [2, 128], F32)
        qkv = P.outp("qkv", [TOK, ncols], BF16)
        phase_A(P, even, x, None, w, mA, cos, sin, qng, qkv)
        _PROG_CACHE[key] = P.close()
    return _PROG_CACHE[key]


def gather_tokens(parts, c0, c1):
    ctx = np.concatenate([parts[0][2048:, c0:c1], parts[1][2048:, c0:c1]], axis=0)
    lat = np.concatenate([p[:2048, c0:c1] for p in parts], axis=0)
    return np.concatenate([ctx, lat], axis=0)


def layout_odd(qkv):
    k_all = gather_tokens(qkv, 1024, 1280)
    v_all = gather_tokens(qkv, 1280, 1536)
    KT = np.ascontiguousarray(k_all.reshape(NKB * 128, 2, 128).transpose(1, 2, 0))
    V = np.ascontiguousarray(v_all.reshape(NKB, 128, 2, 128).transpose(2, 1, 0, 3))
    maps = []
    for r in range(NCORES):
        QT = np.ascontiguousarray(qkv[r][:, 0:1024].reshape(TOK, 8, 128).transpose(1, 2, 0))
        maps.append({"QT": QT, "KT": KT, "V": V})
    return maps


def window_masks():
    m = np.zeros((NCORES, 128, NT, 384), np.float32)
    jj = np.arange(128)[:, None]
    ii = np.arange(128)[None, :]
    left = (ii <= jj).astype(np.float32)
    right = (jj <= ii).astype(np.float32)
    for r in range(NCORES):
        for t in range(16):
            n = 16 * r + t
            if n - 1 >= 0:
                m[r, :, t, 0:128] = left
            m[r, :, t, 128:256] = 1.0
            if n + 1 < SEQ // 128:
                m[r, :, t, 256:384] = right
    return m.astype(NPBF)


def layout_even(qkv, sink, lam4, lam_init, subln_g):
    ak = gather_tokens(qkv, 512, 640)
    av = gather_tokens(qkv, 640, 768)
    bk = gather_tokens(qkv, 1280, 1792)
    bv = gather_tokens(qkv, 1792, 2304)
    KTb = np.ascontiguousarray(bk.reshape(NKB * 128, 4, 128).transpose(1, 2, 0))
    Vb = np.ascontiguousarray(bv.reshape(NKB, 128, 4, 128).transpose(2, 1, 0, 3))
    masks = window_masks()
    sinkp = np.empty((128, 4), np.float32)
    for c in range(4):
        sinkp[:64, c] = sink[2 * c]
        sinkp[64:, c] = sink[2 * c + 1]
    lami = np.empty((128, 2), np.float32)
    lami[:, 0] = lam_init
    lami[:, 1] = 1.0 - lam_init
    akb = ak.reshape(NKB, 128, 128)
    avb = av.reshape(NKB, 128, 2, 64)
    maps = []
    for r in range(NCORES):
        q = qkv[r]
        QTb = np.ascontiguousarray(q[:, 768:1280].reshape(TOK, 4, 128).transpose(1, 2, 0))
        QTa = np.ascontiguousarray(q[:, 0:512].reshape(TOK, 2, 4, 64).transpose(1, 3, 2, 0).reshape(128, 4, TOK))
        kw = np.zeros((20, 128, 128), ak.dtype)
        vw = np.zeros((20, 128, 2, 64), av.dtype)
        kw[0:2] = akb[0:2]
        vw[0:2] = avb[0:2]
        for w in range(2, 20):
            n = 16 * r - 1 + (w - 2)
            if 0 <= n < SEQ // 128:
                kw[w] = akb[2 + n]
                vw[w] = avb[2 + n]
        KTa = np.ascontiguousarray(kw.transpose(2, 0, 1).reshape(128, 20 * 128))
        Va = np.zeros((128, 4, 20, 128), av.dtype)
        for kvh in range(2):
            for lohi in range(2):
                Va[:, kvh * 2 + lohi, :, lohi * 64:lohi * 64 + 64] = vw[:, :, kvh, :].transpose(1, 0, 2)
        maps.append({"QTa": QTa, "KTa": KTa, "Va": Va, "mask": masks[r], "sinkp": sinkp, "QTb": QTb, "KTb": KTb, "Vb": Vb,
                     "lamv": np.ascontiguousarray(lam4.astype(np.float32)), "lami": lami,
                     "subg": np.ascontiguousarray(subln_g.reshape(128, 1).astype(np.float32))})
    return maps


def decl_B(P, even):
    if even:
        return dict(QTa=P.inp("QTa", [128, 4, TOK], BF16), KTa=P.inp("KTa", [128, 20 * 128], BF16), Va=P.inp("Va", [128, 4, 20, 128], BF16),
                    mask=P.inp("mask", [128, NT, 384], BF16), sinkp=P.inp("sinkp", [128, 4], F32), QTb=P.inp("QTb", [4, 128, TOK], BF16),
                    KTb=P.inp("KTb", [4, 128, NKB * 128], BF16), Vb=P.inp("Vb", [4, 128, NKB, 128], BF16), lamv=P.inp("lamv", [4, 64], F32),
                    lami=P.inp("lami", [128, 2], F32), subg=P.inp("subg", [128, 1], F32))
    return dict(QT=P.inp("QT", [8, 128, TOK], BF16), KT=P.inp("KT", [2, 128, NKB * 128], BF16), V=P.inp("V", [2, 128, NKB, 128], BF16))


def emit_B(P, even, OT, b_OT, d):
    if even:
        phase_B_even(P, OT, b_OT, d["QTa"], d["KTa"], d["Va"], d["mask"], d["sinkp"], d["QTb"], d["KTb"], d["Vb"], d["lamv"], d["lami"], d["subg"])
    else:
        phase_B_odd(P, OT, b_OT, d["QT"], d["KT"], d["V"])


def build_Btest(even):
    key = "Bt%d" % even
    if key not in _PROG_CACHE:
        P = Prog()
        d = decl_B(P, even)
        out = P.outp("OT", [128, 8, TOK], BF16)
        OT = P.sb(P.es, [128, 8, TOK], BF16, "OT")
        b_OT = Buf("OT")
        emit_B(P, even, OT, b_OT, d)
        P.T.dma("pool", out[:, :, :], OT[:], reads=[b_OT])
        _PROG_CACHE[key] = P.close()
    return _PROG_CACHE[key]


def build_BCA(even, has_A):
    key = "BCA%d%d" % (even, has_A)
    if key in _PROG_CACHE:
        return _PROG_CACHE[key]
    P = Prog()
    nc = P.nc
    dB = decl_B(P, even)
    x = P.inp("x", [TOK, D], F32)
    wo = P.inp("w_out", [D, D], F32)
    gbc = P.inp("gbc", [2, 2, D], F32)
    mC = P.inp("mC", [128, 2, 2, 8], F32)
    lng = P.inp("lng", [2, D], F32)
    lnb = P.inp("lnb", [2, D], F32)
    wr = P.inp("wr", [D, NEXP], F32)
    br = P.inp("br", [1, NEXP], F32)
    wg = P.inp("wg", [NEXP, D, DEXP], F32)
    wu = P.inp("wu", [NEXP, D, DEXP], F32)
    wd = P.inp("wd", [NEXP, DEXP, D], F32)
    x1s = nc.dram_tensor("x1s", [TOK, D], F32, kind="Internal").ap()
    xo = P.outp("xo", [TOK, D], F32)
    x1b = [Buf("x1s%d" % t) for t in range(NT)]
    xob = [Buf("xo%d" % t) for t in range(NT)]
    if has_A:
        a_even = not even
        ncols = E_COLS if a_even else O_COLS
        hq = 32 if a_even else 64
        w_in = P.inp("w_in", [D, ncols], F32)
        mA = P.inp("mA", [128, 2, 2, 8], F32)
        cos = P.inp("cos", [TOK, hq], F32)
        sin = P.inp("sin", [TOK, hq], F32)
        qng = None if a_even else P.inp("qng", [2, 128], F32)
        qkv = P.outp("qkv", [TOK, ncols], BF16)
    with ExitStack() as es:
        OT = P.sb(es, [128, 8, TOK], BF16, "OT")
        b_OT = Buf("OT")
        emit_B(P, even, OT, b_OT, dB)
        phase_C1(P, OT, b_OT, x, wo, gbc, lng, lnb, x1s, x1b)
    if "noC2" not in os.environ.get("K_DBG", ""):
        phase_C2(P, x1s, x1b, mC, gbc, lng, lnb, wr, br, wg, wu, wd, xo, xob)
    if has_A and "noA" not in os.environ.get("K_DBG", ""):
        phase_A(P, a_even, xo, xob, w_in, mA, cos, sin, qng, qkv)
    print("BCA program: %d instructions, %d waits, %d dma sems" % (P.T.n_ins, P.T.n_wait, P.T.nsem), flush=True)
    _PROG_CACHE[key] = P.close()
    return _PROG_CACHE[key]


def a_inputs(inputs, modv, l, r, tabs):
    even = (l % 2 == 0)
    i = l // 2
    cos_c, sin_c = tabs[64 if even else 128]
    m = {"w_in": inputs["w_in_even"][i] if even else inputs["w_in_odd"][i], "mA": modT_layout(modv, l, [1, 0]),
         "cos": cos_c[r], "sin": sin_c[r]}
    if not even:
        m["qng"] = np.stack([inputs["q_norm_g"][i], inputs["k_norm_g"][i]]).astype(np.float32)
    return m


def kernel(**inputs):
    inputs = {k: np.asarray(v) for k, v in inputs.items()}
    x = inputs["x"][0]
    ctx = inputs["ctx"][0]
    tabs = {64: rope_tables(64), 128: rope_tables(128)}
    modv = run_mod(inputs["c"][0], inputs["c_ctx"], inputs["w_mod"], inputs["b_mod"])
    xres = [np.ascontiguousarray(np.concatenate([x[r * 2048:(r + 1) * 2048], ctx[(r % 2) * 128:(r % 2) * 128 + 128]], 0)) for r in range(NCORES)]
    maps = []
    for r in range(NCORES):
        m = a_inputs(inputs, modv, 0, r, tabs)
        m.update({"ident": IDENT, "x": xres[r]})
        maps.append(m)
    res = run(build_A(True), maps)
    qkv = [res[r]["qkv"] for r in range(NCORES)]
    for l in range(DEPTH):
        even = (l % 2 == 0)
        i = l // 2
        has_A = l < DEPTH - 1
        if even:
            lam_init = 0.8 - 0.6 * float(np.exp(-0.3 * l))
            lam4 = np.stack([inputs["lam_q1"][i], inputs["lam_k1"][i], inputs["lam_q2"][i], inputs["lam_k2"][i]])
            maps = layout_even(qkv, inputs["sink_logits"][i], lam4, lam_init, inputs["subln_g"][i])
        else:
            maps = layout_odd(qkv)
        gbc = np.ascontiguousarray(np.stack([np.stack([modv[l, who, 2048:3072], modv[l, who, 5120:6144]]) for who in range(2)]))
        mC = modT_layout(modv, l, [4, 3])
        for r in range(NCORES):
            m = maps[r]
            m.update({"ident": IDENT, "x": xres[r], "w_out": inputs["w_out_even"][i] if even else inputs["w_out_odd"][i], "gbc": gbc, "mC": mC,
                      "lng": inputs["ln_g"][l], "lnb": inputs["ln_b"][l], "wr": inputs["w_router"], "br": inputs["b_router"].reshape(1, NEXP),
                      "wg": inputs["w_gate"][l], "wu": inputs["w_up"][l], "wd": inputs["w_down"][l]})
            if has_A:
                m.update(a_inputs(inputs, modv, l + 1, r, tabs))
        res = run(build_BCA(even, has_A), maps)
        xres = [res[r]["xo"] for r in range(NCORES)]
        if has_A:
            qkv = [res[r]["qkv"] for r in range(NCORES)]
    out = np.concatenate([xres[r][:2048] for r in range(NCORES)], axis=0)
    return out.reshape(1, SEQ, D).astype(np.float32)
```

```python
import os
import numpy as np
import ml_dtypes
from contextlib import ExitStack
import concourse.bass as bass
import concourse.mybir as mybir
from concourse.bass_utils import run_bass_kernel_spmd

F32 = mybir.dt.float32
BF16 = mybir.dt.bfloat16
AF = mybir.ActivationFunctionType
ALU = mybir.AluOpType
AX = mybir.AxisListType
NPBF = ml_dtypes.bfloat16

NCORES = 8
D = 1024
SEQ = 16384
CTX = 256
NT = 17
TOK = NT * 128
NKB = (SEQ + CTX) // 128
DEPTH = 4
ALPHA = (2 * DEPTH) ** 0.25
LN_EPS = 1e-5
QK_EPS = 1e-6
SUBLN_EPS = 1e-5
E_COLS = 2304
O_COLS = 1536
NEXP = 16
DEXP = 512
CHUNKS = [(0, 512), (512, 512), (1024, 512), (1536, 512), (2048, 128)]


class Buf:
    __slots__ = ("name", "w", "rs", "dsem", "dcnt", "excl")

    def __init__(self, name, excl=False):
        self.name = name
        self.w = None
        self.rs = {}
        self.dsem = None
        self.dcnt = 0
        self.excl = excl


class Trk:
    def __init__(self, nc, es):
        self.nc = nc
        self.es = es
        self.eng = {"pe": nc.tensor, "act": nc.scalar, "dve": nc.vector, "pool": nc.gpsimd, "sp": nc.sync}
        self.sem = {}
        self.cnt = {}
        self.waited = {k: {} for k in self.eng}
        self.nsem = 0
        self.dbufs = []
        for k in self.eng:
            self.sem[k] = es.enter_context(nc.semaphore("e_" + k))
            self.cnt[k] = 0
        self.n_ins = 0
        self.n_wait = 0

    def _deps(self, reads, writes, e=None):
        deps = {}
        own = self.sem.get(e)

        def add(t):
            if t is None:
                return
            k = id(t[0])
            if k not in deps or deps[k][1] < t[1]:
                deps[k] = t
        for b in reads:
            add(b.w)
            if b.excl:
                for t in b.rs.values():
                    if t[0] is not own:
                        add(t)
        for b in writes:
            add(b.w)
            for t in b.rs.values():
                add(t)
        return deps

    def _emit_waits(self, e, deps):
        own = self.sem.get(e)
        eo = self.eng[e]
        wd = self.waited[e]
        for k, (s, v) in deps.items():
            if s is own and e in ("pe", "sp"):
                continue
            if wd.get(k, 0) >= v:
                continue
            eo.wait_ge(s, v)
            wd[k] = v
            self.n_wait += 1

    def _commit(self, t, reads, writes):
        k = id(t[0])
        for b in reads:
            b.rs[k] = t
        for b in writes:
            b.w = t
            b.rs = {}

    def op(self, e, fn, reads=(), writes=()):
        self._emit_waits(e, self._deps(reads, writes, e))
        ins = fn()
        self.cnt[e] += 1
        ins.then_inc(self.sem[e], 1)
        self._commit((self.sem[e], self.cnt[e]), reads, writes)
        self.n_ins += 1
        return ins

    def dma(self, q, out, in_, reads=(), writes=(), **kw):
        self._emit_waits(q, self._deps(reads, writes, q))
        owner = writes[0] if len(writes) else reads[0]
        if owner.dsem is None:
            owner.dsem = self.es.enter_context(self.nc.semaphore("d_%d" % self.nsem))
            self.nsem += 1
            self.dbufs.append(owner)
        ins = self.eng[q].dma_start(out=out, in_=in_, **kw)
        owner.dcnt += 16
        ins.then_inc(owner.dsem, 16)
        self._commit((owner.dsem, owner.dcnt), reads, writes)
        self.n_ins += 1
        return ins

    def barrier(self):
        deps = {}
        for k in self.eng:
            if self.cnt[k]:
                deps[id(self.sem[k])] = (self.sem[k], self.cnt[k])
        for b in self.dbufs:
            deps[id(b.dsem)] = (b.dsem, b.dcnt)
        for e in self.eng:
            own = self.sem[e]
            eo = self.eng[e]
            wd = self.waited[e]
            for k, (s, v) in deps.items():
                if s is own or wd.get(k, 0) >= v:
                    continue
                eo.wait_ge(s, v)
                wd[k] = v

    def finish(self, e="pool"):
        deps = {}
        for b in self.dbufs:
            deps[id(b.dsem)] = (b.dsem, b.dcnt)
        for k in self.eng:
            if self.cnt[k]:
                deps[id(self.sem[k])] = (self.sem[k], self.cnt[k])
        self._emit_waits(e, deps)


class Prog:
    def __init__(self):
        self.nc = bass.Bass("TRN2", target_bir_lowering=False)
        self.es = ExitStack()
        self.T = Trk(self.nc, self.es)
        self.pbig = [self.es.enter_context(self.nc.psum_tensor("pbig%d" % i, [128, 1024], F32)) for i in range(4)]
        self.banks = [self.pbig[i // 2][:, (i % 2) * 512:(i % 2 + 1) * 512] for i in range(8)]
        self.bb = [Buf("bank%d" % i, excl=True) for i in range(8)]
        self.uid = 0
        nc, T = self.nc, self.T
        self.ident_d = self.inp("ident", [128, 128], F32)
        self.ident = self.sb(self.es, [128, 128], F32)
        self.b_ident = Buf("ident")
        T.dma("sp", self.ident[:], self.ident_d[:, :], writes=[self.b_ident])
        self.ones = self.sb(self.es, [128, 128], BF16)
        self.b_ones = Buf("ones")
        T.op("pool", lambda: nc.gpsimd.memset(self.ones[:], 1.0), writes=[self.b_ones])
        self.onesd = self.sb(self.es, [128, 128], F32)
        self.b_onesd = Buf("onesd")
        T.op("pool", lambda: nc.gpsimd.memset(self.onesd[:], 1.0 / 128.0), writes=[self.b_onesd])

    def inp(self, name, shape, dt):
        return self.nc.dram_tensor(name, list(shape), dt, kind="ExternalInput").ap()

    def outp(self, name, shape, dt):
        return self.nc.dram_tensor(name, list(shape), dt, kind="ExternalOutput").ap()

    def sb(self, es, shape, dt, name=None):
        self.uid += 1
        return es.enter_context(self.nc.sbuf_tensor("%s_%d" % (name or "t", self.uid), list(shape), dt))

    def close(self):
        self.T.finish("pool")
        self.es.close()
        return self.nc


def bcast_rows(ap_row, n):
    return ap_row.partition_broadcast(n)


def phase_mod(P, cc_d, wm_d, bm_d, out_d, ncols):
    nc, T = P.nc, P.T
    with ExitStack() as es:
        cc = P.sb(es, [128, 8, 2], F32)
        b_cc = Buf("cc")
        T.dma("sp", cc[:], cc_d[:, :, :], writes=[b_cc])
        ccs = P.sb(es, [128, 8, 2], F32)
        b_ccs = Buf("ccs")
        T.op("act", lambda: nc.scalar.activation(out=ccs[:], in_=cc[:], func=AF.Silu), reads=[b_cc], writes=[b_ccs])
        CW = 384
        stg = [P.sb(es, [128, 8, CW], F32) for _ in range(2)]
        b_stg = [Buf("stg%d" % i) for i in range(2)]
        bt = [P.sb(es, [2, CW], F32) for _ in range(2)]
        b_bt = [Buf("bt%d" % i) for i in range(2)]
        rs = [P.sb(es, [2, CW], F32) for _ in range(2)]
        b_rs = [Buf("rs%d" % i) for i in range(2)]
        it = 0
        for l in range(DEPTH):
            for c0 in range(0, ncols, CW):
                i = it % 2
                it += 1
                T.dma("sp", stg[i][:], wm_d[l, :, c0:c0 + CW].rearrange("(p k) n -> p k n", k=8), writes=[b_stg[i]])
                T.dma("sp", bt[i][:], bm_d[l, :, c0:c0 + CW], writes=[b_bt[i]])
                ps = P.banks[i]
                for k in range(8):
                    T.op("pe", lambda: nc.tensor.matmul(ps[0:2, 0:CW], lhsT=ccs[:, k, :], rhs=stg[i][:, k, :], start=(k == 0), stop=(k == 7)),
                         reads=[b_ccs, b_stg[i]], writes=[P.bb[i]])
                T.op("dve", lambda: nc.vector.tensor_tensor(out=rs[i][:], in0=ps[0:2, 0:CW], in1=bt[i][:], op=ALU.add),
                     reads=[P.bb[i], b_bt[i]], writes=[b_rs[i]])
                T.dma("pool", out_d[l, :, c0:c0 + CW], rs[i][:], reads=[b_rs[i]])
    T.barrier()


def load_weight_bf16(P, es, src, K, ncols, name):
    nc, T = P.nc, P.T
    dst = P.sb(es, [128, K, ncols], BF16, name)
    CW = 4096 // K
    stg = [P.sb(es, [128, K, CW], F32, name + "s") for _ in range(2)]
    b_stg = [Buf(name + "s%d" % i) for i in range(2)]
    bufs = {}
    it = 0
    for c0 in range(0, ncols, CW):
        cw = min(CW, ncols - c0)
        i = it % 2
        it += 1
        T.dma("sp", stg[i][:, :, 0:cw], src[:, c0:c0 + cw].rearrange("(k p) n -> p k n", p=128), writes=[b_stg[i]])
        b = Buf(name + "_c%d" % c0)
        T.op("act", lambda: nc.scalar.copy(out=dst[:, :, c0:c0 + cw], in_=stg[i][:, :, 0:cw]), reads=[b_stg[i]], writes=[b])
        for c in range(c0, c0 + cw, 128):
            bufs[c // 128] = b
    return dst, bufs


def rope_tok(P, src, dst, nh, hd, cos, sin, b_src, b_dst, b_tab, tmp, b_tmp):
    nc, T = P.nc, P.T
    q = hd // 4
    sv = src.rearrange("p (h a j i) -> p h a j i", h=nh, a=2, j=2, i=q)
    dv = dst.rearrange("p (h a j i) -> p h a j i", h=nh, a=2, j=2, i=q)
    x1 = sv[:, :, :, 0, :]
    x2 = sv[:, :, :, 1, :]
    cb = cos.rearrange("p (a i) -> p a i", a=2).unsqueeze(1).to_broadcast([128, nh, 2, q])
    sbc = sin.rearrange("p (a i) -> p a i", a=2).unsqueeze(1).to_broadcast([128, nh, 2, q])
    n = nh * 2 * q
    t = [tmp[j][:, 0:n].rearrange("p (h a i) -> p h a i", h=nh, a=2, i=q) for j in range(4)]
    T.op("dve", lambda: nc.vector.tensor_tensor(out=t[0], in0=x1, in1=cb, op=ALU.mult), reads=[b_src, b_tab], writes=[b_tmp[0]])
    T.op("dve", lambda: nc.vector.tensor_tensor(out=t[1], in0=x2, in1=sbc, op=ALU.mult), reads=[b_src, b_tab], writes=[b_tmp[1]])
    T.op("dve", lambda: nc.vector.tensor_tensor(out=t[2], in0=x1, in1=sbc, op=ALU.mult), reads=[b_src, b_tab], writes=[b_tmp[2]])
    T.op("dve", lambda: nc.vector.tensor_tensor(out=t[3], in0=x2, in1=cb, op=ALU.mult), reads=[b_src, b_tab], writes=[b_tmp[3]])
    T.op("pool", lambda: nc.gpsimd.tensor_tensor(out=dv[:, :, :, 0, :], in0=t[0], in1=t[1], op=ALU.subtract),
         reads=[b_tmp[0], b_tmp[1]], writes=[b_dst])
    T.op("pool", lambda: nc.gpsimd.tensor_tensor(out=dv[:, :, :, 1, :], in0=t[2], in1=t[3], op=ALU.add),
         reads=[b_tmp[2], b_tmp[3]], writes=[b_dst])


def transpose_mod(P, xt, b_xt, scT, shT, b_mod, hT, b_hT, tb, hTf=None, b_hTf=None):
    nc, T = P.nc, P.T
    for k in range(8):
        bk = tb[k // 4]
        T.op("pe", lambda: nc.tensor.transpose(P.banks[bk][:, (k % 4) * 128:(k % 4 + 1) * 128], xt[:, k * 128:(k + 1) * 128], P.ident[:]),
             reads=[b_xt, P.b_ident], writes=[P.bb[bk]])
    for k in range(8):
        bk = tb[k // 4]
        src = P.banks[bk][:, (k % 4) * 128:(k % 4 + 1) * 128]
        if hTf is None:
            T.op("act", lambda: nc.scalar.activation(out=hT[:, k, :], in_=src, func=AF.Identity, bias=shT[:, k:k + 1], scale=scT[:, k:k + 1]),
                 reads=[P.bb[bk], b_mod], writes=[b_hT])
        else:
            T.op("act", lambda: nc.scalar.activation(out=hTf[:, k, :], in_=src, func=AF.Identity, bias=shT[:, k:k + 1], scale=scT[:, k:k + 1]),
                 reads=[P.bb[bk], b_mod], writes=[b_hTf])
    if hTf is not None:
        T.op("dve", lambda: nc.vector.tensor_copy(out=hT, in_=hTf[:]), reads=[b_hTf], writes=[b_hT])


def load_modT(P, es, m_d, name):
    nc, T = P.nc, P.T
    m = P.sb(es, [128, 2, 2, 8], F32, name)
    b = Buf(name)
    T.dma("sp", m[:], m_d[:, :, :, :], writes=[b])
    T.op("dve", lambda: nc.vector.tensor_scalar(out=m[:, :, 0, :], in0=m[:, :, 0, :], scalar1=1.0, scalar2=None, op0=ALU.add),
         reads=[b], writes=[b])
    return m, b


def phase_A(P, even, x_d, x_bufs, w_d, mA_d, cos_d, sin_d, qng_d, qkv_d):
    nc, T = P.nc, P.T
    ncols = E_COLS if even else O_COLS
    hd = 64 if even else 128
    hq = hd // 2
    with ExitStack() as es:
        W, wb = load_weight_bf16(P, es, w_d, 8, ncols, "win")
        m, b_m = load_modT(P, es, mA_d, "mA")
        xt = [P.sb(es, [128, D], F32, "xt") for _ in range(2)]
        b_xt = [Buf("xt%d" % i) for i in range(2)]
        hT = [P.sb(es, [128, 8, 128], BF16, "hT") for _ in range(2)]
        b_hT = [Buf("hT%d" % i) for i in range(2)]
        ot = [P.sb(es, [128, ncols], BF16, "ot") for _ in range(2)]
        b_ot = [Buf("ot%d" % i) for i in range(2)]
        cs = [P.sb(es, [128, 2, hq], F32, "cs") for _ in range(2)]
        b_cs = [Buf("cs%d" % i) for i in range(2)]
        tmp = [P.sb(es, [128, 512], F32, "rt") for _ in range(4)]
        b_tmp = [Buf("rt%d" % i) for i in range(4)]
        if not even:
            gq = P.sb(es, [128, 2, 128], F32, "gq")
            b_gq = Buf("gq")
            T.dma("sp", gq[:, 0, :], qng_d[0:1, :].partition_broadcast(128), writes=[b_gq])
            T.dma("sp", gq[:, 1, :], qng_d[1:2, :].partition_broadcast(128), writes=[b_gq])
            epsq = P.sb(es, [128, 1], F32, "epsq")
            b_epsq = Buf("epsq")
            T.op("pool", lambda: nc.gpsimd.memset(epsq[:], QK_EPS), writes=[b_epsq])
            sq = P.sb(es, [128, 512], F32, "sq")
            b_sq = Buf("sq")
            ssq = P.sb(es, [128, 4], F32, "ssq")
            b_ssq = Buf("ssq")
            xn = [P.sb(es, [128, 512], F32, "xn") for _ in range(2)]
            b_xn = [Buf("xn%d" % i) for i in range(2)]
        nbk = (ncols + 511) // 512
        for t in range(NT):
            i = t % 2
            who = 1 if t == NT - 1 else 0
            rd = [x_bufs[t]] if x_bufs is not None else []
            T.dma("sp", xt[i][:], x_d[t * 128:(t + 1) * 128, :], reads=rd, writes=[b_xt[i]])
            T.dma("sp", cs[i][:, 0, :], cos_d[t * 128:(t + 1) * 128, :], writes=[b_cs[i]])
            T.dma("sp", cs[i][:, 1, :], sin_d[t * 128:(t + 1) * 128, :], writes=[b_cs[i]])
            transpose_mod(P, xt[i], b_xt[i], m[:, who, 0, :], m[:, who, 1, :], b_m, hT[i], b_hT[i], (6, 7))
            for c in range(nbk):
                cw = min(512, ncols - c * 512)
                for k in range(8):
                    T.op("pe", lambda: nc.tensor.matmul(P.banks[c][:, 0:cw], lhsT=hT[i][:, k, :], rhs=W[:, k, c * 512:c * 512 + cw],
                                                        start=(k == 0), stop=(k == 7)),
                         reads=[b_hT[i], wb[c * 4]] + ([wb[c * 4 + 3]] if cw == 512 else []), writes=[P.bb[c]])
            o = ot[i]
            cosv, sinv = cs[i][:, 0, :], cs[i][:, 1, :]
            if even:
                def rp(bank, c0, c1):
                    rope_tok(P, P.banks[bank][:, c0 - bank * 512:c1 - bank * 512], o[:, c0:c1], (c1 - c0) // 64, 64, cosv, sinv,
                             P.bb[bank], b_ot[i], b_cs[i], tmp, b_tmp)
                rp(0, 0, 512)
                rp(1, 512, 640)
                rp(1, 768, 1024)
                rp(2, 1024, 1536)
                rp(3, 1536, 1792)
                T.op("act", lambda: nc.scalar.copy(out=o[:, 640:768], in_=P.banks[1][:, 128:256]), reads=[P.bb[1]], writes=[b_ot[i]])
                T.op("act", lambda: nc.scalar.copy(out=o[:, 1792:2048], in_=P.banks[3][:, 256:512]), reads=[P.bb[3]], writes=[b_ot[i]])
                T.op("act", lambda: nc.scalar.copy(out=o[:, 2048:2304], in_=P.banks[4][:, 0:256]), reads=[P.bb[4]], writes=[b_ot[i]])
            else:
                for bank, nh, gi in ((0, 4, 0), (1, 4, 0), (2, 2, 1)):
                    n = nh * 128
                    src = P.banks[bank][:, 0:n]
                    T.op("act", lambda: nc.scalar.activation(out=sq[:, 0:n], in_=src, func=AF.Square), reads=[P.bb[bank]], writes=[b_sq])
                    T.op("dve", lambda: nc.vector.tensor_reduce(out=ssq[:, 0:nh], in_=sq[:, 0:n].rearrange("p (h d) -> p h d", h=nh),
                                                                axis=AX.X, op=ALU.add), reads=[b_sq], writes=[b_ssq])
                    T.op("act", lambda: nc.scalar.activation(out=ssq[:, 0:nh], in_=ssq[:, 0:nh], func=AF.Sqrt, bias=epsq[:, 0:1], scale=1.0 / 128.0),
                         reads=[b_ssq, b_epsq], writes=[b_ssq])
                    T.op("dve", lambda: nc.vector.reciprocal(out=ssq[:, 0:nh], in_=ssq[:, 0:nh]), reads=[b_ssq], writes=[b_ssq])
                    j = bank % 2
                    T.op("dve", lambda: nc.vector.tensor_tensor(out=xn[j][:, 0:n].rearrange("p (h d) -> p h d", h=nh),
                                                                in0=src.rearrange("p (h d) -> p h d", h=nh),
                                                                in1=ssq[:, 0:nh].unsqueeze(2).to_broadcast([128, nh, 128]), op=ALU.mult),
                         reads=[P.bb[bank], b_ssq], writes=[b_xn[j]])
                    T.op("pool", lambda: nc.gpsimd.tensor_tensor(out=xn[j][:, 0:n].rearrange("p (h d) -> p h d", h=nh),
                                                                 in0=xn[j][:, 0:n].rearrange("p (h d) -> p h d", h=nh),
                                                                 in1=gq[:, gi, :].unsqueeze(1).to_broadcast([128, nh, 128]), op=ALU.mult),
                         reads=[b_xn[j], b_gq], writes=[b_xn[j]])
                    rope_tok(P, xn[j][:, 0:n], o[:, bank * 512:bank * 512 + n], nh, 128, cosv, sinv, b_xn[j], b_ot[i], b_cs[i], tmp, b_tmp)
                T.op("act", lambda: nc.scalar.copy(out=o[:, 1280:1536], in_=P.banks[2][:, 256:512]), reads=[P.bb[2]], writes=[b_ot[i]])
            T.dma("pool", qkv_d[t * 128:(t + 1) * 128, :], o[:], reads=[b_ot[i]])
    T.barrier()


class SweepCtx:
    def __init__(self, P, es, npt=5):
        self.P = P
        self.pT = [P.sb(es, [128, 2, 512], BF16, "pT") for _ in range(npt)]
        self.b_pT = [Buf("pT%d" % i) for i in range(npt)]
        self.n = 0
        self.osel = 0
        self.acc = [[P.sb(es, [128, 512], F32, "sacc") for _ in range(2)] for _ in range(2)]
        self.b_acc = [[Buf("sacc%d%d" % (a, b)) for b in range(2)] for a in range(2)]
        self.onesf = P.sb(es, [128, 128], F32, "onesf")
        self.b_onesf = Buf("onesf")
        P.T.op("pool", lambda: P.nc.gpsimd.memset(self.onesf[:], 1.0), writes=[self.b_onesf])


def sweep(P, S, N, blocks, qT, b_q, kT_of, v_of, b_kv, M, scale):
    nc, T = P.nc, P.T
    SK = 2
    par = S.osel
    ob, sb_ = 6, 7
    S.osel ^= 1
    nb = len(blocks)
    assert nb % 2 == 0
    npair = nb // 2
    idx = []
    started = [False, False]
    engs = ("dve", "pool")
    eobj = (nc.vector, nc.gpsimd)
    for u in range(npair + SK):
        if u < npair:
            n = S.n
            S.n += 1
            idx.append(n)
            sp = n % 3
            p = n % len(S.pT)
            for b in range(2):
                bk = 2 * sp + b
                T.op("pe", lambda: nc.tensor.matmul(P.banks[bk][:, 0:N], lhsT=kT_of(blocks[2 * u + b]), rhs=qT, start=True, stop=True),
                     reads=[b_kv, b_q], writes=[P.bb[bk]])
            src = P.pbig[sp][:, :].rearrange("p (b n) -> p b n", b=2)[:, :, 0:N]
            T.op("act", lambda: nc.scalar.activation(out=S.pT[p][:, :, 0:N], in_=src, func=AF.Exp, scale=scale),
                 reads=[P.bb[2 * sp], P.bb[2 * sp + 1]], writes=[S.b_pT[p]])
        if u >= SK:
            p = idx[u - SK] % len(S.pT)
            for b in range(2):
                j = 2 * (u - SK) + b
                pt = S.pT[p][:, b, 0:N]
                T.op("pe", lambda: nc.tensor.matmul(P.banks[ob][0:M, 0:N], lhsT=v_of(blocks[j]), rhs=pt, start=(j == 0), stop=(j == nb - 1)),
                     reads=[b_kv, S.b_pT[p]], writes=[P.bb[ob]])
                g = 1 if (j % 3 == 2) else 0
                a = S.acc[par][g]
                ba = S.b_acc[par][g]
                if not started[g]:
                    started[g] = True
                    T.op(engs[g], lambda: eobj[g].tensor_copy(out=a[:, 0:N], in_=pt), reads=[S.b_pT[p]], writes=[ba])
                else:
                    T.op(engs[g], lambda: eobj[g].tensor_tensor(out=a[:, 0:N], in0=a[:, 0:N], in1=pt, op=ALU.add),
                         reads=[S.b_pT[p], ba], writes=[ba])
    used = [g for g in range(2) if started[g]]
    for gi, g in enumerate(used):
        T.op("pe", lambda: nc.tensor.matmul(P.banks[sb_][:, 0:N], lhsT=S.onesf[:], rhs=S.acc[par][g][:, 0:N], start=(gi == 0), stop=(gi == len(used) - 1)),
             reads=[S.b_onesf, S.b_acc[par][g]], writes=[P.bb[sb_]])
    return ob, sb_


ALL_BLOCKS = list(range(NKB))
CTX_BLOCKS = [0, 1]


def phase_B_odd(P, OT, b_OT, QT_d, KT_d, V_d):
    nc, T = P.nc, P.T
    scale = 128.0 ** -0.5
    with ExitStack() as es:
        S = SweepCtx(P, es)
        kt = P.sb(es, [128, NKB * 128], BF16, "kt")
        vt = P.sb(es, [128, NKB, 128], BF16, "vt")
        b_kv = Buf("kv")
        qt = [P.sb(es, [128, TOK], BF16, "qt") for _ in range(2)]
        b_qt = [Buf("qt%d" % i) for i in range(2)]
        rec = [P.sb(es, [128, 512], F32, "rec") for _ in range(2)]
        b_rec = [Buf("rec%d" % i) for i in range(2)]
        fi = 0
        for kvh in range(2):
            T.dma("sp", kt[:], KT_d[kvh, :, :], writes=[b_kv])
            T.dma("sp", vt[:], V_d[kvh, :, :, :], writes=[b_kv])
            for g in range(4):
                h = kvh * 4 + g
                qi = h % 2
                T.dma("sp", qt[qi][:], QT_d[h, :, :], writes=[b_qt[qi]])
                for (c0, N) in CHUNKS:
                    blocks = ALL_BLOCKS if N == 512 else CTX_BLOCKS
                    ksl = slice(64, 128) if "k64" in os.environ.get("K_DBG", "") else slice(0, 128)
                    ob, sbk = sweep(P, S, N, blocks, qt[qi][ksl, c0:c0 + N], b_qt[qi],
                                    lambda b: kt[ksl, b * 128:(b + 1) * 128], lambda b: vt[:, b, :], b_kv, 128, scale)
                    r = fi % 2
                    fi += 1
                    T.op("dve", lambda: nc.vector.reciprocal(out=rec[r][:, 0:N], in_=P.banks[sbk][:, 0:N]), reads=[P.bb[sbk]], writes=[b_rec[r]])
                    T.op("dve", lambda: nc.vector.tensor_tensor(out=OT[:, h, c0:c0 + N], in0=P.banks[ob][:, 0:N], in1=rec[r][:, 0:N], op=ALU.mult),
                         reads=[P.bb[ob], b_rec[r]], writes=[b_OT])
    T.barrier()


def phase_B_even(P, OT, b_OT, QTa_d, KTa_d, Va_d, mask_d, sinkp_d, QTb_d, KTb_d, Vb_d, lamv_d, lami_d, subg_d):
    nc, T = P.nc, P.T
    sc = 64.0 ** -0.5
    with ExitStack() as es:
        if "nomA" in os.environ.get("K_DBG", ""):
            es.close()
            return phase_B_even_mixB(P, OT, b_OT, QTb_d, KTb_d, Vb_d, lamv_d, lami_d, subg_d)
        qta = P.sb(es, [128, 4, TOK], BF16, "qta")
        b_qta = Buf("qta")
        T.dma("sp", qta[:], QTa_d[:, :, :], writes=[b_qta])
        ktaw = P.sb(es, [128, 20 * 128], BF16, "ktaw")
        b_kta = Buf("ktaw")
        T.dma("sp", ktaw[:], KTa_d[:, :], writes=[b_kta])
        vaw = P.sb(es, [128, 4, 20, 128], BF16, "vaw")
        b_vaw = Buf("vaw")
        T.dma("sp", vaw[:], Va_d[:, :, :, :], writes=[b_vaw])
        msk = P.sb(es, [128, NT, 384], BF16, "msk")
        b_msk = Buf("msk")
        T.dma("sp", msk[:], mask_d[:, :, :], writes=[b_msk])
        esink = P.sb(es, [128, 4], F32, "esink")
        b_es = Buf("esink")
        T.dma("sp", esink[:], sinkp_d[:, :], writes=[b_es])
        T.op("act", lambda: nc.scalar.activation(out=esink[:], in_=esink[:], func=AF.Exp), reads=[b_es], writes=[b_es])
        oneslh = P.sb(es, [128, 2, 128], BF16, "oneslh")
        b_olh = Buf("oneslh")
        T.op("pool", lambda: nc.gpsimd.memset(oneslh[:], 0.0), writes=[b_olh])
        T.op("pool", lambda: nc.gpsimd.memset(oneslh[:, 0, 0:64], 1.0), reads=[b_olh], writes=[b_olh])
        T.op("pool", lambda: nc.gpsimd.memset(oneslh[:, 1, 64:128], 1.0), reads=[b_olh], writes=[b_olh])
        pTa = [P.sb(es, [128, 640], BF16, "pTa") for _ in range(4)]
        b_pTa = [Buf("pTa%d" % i) for i in range(4)]
        den = [P.sb(es, [128, 128], F32, "den") for _ in range(2)]
        b_den = [Buf("den%d" % i) for i in range(2)]
        n = 0
        fi = 0
        for c in range(4):
            kvh = c // 2
            ksl = slice(kvh * 64, kvh * 64 + 64)
            for t in range(NT):
                wl = (t + 2) if t < NT - 1 else 2
                blocks = [0, 1, wl, wl + 1, wl + 2]
                ob, sbk = (4, 5) if fi % 2 == 0 else (6, 7)
                for hh in range(2):
                    j = (2 * c + hh) % 4
                    p = n % 4
                    s0, s1 = (0, 1) if n % 2 == 0 else (2, 3)
                    n += 1
                    q_ap = qta[ksl, j, t * 128:(t + 1) * 128]
                    for bi, w in enumerate(blocks):
                        bk, col = (s0, bi * 128) if bi < 2 else (s1, (bi - 2) * 128)
                        T.op("pe", lambda: nc.tensor.matmul(P.banks[bk][:, col:col + 128], lhsT=ktaw[ksl, w * 128:(w + 1) * 128], rhs=q_ap,
                                                            start=True, stop=True), reads=[b_kta, b_qta], writes=[P.bb[bk]])
                    T.op("act", lambda: nc.scalar.activation(out=pTa[p][:, 0:256], in_=P.banks[s0][:, 0:256], func=AF.Exp, scale=sc),
                         reads=[P.bb[s0]], writes=[b_pTa[p]])
                    T.op("act", lambda: nc.scalar.activation(out=pTa[p][:, 256:640], in_=P.banks[s1][:, 0:384], func=AF.Exp, scale=sc),
                         reads=[P.bb[s1]], writes=[b_pTa[p]])
                    T.op("pool", lambda: nc.gpsimd.tensor_tensor(out=pTa[p][:, 256:640], in0=pTa[p][:, 256:640], in1=msk[:, t, :], op=ALU.mult),
                         reads=[b_pTa[p], b_msk], writes=[b_pTa[p]])
                    for bi, w in enumerate(blocks):
                        first = (hh == 0 and bi == 0)
                        last = (hh == 1 and bi == 4)
                        T.op("pe", lambda: nc.tensor.matmul(P.banks[ob][:, 0:128], lhsT=vaw[:, kvh * 2 + hh, w, :], rhs=pTa[p][:, bi * 128:(bi + 1) * 128],
                                                            start=first, stop=last), reads=[b_vaw, b_pTa[p]], writes=[P.bb[ob]])
                        T.op("pe", lambda: nc.tensor.matmul(P.banks[sbk][:, 0:128], lhsT=oneslh[:, hh, :], rhs=pTa[p][:, bi * 128:(bi + 1) * 128],
                                                            start=first, stop=last), reads=[b_olh, b_pTa[p]], writes=[P.bb[sbk]])
                r = fi % 2
                fi += 1
                T.op("dve", lambda: nc.vector.tensor_scalar(out=den[r][:], in0=P.banks[sbk][:, 0:128], scalar1=esink[:, c:c + 1], scalar2=None, op0=ALU.add),
                     reads=[P.bb[sbk], b_es], writes=[b_den[r]])
                T.op("dve", lambda: nc.vector.reciprocal(out=den[r][:], in_=den[r][:]), reads=[b_den[r]], writes=[b_den[r]])
                T.op("dve", lambda: nc.vector.tensor_tensor(out=OT[:, c, t * 128:(t + 1) * 128], in0=P.banks[ob][:, 0:128], in1=den[r][:], op=ALU.mult),
                     reads=[P.bb[ob], b_den[r]], writes=[b_OT])
    T.barrier()
    if "nomB" in os.environ.get("K_DBG", ""):
        return
    phase_B_even_mixB(P, OT, b_OT, QTb_d, KTb_d, Vb_d, lamv_d, lami_d, subg_d)


def phase_B_even_mixB(P, OT, b_OT, QTb_d, KTb_d, Vb_d, lamv_d, lami_d, subg_d):
    nc, T = P.nc, P.T
    sc = 64.0 ** -0.5
    with ExitStack() as es:
        S = SweepCtx(P, es)
        ktb = P.sb(es, [128, NKB * 128], BF16, "ktb")
        vtb = P.sb(es, [128, NKB, 128], BF16, "vtb")
        b_kv = Buf("kvb")
        qtb = [P.sb(es, [128, 2, TOK], BF16, "qtb") for _ in range(2)]
        b_qtb = [Buf("qtb%d" % i) for i in range(2)]
        lamv = P.sb(es, [128, 4, 64], F32, "lamv")
        b_lamv = Buf("lamv")
        for i in range(4):
            T.dma("sp", lamv[:, i, :], lamv_d[i:i + 1, :].partition_broadcast(128), writes=[b_lamv])
        lami = P.sb(es, [128, 2], F32, "lami")
        b_lami = Buf("lami")
        T.dma("sp", lami[:], lami_d[:, :], writes=[b_lami])
        lp = P.sb(es, [128, 2, 64], F32, "lp")
        b_lp = Buf("lp")
        ls = P.sb(es, [128, 4], F32, "ls")
        b_ls = Buf("ls")
        T.op("dve", lambda: nc.vector.tensor_tensor(out=lp[:, 0, :], in0=lamv[:, 0, :], in1=lamv[:, 1, :], op=ALU.mult), reads=[b_lamv], writes=[b_lp])
        T.op("dve", lambda: nc.vector.tensor_tensor(out=lp[:, 1, :], in0=lamv[:, 2, :], in1=lamv[:, 3, :], op=ALU.mult), reads=[b_lamv, b_lp], writes=[b_lp])
        T.op("dve", lambda: nc.vector.tensor_reduce(out=ls[:, 0:2], in_=lp[:], axis=AX.X, op=ALU.add), reads=[b_lp], writes=[b_ls])
        T.op("act", lambda: nc.scalar.activation(out=ls[:, 0:2], in_=ls[:, 0:2], func=AF.Exp), reads=[b_ls], writes=[b_ls])
        T.op("dve", lambda: nc.vector.tensor_tensor(out=ls[:, 2:3], in0=ls[:, 1:2], in1=ls[:, 0:1], op=ALU.subtract), reads=[b_ls], writes=[b_ls])
        T.op("dve", lambda: nc.vector.tensor_tensor(out=ls[:, 2:3], in0=ls[:, 2:3], in1=lami[:, 0:1], op=ALU.subtract), reads=[b_ls, b_lami], writes=[b_ls])
        gsc = P.sb(es, [128, 1], F32, "gsc")
        b_gsc = Buf("gsc")
        T.dma("sp", gsc[:], subg_d[:, :], writes=[b_gsc])
        T.op("dve", lambda: nc.vector.tensor_tensor(out=gsc[:], in0=gsc[:], in1=lami[:, 1:2], op=ALU.mult), reads=[b_gsc, b_lami], writes=[b_gsc])
        epss = P.sb(es, [128, 1], F32, "epss")
        b_epss = Buf("epss")
        T.op("pool", lambda: nc.gpsimd.memset(epss[:], SUBLN_EPS), writes=[b_epss])
        rec = P.sb(es, [128, 512], F32, "recb")
        b_rec = Buf("recb")
        am = [P.sb(es, [128, 512], F32, "am") for _ in range(2)]
        b_am = [Buf("am%d" % i) for i in range(2)]
        dm = P.sb(es, [128, 512], F32, "dm")
        b_dm = Buf("dm")
        sq = P.sb(es, [128, 512], F32, "sqb")
        b_sq = Buf("sqb")
        rstd = P.sb(es, [128, 512], F32, "rstd")
        b_rstd = Buf("rstd")
        for h in range(4):
            T.dma("sp", ktb[:], KTb_d[h, :, :], writes=[b_kv])
            T.dma("sp", vtb[:], Vb_d[h, :, :, :], writes=[b_kv])
            qi = h % 2
            T.dma("sp", qtb[qi][:], QTb_d[h, :, :, :], writes=[b_qtb[qi]])
            for (c0, N) in CHUNKS:
                blocks = ALL_BLOCKS if N == 512 else CTX_BLOCKS
                for mm in range(2):
                    ob, sbk = sweep(P, S, N, blocks, qtb[qi][:, mm, c0:c0 + N], b_qtb[qi],
                                    lambda b: ktb[:, b * 128:(b + 1) * 128], lambda b: vtb[:, b, :], b_kv, 128, sc)
                    T.op("dve", lambda: nc.vector.reciprocal(out=rec[:, 0:N], in_=P.banks[sbk][:, 0:N]), reads=[P.bb[sbk]], writes=[b_rec])
                    T.op("dve", lambda: nc.vector.tensor_tensor(out=am[mm][:, 0:N], in0=P.banks[ob][:, 0:N], in1=rec[:, 0:N], op=ALU.mult),
                         reads=[P.bb[ob], b_rec], writes=[b_am[mm]])
                T.op("dve", lambda: nc.vector.scalar_tensor_tensor(out=dm[:, 0:N], in0=am[1][:, 0:N], scalar=ls[:, 2:3], in1=am[0][:, 0:N],
                                                                   op0=ALU.mult, op1=ALU.add), reads=[b_am[0], b_am[1], b_ls], writes=[b_dm])
                T.op("act", lambda: nc.scalar.activation(out=sq[:, 0:N], in_=dm[:, 0:N], func=AF.Square), reads=[b_dm], writes=[b_sq])
                T.op("pe", lambda: nc.tensor.matmul(P.banks[sbk][:, 0:N], lhsT=P.onesd[:], rhs=sq[:, 0:N], start=True, stop=True),
                     reads=[P.b_onesd, b_sq], writes=[P.bb[sbk]])
                T.op("act", lambda: nc.scalar.activation(out=rstd[:, 0:N], in_=P.banks[sbk][:, 0:N], func=AF.Sqrt, bias=epss[:, 0:1], scale=1.0),
                     reads=[P.bb[sbk], b_epss], writes=[b_rstd])
                T.op("dve", lambda: nc.vector.reciprocal(out=rstd[:, 0:N], in_=rstd[:, 0:N]), reads=[b_rstd], writes=[b_rstd])
                T.op("dve", lambda: nc.vector.scalar_tensor_tensor(out=OT[:, 4 + h, c0:c0 + N], in0=dm[:, 0:N], scalar=gsc[:, 0:1], in1=rstd[:, 0:N],
                                                                   op0=ALU.mult, op1=ALU.mult), reads=[b_dm, b_gsc, b_rstd], writes=[b_OT])
    T.barrier()


def layer_norm_tile(P, L, src, b_src, dst, b_dst, gam, bet, b_gb):
    nc, T = P.nc, P.T
    st, b_st, mv, b_mv, xn, b_xn = L
    T.op("dve", lambda: nc.vector.bn_stats(out=st[:, 0, :], in_=src[:, 0:512]), reads=[b_src], writes=[b_st])
    T.op("dve", lambda: nc.vector.bn_stats(out=st[:, 1, :], in_=src[:, 512:1024]), reads=[b_src, b_st], writes=[b_st])
    T.op("dve", lambda: nc.vector.bn_aggr(out=mv[:, 0:2], in_=st[:].rearrange("p a s -> p (a s)")), reads=[b_st], writes=[b_mv])
    T.op("act", lambda: nc.scalar.activation(out=mv[:, 2:3], in_=mv[:, 1:2], func=AF.Sqrt, bias=mv[:, 3:4], scale=1.0), reads=[b_mv], writes=[b_mv])
    T.op("dve", lambda: nc.vector.reciprocal(out=mv[:, 2:3], in_=mv[:, 2:3]), reads=[b_mv], writes=[b_mv])
    T.op("dve", lambda: nc.vector.tensor_scalar(out=xn[:], in0=src, scalar1=mv[:, 0:1], scalar2=mv[:, 2:3], op0=ALU.subtract, op1=ALU.mult),
         reads=[b_src, b_mv], writes=[b_xn])
    T.op("pool", lambda: nc.gpsimd.tensor_tensor(out=xn[:], in0=xn[:], in1=gam, op=ALU.mult), reads=[b_xn, b_gb], writes=[b_xn])
    T.op("pool", lambda: nc.gpsimd.tensor_tensor(out=dst, in0=xn[:], in1=bet, op=ALU.add), reads=[b_xn, b_gb], writes=[b_dst])


def ln_scratch(P, es, tag):
    nc, T = P.nc, P.T
    st = P.sb(es, [128, 2, 6], F32, "lnst")
    mv = P.sb(es, [128, 4], F32, "lnmv")
    xn = P.sb(es, [128, D], F32, "lnxn")
    b_mv = Buf("lnmv" + tag)
    T.op("pool", lambda: nc.gpsimd.memset(mv[:, 3:4], LN_EPS), writes=[b_mv])
    return (st, Buf("lnst" + tag), mv, b_mv, xn, Buf("lnxn" + tag))


def phase_C1(P, OT, b_OT, x_d, wo_d, gbc_d, lng_d, lnb_d, x1_d, x1b):
    nc, T = P.nc, P.T
    with ExitStack() as es:
        wo, wob = load_weight_bf16(P, es, wo_d, 8, D, "wo")
        gb = P.sb(es, [128, 2, D], F32, "g1bc")
        b_gb = Buf("g1bc")
        for who in range(2):
            T.dma("sp", gb[:, who, :], gbc_d[who, 0:1, :].partition_broadcast(128), writes=[b_gb])
        ln = P.sb(es, [128, 2, D], F32, "ln1")
        b_ln = Buf("ln1")
        T.dma("sp", ln[:, 0, :], lng_d[0:1, :].partition_broadcast(128), writes=[b_ln])
        T.dma("sp", ln[:, 1, :], lnb_d[0:1, :].partition_broadcast(128), writes=[b_ln])
        L = ln_scratch(P, es, "1")
        xt = [P.sb(es, [128, D], F32, "cxt") for _ in range(2)]
        b_xt = [Buf("cxt%d" % i) for i in range(2)]
        rr = [P.sb(es, [128, D], F32, "crr") for _ in range(2)]
        b_rr = [Buf("crr%d" % i) for i in range(2)]
        x1t = [P.sb(es, [128, D], F32, "x1t") for _ in range(2)]
        b_x1t = [Buf("x1t%d" % i) for i in range(2)]
        for t in range(NT):
            i = t % 2
            who = 1 if t == NT - 1 else 0
            yb = (0, 1) if i == 0 else (2, 3)
            T.dma("sp", xt[i][:], x_d[t * 128:(t + 1) * 128, :], writes=[b_xt[i]])
            for half in range(2):
                for k in range(8):
                    T.op("pe", lambda: nc.tensor.matmul(P.banks[yb[half]][:, :], lhsT=OT[:, k, t * 128:(t + 1) * 128],
                                                        rhs=wo[:, k, half * 512:(half + 1) * 512], start=(k == 0), stop=(k == 7)),
                         reads=[b_OT, wob[half * 4]], writes=[P.bb[yb[half]]])
                T.op("dve", lambda: nc.vector.tensor_tensor(out=rr[i][:, half * 512:(half + 1) * 512], in0=P.banks[yb[half]][:, :],
                                                            in1=gb[:, who, half * 512:(half + 1) * 512], op=ALU.mult),
                     reads=[P.bb[yb[half]], b_gb], writes=[b_rr[i]])
            T.op("dve", lambda: nc.vector.scalar_tensor_tensor(out=rr[i][:], in0=xt[i][:], scalar=ALPHA, in1=rr[i][:], op0=ALU.mult, op1=ALU.add),
                 reads=[b_xt[i], b_rr[i]], writes=[b_rr[i]])
            layer_norm_tile(P, L, rr[i][:], b_rr[i], x1t[i][:], b_x1t[i], ln[:, 0, :], ln[:, 1, :], b_ln)
            T.dma("pool", x1_d[t * 128:(t + 1) * 128, :], x1t[i][:], reads=[b_x1t[i]], writes=[x1b[t]])
    T.barrier()


def phase_C2(P, x1_d, x1b, mC_d, gbc_d, lng_d, lnb_d, wr_d, br_d, wg_d, wu_d, wd_d, xo_d, xob):
    nc, T = P.nc, P.T
    with ExitStack() as es:
        acc = P.sb(es, [128, NT, D], F32, "acc")
        accb = [Buf("acc%d" % t) for t in range(NT)]
        h2T = P.sb(es, [128, 8, TOK], BF16, "h2T")
        h2b = [Buf("h2T%d" % c) for c in range(len(CHUNKS))]
        m, b_m = load_modT(P, es, mC_d, "mC")
        gb = P.sb(es, [128, 2, D], F32, "g2bc")
        b_gb = Buf("g2bc")
        for who in range(2):
            T.dma("sp", gb[:, who, :], gbc_d[who, 1:2, :].partition_broadcast(128), writes=[b_gb])
        wr = P.sb(es, [128, 8, NEXP], F32, "wr")
        b_wr = Buf("wr")
        T.dma("sp", wr[:], wr_d.rearrange("(k p) e -> p k e", p=128), writes=[b_wr])
        brt = P.sb(es, [128, NEXP], F32, "brt")
        b_brt = Buf("brt")
        T.dma("sp", brt[:], br_d[0:1, :].partition_broadcast(128), writes=[b_brt])
        scs = P.sb(es, [128, NT, NEXP], F32, "scs")
        b_scs = Buf("scs")
        gates = P.sb(es, [128, NT, NEXP], F32, "gates")
        b_gates = Buf("gates")
        with ExitStack() as es2:
            hTf = [P.sb(es2, [128, 8, 128], F32, "hTf") for _ in range(2)]
            b_hTf = [Buf("hTf%d" % i) for i in range(2)]
            for t in range(int(os.environ.get("K_NT", NT))):
                i = t % 2
                who = 1 if t == NT - 1 else 0
                ch = min(t // 4, 4)
                T.dma("sp", acc[:, t, :], x1_d[t * 128:(t + 1) * 128, :], reads=[x1b[t]], writes=[accb[t]])
                transpose_mod(P, acc[:, t, :], accb[t], m[:, who, 0, :], m[:, who, 1, :], b_m, h2T[:, :, t * 128:(t + 1) * 128], h2b[ch],
                              (0, 1) if i == 0 else (2, 3), None if "nohtf" in os.environ.get("K_DBG", "") else hTf[i], b_hTf[i])
                lb = 4 + i
                DBG = os.environ.get("K_DBG", "")
                if "nologit" not in DBG:
                    for k in range(8):
                        T.op("pe", lambda: nc.tensor.matmul(P.banks[lb][:, 0:NEXP], lhsT=hTf[i][:, k, :], rhs=wr[:, k, :], start=(k == 0), stop=(k == 7)),
                             reads=[b_hTf[i], b_wr], writes=[P.bb[lb]])
                    if "nosig" not in DBG:
                        T.op("act", lambda: nc.scalar.activation(out=scs[:, t, :], in_=P.banks[lb][:, 0:NEXP], func=AF.Sigmoid), reads=[P.bb[lb]], writes=[b_scs])
                if "nopool" not in DBG:
                    T.op("pool", lambda: nc.gpsimd.tensor_scalar(out=acc[:, t, :], in0=acc[:, t, :], scalar1=ALPHA, scalar2=None, op0=ALU.mult),
                         reads=[accb[t]], writes=[accb[t]])
            if "noroute" in os.environ.get("K_DBG", ""):
                return
            G = NT * 4
            sel = P.sb(es2, [128, NT, NEXP], F32, "sel")
            sel2 = P.sb(es2, [128, NT, NEXP], F32, "sel2")
            eq = P.sb(es2, [128, NT, NEXP], F32, "eq")
            m1 = P.sb(es2, [128, G], F32, "m1")
            m2 = P.sb(es2, [128, G], F32, "m2")
            gs = P.sb(es2, [128, G], F32, "gs")
            gmx = P.sb(es2, [128, NT], F32, "gmx")
            b_r = Buf("route")
            g4 = lambda a: a[:].rearrange("p t (g j) -> p (t g) j", j=4)
            bc4 = lambda a: a[:].unsqueeze(2).to_broadcast([128, G, 4])
            R = dict(reads=[b_r, b_scs, b_brt], writes=[b_r])
            T.op("dve", lambda: nc.vector.tensor_tensor(out=sel[:], in0=scs[:], in1=brt[:].unsqueeze(1).to_broadcast([128, NT, NEXP]), op=ALU.add), **R)
            T.op("dve", lambda: nc.vector.tensor_reduce(out=m1[:], in_=g4(sel), axis=AX.X, op=ALU.max), **R)
            T.op("dve", lambda: nc.vector.tensor_tensor(out=g4(eq), in0=g4(sel), in1=bc4(m1), op=ALU.is_equal), **R)
            T.op("dve", lambda: nc.vector.scalar_tensor_tensor(out=sel2[:], in0=eq[:], scalar=-1.0e9, in1=sel[:], op0=ALU.mult, op1=ALU.add), **R)
            T.op("dve", lambda: nc.vector.tensor_reduce(out=m2[:], in_=g4(sel2), axis=AX.X, op=ALU.max), **R)
            T.op("dve", lambda: nc.vector.tensor_tensor(out=gs[:], in0=m1[:], in1=m2[:], op=ALU.add), **R)
            T.op("dve", lambda: nc.vector.tensor_reduce(out=gmx[:], in_=gs[:].rearrange("p (t g) -> p t g", g=4), axis=AX.X, op=ALU.max), **R)
            T.op("dve", lambda: nc.vector.tensor_tensor(out=gs[:].rearrange("p (t g) -> p t g", g=4), in0=gs[:].rearrange("p (t g) -> p t g", g=4),
                                                        in1=gmx[:].unsqueeze(2).to_broadcast([128, NT, 4]), op=ALU.is_equal), **R)
            T.op("dve", lambda: nc.vector.tensor_tensor(out=g4(eq), in0=g4(sel), in1=bc4(m2), op=ALU.is_ge), **R)
            T.op("dve", lambda: nc.vector.tensor_tensor(out=g4(eq), in0=g4(eq), in1=bc4(gs), op=ALU.mult), **R)
            T.op("dve", lambda: nc.vector.tensor_tensor(out=sel[:], in0=scs[:], in1=eq[:], op=ALU.mult), **R)
            T.op("dve", lambda: nc.vector.tensor_reduce(out=gmx[:], in_=sel[:], axis=AX.X, op=ALU.add), **R)
            T.op("dve", lambda: nc.vector.reciprocal(out=gmx[:], in_=gmx[:]), **R)
            T.op("dve", lambda: nc.vector.tensor_tensor(out=gates[:], in0=sel[:], in1=gmx[:].unsqueeze(2).to_broadcast([128, NT, NEXP]), op=ALU.mult),
                 reads=[b_r], writes=[b_gates])
        T.barrier()
        DBG = os.environ.get("K_DBG", "")
        if "noexp" in DBG:
            return
        with ExitStack() as es3:
            wg = [P.sb(es3, [128, 8, DEXP], BF16, "wg") for _ in range(2)]
            wu = [P.sb(es3, [128, 8, DEXP], BF16, "wu") for _ in range(2)]
            wd = [P.sb(es3, [128, 4, D], BF16, "wd") for _ in range(2)]
            wbuf = [[[Buf("w%d_%d_%d" % (s_, m_, h_)) for h_ in range(2)] for m_ in range(3)] for s_ in range(2)]
            NSTG = 2
            stg = [P.sb(es3, [128, 2048], F32, "wstg") for _ in range(NSTG)]
            b_stg = [Buf("wstg%d" % i) for i in range(NSTG)]
            sg = [P.sb(es3, [128, 512], F32, "sg") for _ in range(2)]
            b_sg = [Buf("sg%d" % i) for i in range(2)]
            aT = [P.sb(es3, [128, 4, 512], BF16, "aT") for _ in range(2)]
            b_aT = [Buf("aT%d" % i) for i in range(2)]
            tmp = [P.sb(es3, [128, D], F32, "mtmp") for _ in range(2)]
            b_tmp = [Buf("mtmp%d" % i) for i in range(2)]
            si = 0
            ci = 0
            fi = 0
            ti = 0
            for e in range(NEXP if "exp1" not in DBG else 1):
                s_ = e % 2
                for m_, (src, dst, K) in enumerate(((wg_d, wg[s_], 8), (wu_d, wu[s_], 8), (wd_d, wd[s_], 4))):
                    for h_ in range(2):
                        st = stg[si % NSTG]
                        bs = b_stg[si % NSTG]
                        si += 1
                        kh = K // 2
                        ncol = 2048 // kh
                        T.dma("sp", st[:].rearrange("p (k n) -> p k n", k=kh),
                              src[e, h_ * kh * 128:(h_ + 1) * kh * 128, :].rearrange("(k p) n -> p k n", p=128), writes=[bs])
                        T.op("act", lambda: nc.scalar.copy(out=dst[:, h_ * kh:(h_ + 1) * kh, :], in_=st[:].rearrange("p (k n) -> p k n", k=kh)),
                             reads=[bs], writes=[wbuf[s_][m_][h_]])
                for chn, (c0, N) in enumerate(CHUNKS):
                    a = ci % 2
                    ci += 1
                    for f in range(4):
                        gbk = fi % 2
                        ubk = 2 + fi % 2
                        fi += 1
                        for k in range(8):
                            T.op("pe", lambda: nc.tensor.matmul(P.banks[gbk][:, 0:N], lhsT=wg[s_][:, k, f * 128:(f + 1) * 128], rhs=h2T[:, k, c0:c0 + N],
                                                                start=(k == 0), stop=(k == 7)), reads=[wbuf[s_][0][k // 4], h2b[chn]], writes=[P.bb[gbk]])
                        for k in range(8):
                            T.op("pe", lambda: nc.tensor.matmul(P.banks[ubk][:, 0:N], lhsT=wu[s_][:, k, f * 128:(f + 1) * 128], rhs=h2T[:, k, c0:c0 + N],
                                                                start=(k == 0), stop=(k == 7)), reads=[wbuf[s_][1][k // 4], h2b[chn]], writes=[P.bb[ubk]])
                        T.op("act", lambda: nc.scalar.activation(out=sg[gbk][:, 0:N], in_=P.banks[gbk][:, 0:N], func=AF.Silu),
                             reads=[P.bb[gbk]], writes=[b_sg[gbk]])
                        T.op("dve", lambda: nc.vector.tensor_tensor(out=aT[a][:, f, 0:N], in0=sg[gbk][:, 0:N], in1=P.banks[ubk][:, 0:N], op=ALU.mult),
                             reads=[b_sg[gbk], P.bb[ubk]], writes=[b_aT[a]])
                    for tt in range(N // 128):
                        t = c0 // 128 + tt
                        who = 1 if t == NT - 1 else 0
                        yb = (4, 5) if ti % 2 == 0 else (6, 7)
                        tm = ti % 2
                        ti += 1
                        for half in range(2):
                            for f in range(4):
                                T.op("pe", lambda: nc.tensor.matmul(P.banks[yb[half]][:, :], lhsT=aT[a][:, f, tt * 128:(tt + 1) * 128],
                                                                    rhs=wd[s_][:, f, half * 512:(half + 1) * 512], start=(f == 0), stop=(f == 3)),
                                     reads=[b_aT[a], wbuf[s_][2][f // 2]], writes=[P.bb[yb[half]]])
                            T.op("dve", lambda: nc.vector.scalar_tensor_tensor(out=tmp[tm][:, half * 512:(half + 1) * 512], in0=P.banks[yb[half]][:, :],
                                                                               scalar=gates[:, t, e:e + 1], in1=gb[:, who, half * 512:(half + 1) * 512],
                                                                               op0=ALU.mult, op1=ALU.mult),
                                 reads=[P.bb[yb[half]], b_gates, b_gb], writes=[b_tmp[tm]])
                        T.op("pool", lambda: nc.gpsimd.tensor_tensor(out=acc[:, t, :], in0=acc[:, t, :], in1=tmp[tm][:], op=ALU.add),
                             reads=[accb[t], b_tmp[tm]], writes=[accb[t]])
        T.barrier()
        with ExitStack() as es4:
            L = ln_scratch(P, es4, "2")
            ln = P.sb(es4, [128, 2, D], F32, "ln2")
            b_ln = Buf("ln2")
            T.dma("sp", ln[:, 0, :], lng_d[1:2, :].partition_broadcast(128), writes=[b_ln])
            T.dma("sp", ln[:, 1, :], lnb_d[1:2, :].partition_broadcast(128), writes=[b_ln])
            xo = [P.sb(es4, [128, D], F32, "xo") for _ in range(2)]
            b_xo = [Buf("xo%d" % i) for i in range(2)]
            for t in range(NT):
                i = t % 2
                layer_norm_tile(P, L, acc[:, t, :], accb[t], xo[i][:], b_xo[i], ln[:, 0, :], ln[:, 1, :], b_ln)
                T.dma("pool", xo_d[t * 128:(t + 1) * 128, :], xo[i][:], reads=[b_xo[i]], writes=[xob[t]])
    T.barrier()


def rope_tables(hd):
    q = hd // 4
    inv = (10000.0 ** (-np.arange(q, dtype=np.float32) / np.float32(q))).astype(np.float32)
    tpos = np.arange(SEQ)
    ang_r = (tpos // 64).astype(np.float32)[:, None] * inv[None, :]
    ang_c = (tpos % 64).astype(np.float32)[:, None] * inv[None, :]
    cos = np.concatenate([np.cos(ang_r), np.cos(ang_c)], axis=1).astype(np.float32)
    sin = np.concatenate([np.sin(ang_r), np.sin(ang_c)], axis=1).astype(np.float32)
    cos_c = np.ones((NCORES, TOK, 2 * q), np.float32)
    sin_c = np.zeros((NCORES, TOK, 2 * q), np.float32)
    for r in range(NCORES):
        cos_c[r, :2048] = cos[r * 2048:(r + 1) * 2048]
        sin_c[r, :2048] = sin[r * 2048:(r + 1) * 2048]
    return cos_c, sin_c


IDENT = np.eye(128, dtype=np.float32)
_PROG_CACHE = {}


def run(nc, in_maps):
    return run_bass_kernel_spmd(nc, in_maps, core_ids=list(range(NCORES))).results


def modT_layout(modv, l, cols):
    out = np.empty((128, 2, len(cols), 8), np.float32)
    for who in range(2):
        for j, c in enumerate(cols):
            out[:, who, j, :] = modv[l, who, c * 1024:(c + 1) * 1024].reshape(8, 128).T
    return out


def build_mod():
    if "mod" not in _PROG_CACHE:
        P = Prog()
        cc = P.inp("cc", [128, 8, 2], F32)
        wm = P.inp("wm", [DEPTH, D, 768], F32)
        bm = P.inp("bm", [DEPTH, 2, 768], F32)
        out = P.outp("modp", [DEPTH, 2, 768], F32)
        phase_mod(P, cc, wm, bm, out, 768)
        _PROG_CACHE["mod"] = P.close()
    return _PROG_CACHE["mod"]


def run_mod(c, c_ctx, w_mod, b_mod):
    nc = build_mod()
    cc = np.stack([c.reshape(128, 8), c_ctx.reshape(128, 8)], axis=-1).astype(np.float32)
    maps = []
    for r in range(NCORES):
        sl = slice(r * 768, (r + 1) * 768)
        maps.append({"ident": IDENT, "cc": cc, "wm": np.ascontiguousarray(w_mod[:, :, sl]),
                     "bm": np.ascontiguousarray(np.repeat(b_mod[:, None, sl], 2, axis=1))})
    res = run(nc, maps)
    return np.concatenate([res[r]["modp"] for r in range(NCORES)], axis=2)


def build_A(even):
    key = "A%d" % even
    if key not in _PROG_CACHE:
        P = Prog()
        ncols = E_COLS if even else O_COLS
        hq = 32 if even else 64
        x = P.inp("x", [TOK, D], F32)
        w = P.inp("w_in", [D, ncols], F32)
        mA = P.inp("mA", [128, 2, 2, 8], F32)
        cos = P.inp("cos", [TOK, hq], F32)
        sin = P.inp("sin", [TOK, hq], F32)
        qng = None if even else P.inp("qng", [2, 128], F32)
        qkv = P.outp("qkv", [TOK, ncols], BF16)
        phase_A(P, even, x, None, w, mA, cos, sin, qng, qkv)
        _PROG_CACHE[key] = P.close()
    return _PROG_CACHE[key]


def gather_tokens(parts, c0, c1):
    ctx = np.concatenate([parts[0][2048:, c0:c1], parts[1][2048:, c0:c1]], axis=0)
    lat = np.concatenate([p[:2048, c0:c1] for p in parts], axis=0)
    return np.concatenate([ctx, lat], axis=0)


def layout_odd(qkv):
    k_all = gather_tokens(qkv, 1024, 1280)
    v_all = gather_tokens(qkv, 1280, 1536)
    KT = np.ascontiguousarray(k_all.reshape(NKB * 128, 2, 128).transpose(1, 2, 0))
    V = np.ascontiguousarray(v_all.reshape(NKB, 128, 2, 128).transpose(2, 1, 0, 3))
    maps = []
    for r in range(NCORES):
        QT = np.ascontiguousarray(qkv[r][:, 0:1024].reshape(TOK, 8, 128).transpose(1, 2, 0))
        maps.append({"QT": QT, "KT": KT, "V": V})
    return maps


def window_masks():
    m = np.zeros((NCORES, 128, NT, 384), np.float32)
    jj = np.arange(128)[:, None]
    ii = np.arange(128)[None, :]
    left = (ii <= jj).astype(np.float32)
    right = (jj <= ii).astype(np.float32)
    for r in range(NCORES):
        for t in range(16):
            n = 16 * r + t
            if n - 1 >= 0:
                m[r, :, t, 0:128] = left
            m[r, :, t, 128:256] = 1.0
            if n + 1 < SEQ // 128:
                m[r, :, t, 256:384] = right
    return m.astype(NPBF)


def layout_even(qkv, sink, lam4, lam_init, subln_g):
    ak = gather_tokens(qkv, 512, 640)
    av = gather_tokens(qkv, 640, 768)
    bk = gather_tokens(qkv, 1280, 1792)
    bv = gather_tokens(qkv, 1792, 2304)
    KTb = np.ascontiguousarray(bk.reshape(NKB * 128, 4, 128).transpose(1, 2, 0))
    Vb = np.ascontiguousarray(bv.reshape(NKB, 128, 4, 128).transpose(2, 1, 0, 3))
    masks = window_masks()
    sinkp = np.empty((128, 4), np.float32)
    for c in range(4):
        sinkp[:64, c] = sink[2 * c]
        sinkp[64:, c] = sink[2 * c + 1]
    lami = np.empty((128, 2), np.float32)
    lami[:, 0] = lam_init
    lami[:, 1] = 1.0 - lam_init
    akb = ak.reshape(NKB, 128, 128)
    avb = av.reshape(NKB, 128, 2, 64)
    maps = []
    for r in range(NCORES):
        q = qkv[r]
        qb = q[:, 768:1280].reshape(TOK, 4, 2, 64).transpose(1, 2, 3, 0)
        QTb = np.zeros((4, 128, 2, TOK), q.dtype)
        QTb[:, 0:64, 0, :] = qb[:, 0]
        QTb[:, 64:128, 1, :] = qb[:, 1]
        QTa = np.ascontiguousarray(q[:, 0:512].reshape(TOK, 2, 4, 64).transpose(1, 3, 2, 0).reshape(128, 4, TOK))
        kw = np.zeros((20, 128, 128), ak.dtype)
        vw = np.zeros((20, 128, 2, 64), av.dtype)
        kw[0:2] = akb[0:2]
        vw[0:2] = avb[0:2]
        for w in range(2, 20):
            n = 16 * r - 1 + (w - 2)
            if 0 <= n < SEQ // 128:
                kw[w] = akb[2 + n]
                vw[w] = avb[2 + n]
        KTa = np.ascontiguousarray(kw.transpose(2, 0, 1).reshape(128, 20 * 128))
        Va = np.zeros((128, 4, 20, 128), av.dtype)
        for kvh in range(2):
            for lohi in range(2):
                Va[:, kvh * 2 + lohi, :, lohi * 64:lohi * 64 + 64] = vw[:, :, kvh, :].transpose(1, 0, 2)
        maps.append({"QTa": QTa, "KTa": KTa, "Va": Va, "mask": masks[r], "sinkp": sinkp, "QTb": QTb, "KTb": KTb, "Vb": Vb,
                     "lamv": np.ascontiguousarray(lam4.astype(np.float32)), "lami": lami,
                     "subg": np.ascontiguousarray(subln_g.reshape(128, 1).astype(np.float32))})
    return maps


def decl_B(P, even):
    if even:
        return dict(QTa=P.inp("QTa", [128, 4, TOK], BF16), KTa=P.inp("KTa", [128, 20 * 128], BF16), Va=P.inp("Va", [128, 4, 20, 128], BF16),
                    mask=P.inp("mask", [128, NT, 384], BF16), sinkp=P.inp("sinkp", [128, 4], F32), QTb=P.inp("QTb", [4, 128, 2, TOK], BF16),
                    KTb=P.inp("KTb", [4, 128, NKB * 128], BF16), Vb=P.inp("Vb", [4, 128, NKB, 128], BF16), lamv=P.inp("lamv", [4, 64], F32),
                    lami=P.inp("lami", [128, 2], F32), subg=P.inp("subg", [128, 1], F32))
    return dict(QT=P.inp("QT", [8, 128, TOK], BF16), KT=P.inp("KT", [2, 128, NKB * 128], BF16), V=P.inp("V", [2, 128, NKB, 128], BF16))


def emit_B(P, even, OT, b_OT, d):
    if even:
        phase_B_even(P, OT, b_OT, d["QTa"], d["KTa"], d["Va"], d["mask"], d["sinkp"], d["QTb"], d["KTb"], d["Vb"], d["lamv"], d["lami"], d["subg"])
    else:
        phase_B_odd(P, OT, b_OT, d["QT"], d["KT"], d["V"])


def build_Btest(even):
    key = "Bt%d" % even
    if key not in _PROG_CACHE:
        P = Prog()
        d = decl_B(P, even)
        out = P.outp("OT", [128, 8, TOK], BF16)
        OT = P.sb(P.es, [128, 8, TOK], BF16, "OT")
        b_OT = Buf("OT")
        emit_B(P, even, OT, b_OT, d)
        P.T.dma("pool", out[:, :, :], OT[:], reads=[b_OT])
        _PROG_CACHE[key] = P.close()
    return _PROG_CACHE[key]


def build_BCA(even, has_A):
    key = "BCA%d%d" % (even, has_A)
    if key in _PROG_CACHE:
        return _PROG_CACHE[key]
    P = Prog()
    nc = P.nc
    dB = decl_B(P, even)
    x = P.inp("x", [TOK, D], F32)
    wo = P.inp("w_out", [D, D], F32)
    gbc = P.inp("gbc", [2, 2, D], F32)
    mC = P.inp("mC", [128, 2, 2, 8], F32)
    lng = P.inp("lng", [2, D], F32)
    lnb = P.inp("lnb", [2, D], F32)
    wr = P.inp("wr", [D, NEXP], F32)
    br = P.inp("br", [1, NEXP], F32)
    wg = P.inp("wg", [NEXP, D, DEXP], F32)
    wu = P.inp("wu", [NEXP, D, DEXP], F32)
    wd = P.inp("wd", [NEXP, DEXP, D], F32)
    x1s = nc.dram_tensor("x1s", [TOK, D], F32, kind="Internal").ap()
    xo = P.outp("xo", [TOK, D], F32)
    x1b = [Buf("x1s%d" % t) for t in range(NT)]
    xob = [Buf("xo%d" % t) for t in range(NT)]
    if has_A:
        a_even = not even
        ncols = E_COLS if a_even else O_COLS
        hq = 32 if a_even else 64
        w_in = P.inp("w_in", [D, ncols], F32)
        mA = P.inp("mA", [128, 2, 2, 8], F32)
        cos = P.inp("cos", [TOK, hq], F32)
        sin = P.inp("sin", [TOK, hq], F32)
        qng = None if a_even else P.inp("qng", [2, 128], F32)
        qkv = P.outp("qkv", [TOK, ncols], BF16)
    with ExitStack() as es:
        OT = P.sb(es, [128, 8, TOK], BF16, "OT")
        b_OT = Buf("OT")
        emit_B(P, even, OT, b_OT, dB)
        phase_C1(P, OT, b_OT, x, wo, gbc, lng, lnb, x1s, x1b)
    if "noC2" not in os.environ.get("K_DBG", ""):
        phase_C2(P, x1s, x1b, mC, gbc, lng, lnb, wr, br, wg, wu, wd, xo, xob)
    if has_A and "noA" not in os.environ.get("K_DBG", ""):
        phase_A(P, a_even, xo, xob, w_in, mA, cos, sin, qng, qkv)
    print("BCA program: %d instructions, %d waits, %d dma sems" % (P.T.n_ins, P.T.n_wait, P.T.nsem), flush=True)
    _PROG_CACHE[key] = P.close()
    return _PROG_CACHE[key]


def a_inputs(inputs, modv, l, r, tabs):
    even = (l % 2 == 0)
    i = l // 2
    cos_c, sin_c = tabs[64 if even else 128]
    m = {"w_in": inputs["w_in_even"][i] if even else inputs["w_in_odd"][i], "mA": modT_layout(modv, l, [1, 0]),
         "cos": cos_c[r], "sin": sin_c[r]}
    if not even:
        m["qng"] = np.stack([inputs["q_norm_g"][i], inputs["k_norm_g"][i]]).astype(np.float32)
    return m


def kernel(**inputs):
    inputs = {k: np.asarray(v) for k, v in inputs.items()}
    x = inputs["x"][0]
    ctx = inputs["ctx"][0]
    tabs = {64: rope_tables(64), 128: rope_tables(128)}
    modv = run_mod(inputs["c"][0], inputs["c_ctx"], inputs["w_mod"], inputs["b_mod"])
    xres = [np.ascontiguousarray(np.concatenate([x[r * 2048:(r + 1) * 2048], ctx[(r % 2) * 128:(r % 2) * 128 + 128]], 0)) for r in range(NCORES)]
    maps = []
    for r in range(NCORES):
        m = a_inputs(inputs, modv, 0, r, tabs)
        m.update({"ident": IDENT, "x": xres[r]})
        maps.append(m)
    res = run(build_A(True), maps)
    qkv = [res[r]["qkv"] for r in range(NCORES)]
    for l in range(DEPTH):
        even = (l % 2 == 0)
        i = l // 2
        has_A = l < DEPTH - 1
        if even:
            lam_init = 0.8 - 0.6 * float(np.exp(-0.3 * l))
            lam4 = np.stack([inputs["lam_q1"][i], inputs["lam_k1"][i], inputs["lam_q2"][i], inputs["lam_k2"][i]])
            maps = layout_even(qkv, inputs["sink_logits"][i], lam4, lam_init, inputs["subln_g"][i])
        else:
            maps = layout_odd(qkv)
        gbc = np.ascontiguousarray(np.stack([np.stack([modv[l, who, 2048:3072], modv[l, who, 5120:6144]]) for who in range(2)]))
        mC = modT_layout(modv, l, [4, 3])
        for r in range(NCORES):
            m = maps[r]
            m.update({"ident": IDENT, "x": xres[r], "w_out": inputs["w_out_even"][i] if even else inputs["w_out_odd"][i], "gbc": gbc, "mC": mC,
                      "lng": inputs["ln_g"][l], "lnb": inputs["ln_b"][l], "wr": inputs["w_router"], "br": inputs["b_router"].reshape(1, NEXP),
                      "wg": inputs["w_gate"][l], "wu": inputs["w_up"][l], "wd": inputs["w_down"][l]})
            if has_A:
                m.update(a_inputs(inputs, modv, l + 1, r, tabs))
        res = run(build_BCA(even, has_A), maps)
        xres = [res[r]["xo"] for r in range(NCORES)]
        if has_A:
            qkv = [res[r]["qkv"] for r in range(NCORES)]
    out = np.concatenate([xres[r][:2048] for r in range(NCORES)], axis=0)
    return out.reshape(1, SEQ, D).astype(np.float32)
```

```python
import os
import numpy as np
import ml_dtypes
from contextlib import ExitStack
import concourse.bass as bass
import concourse.mybir as mybir
from concourse.bass_utils import run_bass_kernel_spmd

F32 = mybir.dt.float32
BF16 = mybir.dt.bfloat16
AF = mybir.ActivationFunctionType
ALU = mybir.AluOpType
AX = mybir.AxisListType
NPBF = ml_dtypes.bfloat16

NCORES = 8
D = 1024
SEQ = 16384
CTX = 256
NT = 17
TOK = NT * 128
NKB = (SEQ + CTX) // 128
DEPTH = 4
ALPHA = (2 * DEPTH) ** 0.25
LN_EPS = 1e-5
QK_EPS = 1e-6
SUBLN_EPS = 1e-5
E_COLS = 2304
O_COLS = 1536
NEXP = 16
DEXP = 512
CHUNKS = [(0, 512), (512, 512), (1024, 512), (1536, 512), (2048, 128)]


class Buf:
    __slots__ = ("name", "w", "rs", "dsem", "dcnt", "excl")

    def __init__(self, name, excl=False):
        self.name = name
        self.w = None
        self.rs = {}
        self.dsem = None
        self.dcnt = 0
        self.excl = excl


class Trk:
    def __init__(self, nc, es):
        self.nc = nc
        self.es = es
        self.eng = {"pe": nc.tensor, "act": nc.scalar, "dve": nc.vector, "pool": nc.gpsimd, "sp": nc.sync}
        self.sem = {}
        self.cnt = {}
        self.waited = {k: {} for k in self.eng}
        self.nsem = 0
        self.dbufs = []
        for k in self.eng:
            self.sem[k] = es.enter_context(nc.semaphore("e_" + k))
            self.cnt[k] = 0
        self.n_ins = 0
        self.n_wait = 0

    def _deps(self, reads, writes, e=None):
        deps = {}
        own = self.sem.get(e)

        def add(t):
            if t is None:
                return
            k = id(t[0])
            if k not in deps or deps[k][1] < t[1]:
                deps[k] = t
        for b in reads:
            add(b.w)
            if b.excl:
                for t in b.rs.values():
                    if t[0] is not own:
                        add(t)
        for b in writes:
            add(b.w)
            for t in b.rs.values():
                add(t)
        return deps

    def _emit_waits(self, e, deps):
        own = self.sem.get(e)
        eo = self.eng[e]
        wd = self.waited[e]
        for k, (s, v) in deps.items():
            if s is own and e in ("pe", "sp"):
                continue
            if wd.get(k, 0) >= v:
                continue
            eo.wait_ge(s, v)
            wd[k] = v
            self.n_wait += 1

    def _commit(self, t, reads, writes):
        k = id(t[0])
        for b in reads:
            b.rs[k] = t
        for b in writes:
            b.w = t
            b.rs = {}

    def op(self, e, fn, reads=(), writes=()):
        self._emit_waits(e, self._deps(reads, writes, e))
        ins = fn()
        self.cnt[e] += 1
        ins.then_inc(self.sem[e], 1)
        self._commit((self.sem[e], self.cnt[e]), reads, writes)
        self.n_ins += 1
        return ins

    def dma(self, q, out, in_, reads=(), writes=(), **kw):
        self._emit_waits(q, self._deps(reads, writes, q))
        owner = writes[0] if len(writes) else reads[0]
        if owner.dsem is None:
            owner.dsem = self.es.enter_context(self.nc.semaphore("d_%d" % self.nsem))
            self.nsem += 1
            self.dbufs.append(owner)
        ins = self.eng[q].dma_start(out=out, in_=in_, **kw)
        owner.dcnt += 16
        ins.then_inc(owner.dsem, 16)
        self._commit((owner.dsem, owner.dcnt), reads, writes)
        self.n_ins += 1
        return ins

    def barrier(self):
        deps = {}
        for k in self.eng:
            if self.cnt[k]:
                deps[id(self.sem[k])] = (self.sem[k], self.cnt[k])
        for b in self.dbufs:
            deps[id(b.dsem)] = (b.dsem, b.dcnt)
        for e in self.eng:
            own = self.sem[e]
            eo = self.eng[e]
            wd = self.waited[e]
            for k, (s, v) in deps.items():
                if s is own or wd.get(k, 0) >= v:
                    continue
                eo.wait_ge(s, v)
                wd[k] = v

    def finish(self, e="pool"):
        deps = {}
        for b in self.dbufs:
            deps[id(b.dsem)] = (b.dsem, b.dcnt)
        for k in self.eng:
            if self.cnt[k]:
                deps[id(self.sem[k])] = (self.sem[k], self.cnt[k])
        self._emit_waits(e, deps)


class Prog:
    def __init__(self):
        self.nc = bass.Bass("TRN2", target_bir_lowering=False)
        self.es = ExitStack()
        self.T = Trk(self.nc, self.es)
        self.pbig = [self.es.enter_context(self.nc.psum_tensor("pbig%d" % i, [128, 1024], F32)) for i in range(4)]
        self.banks = [self.pbig[i // 2][:, (i % 2) * 512:(i % 2 + 1) * 512] for i in range(8)]
        self.bb = [Buf("bank%d" % i, excl=True) for i in range(8)]
        self.uid = 0
        nc, T = self.nc, self.T
        self.ident_d = self.inp("ident", [128, 128], F32)
        self.ident = self.sb(self.es, [128, 128], F32)
        self.b_ident = Buf("ident")
        T.dma("sp", self.ident[:], self.ident_d[:, :], writes=[self.b_ident])
        self.ones = self.sb(self.es, [128, 128], BF16)
        self.b_ones = Buf("ones")
        T.op("pool", lambda: nc.gpsimd.memset(self.ones[:], 1.0), writes=[self.b_ones])
        self.onesd = self.sb(self.es, [128, 128], F32)
        self.b_onesd = Buf("onesd")
        T.op("pool", lambda: nc.gpsimd.memset(self.onesd[:], 1.0 / 128.0), writes=[self.b_onesd])

    def inp(self, name, shape, dt):
        return self.nc.dram_tensor(name, list(shape), dt, kind="ExternalInput").ap()

    def outp(self, name, shape, dt):
        return self.nc.dram_tensor(name, list(shape), dt, kind="ExternalOutput").ap()

    def sb(self, es, shape, dt, name=None):
        self.uid += 1
        return es.enter_context(self.nc.sbuf_tensor("%s_%d" % (name or "t", self.uid), list(shape), dt))

    def close(self):
        self.T.finish("pool")
        self.es.close()
        return self.nc


def bcast_rows(ap_row, n):
    return ap_row.partition_broadcast(n)


def phase_mod(P, cc_d, wm_d, bm_d, out_d, ncols):
    nc, T = P.nc, P.T
    with ExitStack() as es:
        cc = P.sb(es, [128, 8, 2], F32)
        b_cc = Buf("cc")
        T.dma("sp", cc[:], cc_d[:, :, :], writes=[b_cc])
        ccs = P.sb(es, [128, 8, 2], F32)
        b_ccs = Buf("ccs")
        T.op("act", lambda: nc.scalar.activation(out=ccs[:], in_=cc[:], func=AF.Silu), reads=[b_cc], writes=[b_ccs])
        CW = 384
        stg = [P.sb(es, [128, 8, CW], F32) for _ in range(2)]
        b_stg = [Buf("stg%d" % i) for i in range(2)]
        bt = [P.sb(es, [2, CW], F32) for _ in range(2)]
        b_bt = [Buf("bt%d" % i) for i in range(2)]
        rs = [P.sb(es, [2, CW], F32) for _ in range(2)]
        b_rs = [Buf("rs%d" % i) for i in range(2)]
        it = 0
        for l in range(DEPTH):
            for c0 in range(0, ncols, CW):
                i = it % 2
                it += 1
                T.dma("sp", stg[i][:], wm_d[l, :, c0:c0 + CW].rearrange("(p k) n -> p k n", k=8), writes=[b_stg[i]])
                T.dma("sp", bt[i][:], bm_d[l, :, c0:c0 + CW], writes=[b_bt[i]])
                ps = P.banks[i]
                for k in range(8):
                    T.op("pe", lambda: nc.tensor.matmul(ps[0:2, 0:CW], lhsT=ccs[:, k, :], rhs=stg[i][:, k, :], start=(k == 0), stop=(k == 7)),
                         reads=[b_ccs, b_stg[i]], writes=[P.bb[i]])
                T.op("dve", lambda: nc.vector.tensor_tensor(out=rs[i][:], in0=ps[0:2, 0:CW], in1=bt[i][:], op=ALU.add),
                     reads=[P.bb[i], b_bt[i]], writes=[b_rs[i]])
                T.dma("pool", out_d[l, :, c0:c0 + CW], rs[i][:], reads=[b_rs[i]])
    T.barrier()


def load_weight_bf16(P, es, src, K, ncols, name):
    nc, T = P.nc, P.T
    dst = P.sb(es, [128, K, ncols], BF16, name)
    CW = 4096 // K
    stg = [P.sb(es, [128, K, CW], F32, name + "s") for _ in range(2)]
    b_stg = [Buf(name + "s%d" % i) for i in range(2)]
    bufs = {}
    it = 0
    for c0 in range(0, ncols, CW):
        cw = min(CW, ncols - c0)
        i = it % 2
        it += 1
        T.dma("sp", stg[i][:, :, 0:cw], src[:, c0:c0 + cw].rearrange("(k p) n -> p k n", p=128), writes=[b_stg[i]])
        b = Buf(name + "_c%d" % c0)
        T.op("act", lambda: nc.scalar.copy(out=dst[:, :, c0:c0 + cw], in_=stg[i][:, :, 0:cw]), reads=[b_stg[i]], writes=[b])
        for c in range(c0, c0 + cw, 128):
            bufs[c // 128] = b
    return dst, bufs


def rope_tok(P, src, dst, nh, hd, cos, sin, b_src, b_dst, b_tab, tmp, b_tmp):
    nc, T = P.nc, P.T
    q = hd // 4
    sv = src.rearrange("p (h a j i) -> p h a j i", h=nh, a=2, j=2, i=q)
    dv = dst.rearrange("p (h a j i) -> p h a j i", h=nh, a=2, j=2, i=q)
    x1 = sv[:, :, :, 0, :]
    x2 = sv[:, :, :, 1, :]
    cb = cos.rearrange("p (a i) -> p a i", a=2).unsqueeze(1).to_broadcast([128, nh, 2, q])
    sbc = sin.rearrange("p (a i) -> p a i", a=2).unsqueeze(1).to_broadcast([128, nh, 2, q])
    n = nh * 2 * q
    t = [tmp[j][:, 0:n].rearrange("p (h a i) -> p h a i", h=nh, a=2, i=q) for j in range(4)]
    T.op("dve", lambda: nc.vector.tensor_tensor(out=t[0], in0=x1, in1=cb, op=ALU.mult), reads=[b_src, b_tab], writes=[b_tmp[0]])
    T.op("dve", lambda: nc.vector.tensor_tensor(out=t[1], in0=x2, in1=sbc, op=ALU.mult), reads=[b_src, b_tab], writes=[b_tmp[1]])
    T.op("dve", lambda: nc.vector.tensor_tensor(out=t[2], in0=x1, in1=sbc, op=ALU.mult), reads=[b_src, b_tab], writes=[b_tmp[2]])
    T.op("dve", lambda: nc.vector.tensor_tensor(out=t[3], in0=x2, in1=cb, op=ALU.mult), reads=[b_src, b_tab], writes=[b_tmp[3]])
    T.op("pool", lambda: nc.gpsimd.tensor_tensor(out=dv[:, :, :, 0, :], in0=t[0], in1=t[1], op=ALU.subtract),
         reads=[b_tmp[0], b_tmp[1]], writes=[b_dst])
    T.op("pool", lambda: nc.gpsimd.tensor_tensor(out=dv[:, :, :, 1, :], in0=t[2], in1=t[3], op=ALU.add),
         reads=[b_tmp[2], b_tmp[3]], writes=[b_dst])


def transpose_mod(P, xt, b_xt, scT, shT, b_mod, hT, b_hT, tb, hTf=None, b_hTf=None):
    nc, T = P.nc, P.T
    for k in range(8):
        bk = tb[k // 4]
        T.op("pe", lambda: nc.tensor.transpose(P.banks[bk][:, (k % 4) * 128:(k % 4 + 1) * 128], xt[:, k * 128:(k + 1) * 128], P.ident[:]),
             reads=[b_xt, P.b_ident], writes=[P.bb[bk]])
    for k in range(8):
        bk = tb[k // 4]
        src = P.banks[bk][:, (k % 4) * 128:(k % 4 + 1) * 128]
        if hTf is None:
            T.op("act", lambda: nc.scalar.activation(out=hT[:, k, :], in_=src, func=AF.Identity, bias=shT[:, k:k + 1], scale=scT[:, k:k + 1]),
                 reads=[P.bb[bk], b_mod], writes=[b_hT])
        else:
            T.op("act", lambda: nc.scalar.activation(out=hTf[:, k, :], in_=src, func=AF.Identity, bias=shT[:, k:k + 1], scale=scT[:, k:k + 1]),
                 reads=[P.bb[bk], b_mod], writes=[b_hTf])
    if hTf is not None:
        T.op("dve", lambda: nc.vector.tensor_copy(out=hT, in_=hTf[:]), reads=[b_hTf], writes=[b_hT])


def load_modT(P, es, m_d, name):
    nc, T = P.nc, P.T
    m = P.sb(es, [128, 2, 2, 8], F32, name)
    b = Buf(name)
    T.dma("sp", m[:], m_d[:, :, :, :], writes=[b])
    T.op("dve", lambda: nc.vector.tensor_scalar(out=m[:, :, 0, :], in0=m[:, :, 0, :], scalar1=1.0, scalar2=None, op0=ALU.add),
         reads=[b], writes=[b])
    return m, b


def phase_A(P, even, x_d, x_bufs, w_d, mA_d, cos_d, sin_d, qng_d, qkv_d):
    nc, T = P.nc, P.T
    ncols = E_COLS if even else O_COLS
    hd = 64 if even else 128
    hq = hd // 2
    with ExitStack() as es:
        W, wb = load_weight_bf16(P, es, w_d, 8, ncols, "win")
        m, b_m = load_modT(P, es, mA_d, "mA")
        xt = [P.sb(es, [128, D], F32, "xt") for _ in range(2)]
        b_xt = [Buf("xt%d" % i) for i in range(2)]
        hT = [P.sb(es, [128, 8, 128], BF16, "hT") for _ in range(2)]
        b_hT = [Buf("hT%d" % i) for i in range(2)]
        ot = [P.sb(es, [128, ncols], BF16, "ot") for _ in range(2)]
        b_ot = [Buf("ot%d" % i) for i in range(2)]
        cs = [P.sb(es, [128, 2, hq], F32, "cs") for _ in range(2)]
        b_cs = [Buf("cs%d" % i) for i in range(2)]
        tmp = [P.sb(es, [128, 512], F32, "rt") for _ in range(4)]
        b_tmp = [Buf("rt%d" % i) for i in range(4)]
        if not even:
            gq = P.sb(es, [128, 2, 128], F32, "gq")
            b_gq = Buf("gq")
            T.dma("sp", gq[:, 0, :], qng_d[0:1, :].partition_broadcast(128), writes=[b_gq])
            T.dma("sp", gq[:, 1, :], qng_d[1:2, :].partition_broadcast(128), writes=[b_gq])
            epsq = P.sb(es, [128, 1], F32, "epsq")
            b_epsq = Buf("epsq")
            T.op("pool", lambda: nc.gpsimd.memset(epsq[:], QK_EPS), writes=[b_epsq])
            sq = P.sb(es, [128, 512], F32, "sq")
            b_sq = Buf("sq")
            ssq = P.sb(es, [128, 4], F32, "ssq")
            b_ssq = Buf("ssq")
            xn = [P.sb(es, [128, 512], F32, "xn") for _ in range(2)]
            b_xn = [Buf("xn%d" % i) for i in range(2)]
        nbk = (ncols + 511) // 512
        for t in range(NT):
            i = t % 2
            who = 1 if t == NT - 1 else 0
            rd = [x_bufs[t]] if x_bufs is not None else []
            T.dma("sp", xt[i][:], x_d[t * 128:(t + 1) * 128, :], reads=rd, writes=[b_xt[i]])
            T.dma("sp", cs[i][:, 0, :], cos_d[t * 128:(t + 1) * 128, :], writes=[b_cs[i]])
            T.dma("sp", cs[i][:, 1, :], sin_d[t * 128:(t + 1) * 128, :], writes=[b_cs[i]])
            transpose_mod(P, xt[i], b_xt[i], m[:, who, 0, :], m[:, who, 1, :], b_m, hT[i], b_hT[i], (6, 7))
            for c in range(nbk):
                cw = min(512, ncols - c * 512)
                for k in range(8):
                    T.op("pe", lambda: nc.tensor.matmul(P.banks[c][:, 0:cw], lhsT=hT[i][:, k, :], rhs=W[:, k, c * 512:c * 512 + cw],
                                                        start=(k == 0), stop=(k == 7)),
                         reads=[b_hT[i], wb[c * 4]] + ([wb[c * 4 + 3]] if cw == 512 else []), writes=[P.bb[c]])
            o = ot[i]
            cosv, sinv = cs[i][:, 0, :], cs[i][:, 1, :]
            if even:
                def rp(bank, c0, c1):
                    rope_tok(P, P.banks[bank][:, c0 - bank * 512:c1 - bank * 512], o[:, c0:c1], (c1 - c0) // 64, 64, cosv, sinv,
                             P.bb[bank], b_ot[i], b_cs[i], tmp, b_tmp)
                rp(0, 0, 512)
                rp(1, 512, 640)
                rp(1, 768, 1024)
                rp(2, 1024, 1536)
                rp(3, 1536, 1792)
                T.op("act", lambda: nc.scalar.copy(out=o[:, 640:768], in_=P.banks[1][:, 128:256]), reads=[P.bb[1]], writes=[b_ot[i]])
                T.op("act", lambda: nc.scalar.copy(out=o[:, 1792:2048], in_=P.banks[3][:, 256:512]), reads=[P.bb[3]], writes=[b_ot[i]])
                T.op("act", lambda: nc.scalar.copy(out=o[:, 2048:2304], in_=P.banks[4][:, 0:256]), reads=[P.bb[4]], writes=[b_ot[i]])
            else:
                for bank, nh, gi in ((0, 4, 0), (1, 4, 0), (2, 2, 1)):
                    n = nh * 128
                    src = P.banks[bank][:, 0:n]
                    T.op("act", lambda: nc.scalar.activation(out=sq[:, 0:n], in_=src, func=AF.Square), reads=[P.bb[bank]], writes=[b_sq])
                    T.op("dve", lambda: nc.vector.tensor_reduce(out=ssq[:, 0:nh], in_=sq[:, 0:n].rearrange("p (h d) -> p h d", h=nh),
                                                                axis=AX.X, op=ALU.add), reads=[b_sq], writes=[b_ssq])
                    T.op("act", lambda: nc.scalar.activation(out=ssq[:, 0:nh], in_=ssq[:, 0:nh], func=AF.Sqrt, bias=epsq[:, 0:1], scale=1.0 / 128.0),
                         reads=[b_ssq, b_epsq], writes=[b_ssq])
                    T.op("dve", lambda: nc.vector.reciprocal(out=ssq[:, 0:nh], in_=ssq[:, 0:nh]), reads=[b_ssq], writes=[b_ssq])
                    j = bank % 2
                    T.op("dve", lambda: nc.vector.tensor_tensor(out=xn[j][:, 0:n].rearrange("p (h d) -> p h d", h=nh),
                                                                in0=src.rearrange("p (h d) -> p h d", h=nh),
                                                                in1=ssq[:, 0:nh].unsqueeze(2).to_broadcast([128, nh, 128]), op=ALU.mult),
                         reads=[P.bb[bank], b_ssq], writes=[b_xn[j]])
                    T.op("pool", lambda: nc.gpsimd.tensor_tensor(out=xn[j][:, 0:n].rearrange("p (h d) -> p h d", h=nh),
                                                                 in0=xn[j][:, 0:n].rearrange("p (h d) -> p h d", h=nh),
                                                                 in1=gq[:, gi, :].unsqueeze(1).to_broadcast([128, nh, 128]), op=ALU.mult),
                         reads=[b_xn[j], b_gq], writes=[b_xn[j]])
                    rope_tok(P, xn[j][:, 0:n], o[:, bank * 512:bank * 512 + n], nh, 128, cosv, sinv, b_xn[j], b_ot[i], b_cs[i], tmp, b_tmp)
                T.op("act", lambda: nc.scalar.copy(out=o[:, 1280:1536], in_=P.banks[2][:, 256:512]), reads=[P.bb[2]], writes=[b_ot[i]])
            T.dma("pool", qkv_d[t * 128:(t + 1) * 128, :], o[:], reads=[b_ot[i]])
    T.barrier()


class SweepCtx:
    def __init__(self, P, es, npt=5):
        self.P = P
        self.pT = [P.sb(es, [128, 2, 512], BF16, "pT") for _ in range(npt)]
        self.b_pT = [Buf("pT%d" % i) for i in range(npt)]
        self.n = 0
        self.ps = [P.sb(es, [128, 512], BF16, "psum2") for _ in range(4)]
        self.b_ps = [Buf("psum2_%d" % i) for i in range(4)]
        self.m = 0


def sweep(P, S, N, blocks, qT, b_q, kT_of, v_of, b_kv, M, scale):
    nc, T = P.nc, P.T
    SK = 2
    ob, sb_ = 6, 7
    nb = len(blocks)
    assert nb % 2 == 0
    npair = nb // 2
    idx = []
    pidx = []
    for u in range(npair + SK):
        if u < npair:
            n = S.n
            S.n += 1
            idx.append(n)
            sp = n % 3
            p = n % len(S.pT)
            for b in range(2):
                bk = 2 * sp + b
                T.op("pe", lambda: nc.tensor.matmul(P.banks[bk][:, 0:N], lhsT=kT_of(blocks[2 * u + b]), rhs=qT, start=True, stop=True),
                     reads=[b_kv, b_q], writes=[P.bb[bk]])
            src = P.pbig[sp][:, :].rearrange("p (b n) -> p b n", b=2)[:, :, 0:N]
            T.op("act", lambda: nc.scalar.activation(out=S.pT[p][:, :, 0:N], in_=src, func=AF.Exp, scale=scale),
                 reads=[P.bb[2 * sp], P.bb[2 * sp + 1]], writes=[S.b_pT[p]])
        if 1 <= u <= npair:
            p = idx[u - 1] % len(S.pT)
            q = S.m % len(S.ps)
            S.m += 1
            pidx.append(q)
            T.op("dve", lambda: nc.vector.tensor_tensor(out=S.ps[q][:, 0:N], in0=S.pT[p][:, 0, 0:N], in1=S.pT[p][:, 1, 0:N], op=ALU.add),
                 reads=[S.b_pT[p]], writes=[S.b_ps[q]])
        if u >= SK:
            v = u - SK
            p = idx[v] % len(S.pT)
            for b in range(2):
                j = 2 * v + b
                T.op("pe", lambda: nc.tensor.matmul(P.banks[ob][0:M, 0:N], lhsT=v_of(blocks[j]), rhs=S.pT[p][:, b, 0:N], start=(j == 0), stop=(j == nb - 1)),
                     reads=[b_kv, S.b_pT[p]], writes=[P.bb[ob]])
            q = pidx[v]
            T.op("pe", lambda: nc.tensor.matmul(P.banks[sb_][:, 0:N], lhsT=P.ones[:], rhs=S.ps[q][:, 0:N], start=(v == 0), stop=(v == npair - 1)),
                 reads=[P.b_ones, S.b_ps[q]], writes=[P.bb[sb_]])
    return ob, sb_


ALL_BLOCKS = list(range(NKB))
CTX_BLOCKS = [0, 1]


def phase_B_odd(P, OT, b_OT, QT_d, KT_d, V_d):
    nc, T = P.nc, P.T
    scale = 128.0 ** -0.5
    with ExitStack() as es:
        S = SweepCtx(P, es)
        kts = [P.sb(es, [128, NKB * 128], BF16, "kt") for _ in range(2)]
        vts = [P.sb(es, [128, NKB, 128], BF16, "vt") for _ in range(2)]
        b_kvs = [Buf("kv%d" % i) for i in range(2)]
        qt = [P.sb(es, [128, TOK], BF16, "qt") for _ in range(2)]
        b_qt = [Buf("qt%d" % i) for i in range(2)]
        rec = [P.sb(es, [128, 512], F32, "rec") for _ in range(2)]
        b_rec = [Buf("rec%d" % i) for i in range(2)]
        fi = 0
        for kvh in range(2):
            kt, vt, b_kv = kts[kvh], vts[kvh], b_kvs[kvh]
            T.dma("sp", kt[:], KT_d[kvh, :, :], writes=[b_kv])
            T.dma("sp", vt[:], V_d[kvh, :, :, :], writes=[b_kv])
        for kvh in range(2):
            kt, vt, b_kv = kts[kvh], vts[kvh], b_kvs[kvh]
            for g in range(4):
                h = kvh * 4 + g
                qi = h % 2
                T.dma("sp", qt[qi][:], QT_d[h, :, :], writes=[b_qt[qi]])
                for (c0, N) in CHUNKS:
                    blocks = ALL_BLOCKS if N == 512 else CTX_BLOCKS
                    ksl = slice(64, 128) if "k64" in os.environ.get("K_DBG", "") else slice(0, 128)
                    ob, sbk = sweep(P, S, N, blocks, qt[qi][ksl, c0:c0 + N], b_qt[qi],
                                    lambda b: kt[ksl, b * 128:(b + 1) * 128], lambda b: vt[:, b, :], b_kv, 128, scale)
                    r = fi % 2
                    fi += 1
                    T.op("dve", lambda: nc.vector.reciprocal(out=rec[r][:, 0:N], in_=P.banks[sbk][:, 0:N]), reads=[P.bb[sbk]], writes=[b_rec[r]])
                    T.op("dve", lambda: nc.vector.tensor_tensor(out=OT[:, h, c0:c0 + N], in0=P.banks[ob][:, 0:N], in1=rec[r][:, 0:N], op=ALU.mult),
                         reads=[P.bb[ob], b_rec[r]], writes=[b_OT])
    T.barrier()


def phase_B_even(P, OT, b_OT, QTa_d, KTa_d, Va_d, mask_d, sinkp_d, QTb_d, KTb_d, Vb_d, lamv_d, lami_d, subg_d):
    nc, T = P.nc, P.T
    sc = 64.0 ** -0.5
    with ExitStack() as es:
        if "nomA" in os.environ.get("K_DBG", ""):
            es.close()
            return phase_B_even_mixB(P, OT, b_OT, QTb_d, KTb_d, Vb_d, lamv_d, lami_d, subg_d)
        qta = P.sb(es, [128, 4, TOK], BF16, "qta")
        b_qta = Buf("qta")
        T.dma("sp", qta[:], QTa_d[:, :, :], writes=[b_qta])
        ktaw = P.sb(es, [128, 20 * 128], BF16, "ktaw")
        b_kta = Buf("ktaw")
        T.dma("sp", ktaw[:], KTa_d[:, :], writes=[b_kta])
        vaw = P.sb(es, [128, 4, 20, 128], BF16, "vaw")
        b_vaw = Buf("vaw")
        T.dma("sp", vaw[:], Va_d[:, :, :, :], writes=[b_vaw])
        msk = P.sb(es, [128, NT, 384], BF16, "msk")
        b_msk = Buf("msk")
        T.dma("sp", msk[:], mask_d[:, :, :], writes=[b_msk])
        esink = P.sb(es, [128, 4], F32, "esink")
        b_es = Buf("esink")
        T.dma("sp", esink[:], sinkp_d[:, :], writes=[b_es])
        T.op("act", lambda: nc.scalar.activation(out=esink[:], in_=esink[:], func=AF.Exp), reads=[b_es], writes=[b_es])
        oneslh = P.sb(es, [128, 2, 128], BF16, "oneslh")
        b_olh = Buf("oneslh")
        T.op("pool", lambda: nc.gpsimd.memset(oneslh[:], 0.0), writes=[b_olh])
        T.op("pool", lambda: nc.gpsimd.memset(oneslh[:, 0, 0:64], 1.0), reads=[b_olh], writes=[b_olh])
        T.op("pool", lambda: nc.gpsimd.memset(oneslh[:, 1, 64:128], 1.0), reads=[b_olh], writes=[b_olh])
        pTa = [P.sb(es, [128, 640], BF16, "pTa") for _ in range(4)]
        b_pTa = [Buf("pTa%d" % i) for i in range(4)]
        den = [P.sb(es, [128, 128], F32, "den") for _ in range(2)]
        b_den = [Buf("den%d" % i) for i in range(2)]
        n = 0
        fi = 0
        for c in range(4):
            kvh = c // 2
            ksl = slice(kvh * 64, kvh * 64 + 64)
            for t in range(NT):
                wl = (t + 2) if t < NT - 1 else 2
                blocks = [0, 1, wl, wl + 1, wl + 2]
                ob, sbk = (4, 5) if fi % 2 == 0 else (6, 7)
                for hh in range(2):
                    j = (2 * c + hh) % 4
                    p = n % 4
                    s0, s1 = (0, 1) if n % 2 == 0 else (2, 3)
                    n += 1
                    q_ap = qta[ksl, j, t * 128:(t + 1) * 128]
                    for bi, w in enumerate(blocks):
                        bk, col = (s0, bi * 128) if bi < 2 else (s1, (bi - 2) * 128)
                        T.op("pe", lambda: nc.tensor.matmul(P.banks[bk][:, col:col + 128], lhsT=ktaw[ksl, w * 128:(w + 1) * 128], rhs=q_ap,
                                                            start=True, stop=True), reads=[b_kta, b_qta], writes=[P.bb[bk]])
                    T.op("act", lambda: nc.scalar.activation(out=pTa[p][:, 0:256], in_=P.banks[s0][:, 0:256], func=AF.Exp, scale=sc),
                         reads=[P.bb[s0]], writes=[b_pTa[p]])
                    T.op("act", lambda: nc.scalar.activation(out=pTa[p][:, 256:640], in_=P.banks[s1][:, 0:384], func=AF.Exp, scale=sc),
                         reads=[P.bb[s1]], writes=[b_pTa[p]])
                    T.op("pool", lambda: nc.gpsimd.tensor_tensor(out=pTa[p][:, 256:640], in0=pTa[p][:, 256:640], in1=msk[:, t, :], op=ALU.mult),
                         reads=[b_pTa[p], b_msk], writes=[b_pTa[p]])
                    for bi, w in enumerate(blocks):
                        first = (hh == 0 and bi == 0)
                        last = (hh == 1 and bi == 4)
                        T.op("pe", lambda: nc.tensor.matmul(P.banks[ob][:, 0:128], lhsT=vaw[:, kvh * 2 + hh, w, :], rhs=pTa[p][:, bi * 128:(bi + 1) * 128],
                                                            start=first, stop=last), reads=[b_vaw, b_pTa[p]], writes=[P.bb[ob]])
                        T.op("pe", lambda: nc.tensor.matmul(P.banks[sbk][:, 0:128], lhsT=oneslh[:, hh, :], rhs=pTa[p][:, bi * 128:(bi + 1) * 128],
                                                            start=first, stop=last), reads=[b_olh, b_pTa[p]], writes=[P.bb[sbk]])
                r = fi % 2
                fi += 1
                T.op("dve", lambda: nc.vector.tensor_scalar(out=den[r][:], in0=P.banks[sbk][:, 0:128], scalar1=esink[:, c:c + 1], scalar2=None, op0=ALU.add),
                     reads=[P.bb[sbk], b_es], writes=[b_den[r]])
                T.op("dve", lambda: nc.vector.reciprocal(out=den[r][:], in_=den[r][:]), reads=[b_den[r]], writes=[b_den[r]])
                T.op("dve", lambda: nc.vector.tensor_tensor(out=OT[:, c, t * 128:(t + 1) * 128], in0=P.banks[ob][:, 0:128], in1=den[r][:], op=ALU.mult),
                     reads=[P.bb[ob], b_den[r]], writes=[b_OT])
    T.barrier()
    if "nomB" in os.environ.get("K_DBG", ""):
        return
    phase_B_even_mixB(P, OT, b_OT, QTb_d, KTb_d, Vb_d, lamv_d, lami_d, subg_d)


def phase_B_even_mixB(P, OT, b_OT, QTb_d, KTb_d, Vb_d, lamv_d, lami_d, subg_d):
    nc, T = P.nc, P.T
    sc = 64.0 ** -0.5
    with ExitStack() as es:
        S = SweepCtx(P, es)
        ktbs = [P.sb(es, [128, NKB * 128], BF16, "ktb")] * 2
        vtbs = [P.sb(es, [128, NKB, 128], BF16, "vtb")] * 2
        b_kvs = [Buf("kvb")] * 2
        qtb = [P.sb(es, [128, 2, TOK], BF16, "qtb") for _ in range(2)]
        b_qtb = [Buf("qtb%d" % i) for i in range(2)]
        lamv = P.sb(es, [128, 4, 64], F32, "lamv")
        b_lamv = Buf("lamv")
        for i in range(4):
            T.dma("sp", lamv[:, i, :], lamv_d[i:i + 1, :].partition_broadcast(128), writes=[b_lamv])
        lami = P.sb(es, [128, 2], F32, "lami")
        b_lami = Buf("lami")
        T.dma("sp", lami[:], lami_d[:, :], writes=[b_lami])
        lp = P.sb(es, [128, 2, 64], F32, "lp")
        b_lp = Buf("lp")
        ls = P.sb(es, [128, 4], F32, "ls")
        b_ls = Buf("ls")
        T.op("dve", lambda: nc.vector.tensor_tensor(out=lp[:, 0, :], in0=lamv[:, 0, :], in1=lamv[:, 1, :], op=ALU.mult), reads=[b_lamv], writes=[b_lp])
        T.op("dve", lambda: nc.vector.tensor_tensor(out=lp[:, 1, :], in0=lamv[:, 2, :], in1=lamv[:, 3, :], op=ALU.mult), reads=[b_lamv, b_lp], writes=[b_lp])
        T.op("dve", lambda: nc.vector.tensor_reduce(out=ls[:, 0:2], in_=lp[:], axis=AX.X, op=ALU.add), reads=[b_lp], writes=[b_ls])
        T.op("act", lambda: nc.scalar.activation(out=ls[:, 0:2], in_=ls[:, 0:2], func=AF.Exp), reads=[b_ls], writes=[b_ls])
        T.op("dve", lambda: nc.vector.tensor_tensor(out=ls[:, 2:3], in0=ls[:, 1:2], in1=ls[:, 0:1], op=ALU.subtract), reads=[b_ls], writes=[b_ls])
        T.op("dve", lambda: nc.vector.tensor_tensor(out=ls[:, 2:3], in0=ls[:, 2:3], in1=lami[:, 0:1], op=ALU.subtract), reads=[b_ls, b_lami], writes=[b_ls])
        gsc = P.sb(es, [128, 1], F32, "gsc")
        b_gsc = Buf("gsc")
        T.dma("sp", gsc[:], subg_d[:, :], writes=[b_gsc])
        T.op("dve", lambda: nc.vector.tensor_tensor(out=gsc[:], in0=gsc[:], in1=lami[:, 1:2], op=ALU.mult), reads=[b_gsc, b_lami], writes=[b_gsc])
        epss = P.sb(es, [128, 1], F32, "epss")
        b_epss = Buf("epss")
        T.op("pool", lambda: nc.gpsimd.memset(epss[:], SUBLN_EPS), writes=[b_epss])
        rec = P.sb(es, [128, 512], F32, "recb")
        b_rec = Buf("recb")
        am = [P.sb(es, [128, 512], F32, "am") for _ in range(2)]
        b_am = [Buf("am%d" % i) for i in range(2)]
        dm = P.sb(es, [128, 512], F32, "dm")
        b_dm = Buf("dm")
        sq = P.sb(es, [128, 512], F32, "sqb")
        b_sq = Buf("sqb")
        rstd = P.sb(es, [128, 512], F32, "rstd")
        b_rstd = Buf("rstd")
        def load_head(h):
            T.dma("sp", ktbs[h % 2][:], KTb_d[h, :, :], writes=[b_kvs[h % 2]])
            T.dma("sp", vtbs[h % 2][:], Vb_d[h, :, :, :], writes=[b_kvs[h % 2]])
            T.dma("sp", qtb[h % 2][:], QTb_d[h, :, :, :], writes=[b_qtb[h % 2]])
        load_head(0)
        for h in range(4):
            ktb, vtb, b_kv = ktbs[h % 2], vtbs[h % 2], b_kvs[h % 2]
            qi = h % 2
            if h > 0:
                load_head(h)
            for (c0, N) in CHUNKS:
                blocks = ALL_BLOCKS if N == 512 else CTX_BLOCKS
                for mm in range(2):
                    ob, sbk = sweep(P, S, N, blocks, qtb[qi][:, mm, c0:c0 + N], b_qtb[qi],
                                    lambda b: ktb[:, b * 128:(b + 1) * 128], lambda b: vtb[:, b, :], b_kv, 128, sc)
                    T.op("dve", lambda: nc.vector.reciprocal(out=rec[:, 0:N], in_=P.banks[sbk][:, 0:N]), reads=[P.bb[sbk]], writes=[b_rec])
                    T.op("dve", lambda: nc.vector.tensor_tensor(out=am[mm][:, 0:N], in0=P.banks[ob][:, 0:N], in1=rec[:, 0:N], op=ALU.mult),
                         reads=[P.bb[ob], b_rec], writes=[b_am[mm]])
                T.op("dve", lambda: nc.vector.scalar_tensor_tensor(out=dm[:, 0:N], in0=am[1][:, 0:N], scalar=ls[:, 2:3], in1=am[0][:, 0:N],
                                                                   op0=ALU.mult, op1=ALU.add), reads=[b_am[0], b_am[1], b_ls], writes=[b_dm])
                T.op("act", lambda: nc.scalar.activation(out=sq[:, 0:N], in_=dm[:, 0:N], func=AF.Square), reads=[b_dm], writes=[b_sq])
                T.op("pe", lambda: nc.tensor.matmul(P.banks[sbk][:, 0:N], lhsT=P.onesd[:], rhs=sq[:, 0:N], start=True, stop=True),
                     reads=[P.b_onesd, b_sq], writes=[P.bb[sbk]])
                T.op("act", lambda: nc.scalar.activation(out=rstd[:, 0:N], in_=P.banks[sbk][:, 0:N], func=AF.Sqrt, bias=epss[:, 0:1], scale=1.0),
                     reads=[P.bb[sbk], b_epss], writes=[b_rstd])
                T.op("dve", lambda: nc.vector.reciprocal(out=rstd[:, 0:N], in_=rstd[:, 0:N]), reads=[b_rstd], writes=[b_rstd])
                T.op("dve", lambda: nc.vector.scalar_tensor_tensor(out=OT[:, 4 + h, c0:c0 + N], in0=dm[:, 0:N], scalar=gsc[:, 0:1], in1=rstd[:, 0:N],
                                                                   op0=ALU.mult, op1=ALU.mult), reads=[b_dm, b_gsc, b_rstd], writes=[b_OT])
    T.barrier()


def layer_norm_tile(P, L, src, b_src, dst, b_dst, gam, bet, b_gb):
    nc, T = P.nc, P.T
    st, b_st, mv, b_mv, xn, b_xn = L
    T.op("dve", lambda: nc.vector.bn_stats(out=st[:, 0, :], in_=src[:, 0:512]), reads=[b_src], writes=[b_st])
    T.op("dve", lambda: nc.vector.bn_stats(out=st[:, 1, :], in_=src[:, 512:1024]), reads=[b_src, b_st], writes=[b_st])
    T.op("dve", lambda: nc.vector.bn_aggr(out=mv[:, 0:2], in_=st[:].rearrange("p a s -> p (a s)")), reads=[b_st], writes=[b_mv])
    T.op("act", lambda: nc.scalar.activation(out=mv[:, 2:3], in_=mv[:, 1:2], func=AF.Sqrt, bias=mv[:, 3:4], scale=1.0), reads=[b_mv], writes=[b_mv])
    T.op("dve", lambda: nc.vector.reciprocal(out=mv[:, 2:3], in_=mv[:, 2:3]), reads=[b_mv], writes=[b_mv])
    T.op("dve", lambda: nc.vector.tensor_scalar(out=xn[:], in0=src, scalar1=mv[:, 0:1], scalar2=mv[:, 2:3], op0=ALU.subtract, op1=ALU.mult),
         reads=[b_src, b_mv], writes=[b_xn])
    T.op("pool", lambda: nc.gpsimd.tensor_tensor(out=xn[:], in0=xn[:], in1=gam, op=ALU.mult), reads=[b_xn, b_gb], writes=[b_xn])
    T.op("pool", lambda: nc.gpsimd.tensor_tensor(out=dst, in0=xn[:], in1=bet, op=ALU.add), reads=[b_xn, b_gb], writes=[b_dst])


def ln_scratch(P, es, tag):
    nc, T = P.nc, P.T
    st = P.sb(es, [128, 2, 6], F32, "lnst")
    mv = P.sb(es, [128, 4], F32, "lnmv")
    xn = P.sb(es, [128, D], F32, "lnxn")
    b_mv = Buf("lnmv" + tag)
    T.op("pool", lambda: nc.gpsimd.memset(mv[:, 3:4], LN_EPS), writes=[b_mv])
    return (st, Buf("lnst" + tag), mv, b_mv, xn, Buf("lnxn" + tag))


def phase_C1(P, OT, b_OT, x_d, wo_d, gbc_d, lng_d, lnb_d, x1_d, x1b):
    nc, T = P.nc, P.T
    with ExitStack() as es:
        wo, wob = load_weight_bf16(P, es, wo_d, 8, D, "wo")
        gb = P.sb(es, [128, 2, D], F32, "g1bc")
        b_gb = Buf("g1bc")
        for who in range(2):
            T.dma("sp", gb[:, who, :], gbc_d[who, 0:1, :].partition_broadcast(128), writes=[b_gb])
        ln = P.sb(es, [128, 2, D], F32, "ln1")
        b_ln = Buf("ln1")
        T.dma("sp", ln[:, 0, :], lng_d[0:1, :].partition_broadcast(128), writes=[b_ln])
        T.dma("sp", ln[:, 1, :], lnb_d[0:1, :].partition_broadcast(128), writes=[b_ln])
        L = ln_scratch(P, es, "1")
        xt = [P.sb(es, [128, D], F32, "cxt") for _ in range(2)]
        b_xt = [Buf("cxt%d" % i) for i in range(2)]
        rr = [P.sb(es, [128, D], F32, "crr") for _ in range(2)]
        b_rr = [Buf("crr%d" % i) for i in range(2)]
        x1t = [P.sb(es, [128, D], F32, "x1t") for _ in range(2)]
        b_x1t = [Buf("x1t%d" % i) for i in range(2)]
        for t in range(NT):
            i = t % 2
            who = 1 if t == NT - 1 else 0
            yb = (0, 1) if i == 0 else (2, 3)
            T.dma("sp", xt[i][:], x_d[t * 128:(t + 1) * 128, :], writes=[b_xt[i]])
            for half in range(2):
                for k in range(8):
                    T.op("pe", lambda: nc.tensor.matmul(P.banks[yb[half]][:, :], lhsT=OT[:, k, t * 128:(t + 1) * 128],
                                                        rhs=wo[:, k, half * 512:(half + 1) * 512], start=(k == 0), stop=(k == 7)),
                         reads=[b_OT, wob[half * 4]], writes=[P.bb[yb[half]]])
                T.op("dve", lambda: nc.vector.tensor_tensor(out=rr[i][:, half * 512:(half + 1) * 512], in0=P.banks[yb[half]][:, :],
                                                            in1=gb[:, who, half * 512:(half + 1) * 512], op=ALU.mult),
                     reads=[P.bb[yb[half]], b_gb], writes=[b_rr[i]])
            T.op("dve", lambda: nc.vector.scalar_tensor_tensor(out=rr[i][:], in0=xt[i][:], scalar=ALPHA, in1=rr[i][:], op0=ALU.mult, op1=ALU.add),
                 reads=[b_xt[i], b_rr[i]], writes=[b_rr[i]])
            layer_norm_tile(P, L, rr[i][:], b_rr[i], x1t[i][:], b_x1t[i], ln[:, 0, :], ln[:, 1, :], b_ln)
            T.dma("pool", x1_d[t * 128:(t + 1) * 128, :], x1t[i][:], reads=[b_x1t[i]], writes=[x1b[t]])
    T.barrier()


def phase_C2(P, x1_d, x1b, mC_d, gbc_d, lng_d, lnb_d, wr_d, br_d, wg_d, wu_d, wd_d, xo_d, xob):
    nc, T = P.nc, P.T
    with ExitStack() as es:
        acc = P.sb(es, [128, NT, D], F32, "acc")
        accb = [Buf("acc%d" % t) for t in range(NT)]
        h2T = P.sb(es, [128, 8, TOK], BF16, "h2T")
        h2b = [Buf("h2T%d" % c) for c in range(len(CHUNKS))]
        m, b_m = load_modT(P, es, mC_d, "mC")
        gb = P.sb(es, [128, 2, D], F32, "g2bc")
        b_gb = Buf("g2bc")
        for who in range(2):
            T.dma("sp", gb[:, who, :], gbc_d[who, 1:2, :].partition_broadcast(128), writes=[b_gb])
        wr = P.sb(es, [128, 8, NEXP], F32, "wr")
        b_wr = Buf("wr")
        T.dma("sp", wr[:], wr_d.rearrange("(k p) e -> p k e", p=128), writes=[b_wr])
        brt = P.sb(es, [128, NEXP], F32, "brt")
        b_brt = Buf("brt")
        T.dma("sp", brt[:], br_d[0:1, :].partition_broadcast(128), writes=[b_brt])
        scs = P.sb(es, [128, NT, NEXP], F32, "scs")
        b_scs = Buf("scs")
        gates = P.sb(es, [128, NT, NEXP], F32, "gates")
        b_gates = Buf("gates")
        with ExitStack() as es2:
            hTf = [P.sb(es2, [128, 8, 128], F32, "hTf") for _ in range(2)]
            b_hTf = [Buf("hTf%d" % i) for i in range(2)]
            for t in range(int(os.environ.get("K_NT", NT))):
                i = t % 2
                who = 1 if t == NT - 1 else 0
                ch = min(t // 4, 4)
                T.dma("sp", acc[:, t, :], x1_d[t * 128:(t + 1) * 128, :], reads=[x1b[t]], writes=[accb[t]])
                transpose_mod(P, acc[:, t, :], accb[t], m[:, who, 0, :], m[:, who, 1, :], b_m, h2T[:, :, t * 128:(t + 1) * 128], h2b[ch],
                              (0, 1) if i == 0 else (2, 3), None if "nohtf" in os.environ.get("K_DBG", "") else hTf[i], b_hTf[i])
                lb = 4 + i
                DBG = os.environ.get("K_DBG", "")
                if "nologit" not in DBG:
                    for k in range(8):
                        T.op("pe", lambda: nc.tensor.matmul(P.banks[lb][:, 0:NEXP], lhsT=hTf[i][:, k, :], rhs=wr[:, k, :], start=(k == 0), stop=(k == 7)),
                             reads=[b_hTf[i], b_wr], writes=[P.bb[lb]])
                    if "nosig" not in DBG:
                        T.op("act", lambda: nc.scalar.activation(out=scs[:, t, :], in_=P.banks[lb][:, 0:NEXP], func=AF.Sigmoid), reads=[P.bb[lb]], writes=[b_scs])
                if "nopool" not in DBG:
                    T.op("pool", lambda: nc.gpsimd.tensor_scalar(out=acc[:, t, :], in0=acc[:, t, :], scalar1=ALPHA, scalar2=None, op0=ALU.mult),
                         reads=[accb[t]], writes=[accb[t]])
            if "noroute" in os.environ.get("K_DBG", ""):
                return
            G = NT * 4
            sel = P.sb(es2, [128, NT, NEXP], F32, "sel")
            sel2 = P.sb(es2, [128, NT, NEXP], F32, "sel2")
            eq = P.sb(es2, [128, NT, NEXP], F32, "eq")
            m1 = P.sb(es2, [128, G], F32, "m1")
            m2 = P.sb(es2, [128, G], F32, "m2")
            gs = P.sb(es2, [128, G], F32, "gs")
            gmx = P.sb(es2, [128, NT], F32, "gmx")
            b_r = Buf("route")
            g4 = lambda a: a[:].rearrange("p t (g j) -> p (t g) j", j=4)
            bc4 = lambda a: a[:].unsqueeze(2).to_broadcast([128, G, 4])
            R = dict(reads=[b_r, b_scs, b_brt], writes=[b_r])
            T.op("dve", lambda: nc.vector.tensor_tensor(out=sel[:], in0=scs[:], in1=brt[:].unsqueeze(1).to_broadcast([128, NT, NEXP]), op=ALU.add), **R)
            T.op("dve", lambda: nc.vector.tensor_reduce(out=m1[:], in_=g4(sel), axis=AX.X, op=ALU.max), **R)
            T.op("dve", lambda: nc.vector.tensor_tensor(out=g4(eq), in0=g4(sel), in1=bc4(m1), op=ALU.is_equal), **R)
            T.op("dve", lambda: nc.vector.scalar_tensor_tensor(out=sel2[:], in0=eq[:], scalar=-1.0e9, in1=sel[:], op0=ALU.mult, op1=ALU.add), **R)
            T.op("dve", lambda: nc.vector.tensor_reduce(out=m2[:], in_=g4(sel2), axis=AX.X, op=ALU.max), **R)
            T.op("dve", lambda: nc.vector.tensor_tensor(out=gs[:], in0=m1[:], in1=m2[:], op=ALU.add), **R)
            T.op("dve", lambda: nc.vector.tensor_reduce(out=gmx[:], in_=gs[:].rearrange("p (t g) -> p t g", g=4), axis=AX.X, op=ALU.max), **R)
            T.op("dve", lambda: nc.vector.tensor_tensor(out=gs[:].rearrange("p (t g) -> p t g", g=4), in0=gs[:].rearrange("p (t g) -> p t g", g=4),
                                                        in1=gmx[:].unsqueeze(2).to_broadcast([128, NT, 4]), op=ALU.is_equal), **R)
            T.op("dve", lambda: nc.vector.tensor_tensor(out=g4(eq), in0=g4(sel), in1=bc4(m2), op=ALU.is_ge), **R)
            T.op("dve", lambda: nc.vector.tensor_tensor(out=g4(eq), in0=g4(eq), in1=bc4(gs), op=ALU.mult), **R)
            T.op("dve", lambda: nc.vector.tensor_tensor(out=sel[:], in0=scs[:], in1=eq[:], op=ALU.mult), **R)
            T.op("dve", lambda: nc.vector.tensor_reduce(out=gmx[:], in_=sel[:], axis=AX.X, op=ALU.add), **R)
            T.op("dve", lambda: nc.vector.reciprocal(out=gmx[:], in_=gmx[:]), **R)
            T.op("dve", lambda: nc.vector.tensor_tensor(out=gates[:], in0=sel[:], in1=gmx[:].unsqueeze(2).to_broadcast([128, NT, NEXP]), op=ALU.mult),
                 reads=[b_r], writes=[b_gates])
        T.barrier()
        DBG = os.environ.get("K_DBG", "")
        if "noexp" in DBG:
            return
        with ExitStack() as es3:
            wg = [P.sb(es3, [128, 8, DEXP], BF16, "wg") for _ in range(2)]
            wu = [P.sb(es3, [128, 8, DEXP], BF16, "wu") for _ in range(2)]
            wd = [P.sb(es3, [128, 4, D], BF16, "wd") for _ in range(2)]
            wbuf = [[[Buf("w%d_%d_%d" % (s_, m_, h_)) for h_ in range(2)] for m_ in range(3)] for s_ in range(2)]
            NSTG = 2
            stg = [P.sb(es3, [128, 2048], F32, "wstg") for _ in range(NSTG)]
            b_stg = [Buf("wstg%d" % i) for i in range(NSTG)]
            sg = [P.sb(es3, [128, 512], F32, "sg") for _ in range(2)]
            b_sg = [Buf("sg%d" % i) for i in range(2)]
            aT = [P.sb(es3, [128, 4, 512], BF16, "aT") for _ in range(2)]
            b_aT = [Buf("aT%d" % i) for i in range(2)]
            tmp = [P.sb(es3, [128, D], F32, "mtmp") for _ in range(2)]
            b_tmp = [Buf("mtmp%d" % i) for i in range(2)]
            si = 0
            ci = 0
            fi = 0
            ti = 0
            for e in range(NEXP if "exp1" not in DBG else 1):
                s_ = e % 2
                for m_, (src, dst, K) in enumerate(((wg_d, wg[s_], 8), (wu_d, wu[s_], 8), (wd_d, wd[s_], 4))):
                    for h_ in range(2):
                        st = stg[si % NSTG]
                        bs = b_stg[si % NSTG]
                        si += 1
                        kh = K // 2
                        ncol = 2048 // kh
                        T.dma("sp", st[:].rearrange("p (k n) -> p k n", k=kh),
                              src[e, h_ * kh * 128:(h_ + 1) * kh * 128, :].rearrange("(k p) n -> p k n", p=128), writes=[bs])
                        T.op("act", lambda: nc.scalar.copy(out=dst[:, h_ * kh:(h_ + 1) * kh, :], in_=st[:].rearrange("p (k n) -> p k n", k=kh)),
                             reads=[bs], writes=[wbuf[s_][m_][h_]])
                for chn, (c0, N) in enumerate(CHUNKS):
                    a = ci % 2
                    ci += 1
                    for f in range(4):
                        gbk = fi % 2
                        ubk = 2 + fi % 2
                        fi += 1
                        for k in range(8):
                            T.op("pe", lambda: nc.tensor.matmul(P.banks[gbk][:, 0:N], lhsT=wg[s_][:, k, f * 128:(f + 1) * 128], rhs=h2T[:, k, c0:c0 + N],
                                                                start=(k == 0), stop=(k == 7)), reads=[wbuf[s_][0][k // 4], h2b[chn]], writes=[P.bb[gbk]])
                        for k in range(8):
                            T.op("pe", lambda: nc.tensor.matmul(P.banks[ubk][:, 0:N], lhsT=wu[s_][:, k, f * 128:(f + 1) * 128], rhs=h2T[:, k, c0:c0 + N],
                                                                start=(k == 0), stop=(k == 7)), reads=[wbuf[s_][1][k // 4], h2b[chn]], writes=[P.bb[ubk]])
                        T.op("act", lambda: nc.scalar.activation(out=sg[gbk][:, 0:N], in_=P.banks[gbk][:, 0:N], func=AF.Silu),
                             reads=[P.bb[gbk]], writes=[b_sg[gbk]])
                        T.op("dve", lambda: nc.vector.tensor_tensor(out=aT[a][:, f, 0:N], in0=sg[gbk][:, 0:N], in1=P.banks[ubk][:, 0:N], op=ALU.mult),
                             reads=[b_sg[gbk], P.bb[ubk]], writes=[b_aT[a]])
                    for tt in range(N // 128):
                        t = c0 // 128 + tt
                        who = 1 if t == NT - 1 else 0
                        yb = (4, 5) if ti % 2 == 0 else (6, 7)
                        tm = ti % 2
                        ti += 1
                        for half in range(2):
                            for f in range(4):
                                T.op("pe", lambda: nc.tensor.matmul(P.banks[yb[half]][:, :], lhsT=aT[a][:, f, tt * 128:(tt + 1) * 128],
                                                                    rhs=wd[s_][:, f, half * 512:(half + 1) * 512], start=(f == 0), stop=(f == 3)),
                                     reads=[b_aT[a], wbuf[s_][2][f // 2]], writes=[P.bb[yb[half]]])
                            T.op("dve", lambda: nc.vector.scalar_tensor_tensor(out=tmp[tm][:, half * 512:(half + 1) * 512], in0=P.banks[yb[half]][:, :],
                                                                               scalar=gates[:, t, e:e + 1], in1=gb[:, who, half * 512:(half + 1) * 512],
                                                                               op0=ALU.mult, op1=ALU.mult),
                                 reads=[P.bb[yb[half]], b_gates, b_gb], writes=[b_tmp[tm]])
                        T.op("pool", lambda: nc.gpsimd.tensor_tensor(out=acc[:, t, :], in0=acc[:, t, :], in1=tmp[tm][:], op=ALU.add),
                             reads=[accb[t], b_tmp[tm]], writes=[accb[t]])
        T.barrier()
        with ExitStack() as es4:
            L = ln_scratch(P, es4, "2")
            ln = P.sb(es4, [128, 2, D], F32, "ln2")
            b_ln = Buf("ln2")
            T.dma("sp", ln[:, 0, :], lng_d[1:2, :].partition_broadcast(128), writes=[b_ln])
            T.dma("sp", ln[:, 1, :], lnb_d[1:2, :].partition_broadcast(128), writes=[b_ln])
            xo = [P.sb(es4, [128, D], F32, "xo") for _ in range(2)]
            b_xo = [Buf("xo%d" % i) for i in range(2)]
            for t in range(NT):
                i = t % 2
                layer_norm_tile(P, L, acc[:, t, :], accb[t], xo[i][:], b_xo[i], ln[:, 0, :], ln[:, 1, :], b_ln)
                T.dma("pool", xo_d[t * 128:(t + 1) * 128, :], xo[i][:], reads=[b_xo[i]], writes=[xob[t]])
    T.barrier()


def rope_tables(hd):
    q = hd // 4
    inv = (10000.0 ** (-np.arange(q, dtype=np.float32) / np.float32(q))).astype(np.float32)
    tpos = np.arange(SEQ)
    ang_r = (tpos // 64).astype(np.float32)[:, None] * inv[None, :]
    ang_c = (tpos % 64).astype(np.float32)[:, None] * inv[None, :]
    cos = np.concatenate([np.cos(ang_r), np.cos(ang_c)], axis=1).astype(np.float32)
    sin = np.concatenate([np.sin(ang_r), np.sin(ang_c)], axis=1).astype(np.float32)
    cos_c = np.ones((NCORES, TOK, 2 * q), np.float32)
    sin_c = np.zeros((NCORES, TOK, 2 * q), np.float32)
    for r in range(NCORES):
        cos_c[r, :2048] = cos[r * 2048:(r + 1) * 2048]
        sin_c[r, :2048] = sin[r * 2048:(r + 1) * 2048]
    return cos_c, sin_c


IDENT = np.eye(128, dtype=np.float32)
_PROG_CACHE = {}


def run(nc, in_maps):
    return run_bass_kernel_spmd(nc, in_maps, core_ids=list(range(NCORES))).results


def modT_layout(modv, l, cols):
    out = np.empty((128, 2, len(cols), 8), np.float32)
    for who in range(2):
        for j, c in enumerate(cols):
            out[:, who, j, :] = modv[l, who, c * 1024:(c + 1) * 1024].reshape(8, 128).T
    return out


def build_mod():
    if "mod" not in _PROG_CACHE:
        P = Prog()
        cc = P.inp("cc", [128, 8, 2], F32)
        wm = P.inp("wm", [DEPTH, D, 768], F32)
        bm = P.inp("bm", [DEPTH, 2, 768], F32)
        out = P.outp("modp", [DEPTH, 2, 768], F32)
        phase_mod(P, cc, wm, bm, out, 768)
        _PROG_CACHE["mod"] = P.close()
    return _PROG_CACHE["mod"]


def run_mod(c, c_ctx, w_mod, b_mod):
    nc = build_mod()
    cc = np.stack([c.reshape(128, 8), c_ctx.reshape(128, 8)], axis=-1).astype(np.float32)
    maps = []
    for r in range(NCORES):
        sl = slice(r * 768, (r + 1) * 768)
        maps.append({"ident": IDENT, "cc": cc, "wm": np.ascontiguousarray(w_mod[:, :, sl]),
                     "bm": np.ascontiguousarray(np.repeat(b_mod[:, None, sl], 2, axis=1))})
    res = run(nc, maps)
    return np.concatenate([res[r]["modp"] for r in range(NCORES)], axis=2)


def build_A(even):
    key = "A%d" % even
    if key not in _PROG_CACHE:
        P = Prog()
        ncols = E_COLS if even else O_COLS
        hq = 32 if even else 64
        x = P.inp("x", [TOK, D], F32)
        w = P.inp("w_in", [D, ncols], F32)
        mA = P.inp("mA", [128, 2, 2, 8], F32)
        cos = P.inp("cos", [TOK, hq], F32)
        sin = P.inp("sin", [TOK, hq], F32)
        qng = None if even else P.inp("qng", [2, 128], F32)
        qkv = P.outp("qkv", [TOK, ncols], BF16)
        phase_A(P, even, x, None, w, mA, cos, sin, qng, qkv)
        _PROG_CACHE[key] = P.close()
    return _PROG_CACHE[key]


def gather_tokens(parts, c0, c1):
    ctx = np.concatenate([parts[0][2048:, c0:c1], parts[1][2048:, c0:c1]], axis=0)
    lat = np.concatenate([p[:2048, c0:c1] for p in parts], axis=0)
    return np.concatenate([ctx, lat], axis=0)


def layout_odd(qkv):
    k_all = gather_tokens(qkv, 1024, 1280)
    v_all = gather_tokens(qkv, 1280, 1536)
    KT = np.ascontiguousarray(k_all.reshape(NKB * 128, 2, 128).transpose(1, 2, 0))
    V = np.ascontiguousarray(v_all.reshape(NKB, 128, 2, 128).transpose(2, 1, 0, 3))
    maps = []
    for r in range(NCORES):
        QT = np.ascontiguousarray(qkv[r][:, 0:1024].reshape(TOK, 8, 128).transpose(1, 2, 0))
        maps.append({"QT": QT, "KT": KT, "V": V})
    return maps


def window_masks():
    m = np.zeros((NCORES, 128, NT, 384), np.float32)
    jj = np.arange(128)[:, None]
    ii = np.arange(128)[None, :]
    left = (ii <= jj).astype(np.float32)
    right = (jj <= ii).astype(np.float32)
    for r in range(NCORES):
        for t in range(16):
            n = 16 * r + t
            if n - 1 >= 0:
                m[r, :, t, 0:128] = left
            m[r, :, t, 128:256] = 1.0
            if n + 1 < SEQ // 128:
                m[r, :, t, 256:384] = right
    return m.astype(NPBF)


def layout_even(qkv, sink, lam4, lam_init, subln_g):
    ak = gather_tokens(qkv, 512, 640)
    av = gather_tokens(qkv, 640, 768)
    bk = gather_tokens(qkv, 1280, 1792)
    bv = gather_tokens(qkv, 1792, 2304)
    KTb = np.ascontiguousarray(bk.reshape(NKB * 128, 4, 128).transpose(1, 2, 0))
    Vb = np.ascontiguousarray(bv.reshape(NKB, 128, 4, 128).transpose(2, 1, 0, 3))
    masks = window_masks()
    sinkp = np.empty((128, 4), np.float32)
    for c in range(4):
        sinkp[:64, c] = sink[2 * c]
        sinkp[64:, c] = sink[2 * c + 1]
    lami = np.empty((128, 2), np.float32)
    lami[:, 0] = lam_init
    lami[:, 1] = 1.0 - lam_init
    akb = ak.reshape(NKB, 128, 128)
    avb = av.reshape(NKB, 128, 2, 64)
    maps = []
    for r in range(NCORES):
        q = qkv[r]
        qb = q[:, 768:1280].reshape(TOK, 4, 2, 64).transpose(1, 2, 3, 0)
        QTb = np.zeros((4, 128, 2, TOK), q.dtype)
        QTb[:, 0:64, 0, :] = qb[:, 0]
        QTb[:, 64:128, 1, :] = qb[:, 1]
        QTa = np.ascontiguousarray(q[:, 0:512].reshape(TOK, 2, 4, 64).transpose(1, 3, 2, 0).reshape(128, 4, TOK))
        kw = np.zeros((20, 128, 128), ak.dtype)
        vw = np.zeros((20, 128, 2, 64), av.dtype)
        kw[0:2] = akb[0:2]
        vw[0:2] = avb[0:2]
        for w in range(2, 20):
            n = 16 * r - 1 + (w - 2)
            if 0 <= n < SEQ // 128:
                kw[w] = akb[2 + n]
                vw[w] = avb[2 + n]
        KTa = np.ascontiguousarray(kw.transpose(2, 0, 1).reshape(128, 20 * 128))
        Va = np.zeros((128, 4, 20, 128), av.dtype)
        for kvh in range(2):
            for lohi in range(2):
                Va[:, kvh * 2 + lohi, :, lohi * 64:lohi * 64 + 64] = vw[:, :, kvh, :].transpose(1, 0, 2)
        maps.append({"QTa": QTa, "KTa": KTa, "Va": Va, "mask": masks[r], "sinkp": sinkp, "QTb": QTb, "KTb": KTb, "Vb": Vb,
                     "lamv": np.ascontiguousarray(lam4.astype(np.float32)), "lami": lami,
                     "subg": np.ascontiguousarray(subln_g.reshape(128, 1).astype(np.float32))})
    return maps


def decl_B(P, even):
    if even:
        return dict(QTa=P.inp("QTa", [128, 4, TOK], BF16), KTa=P.inp("KTa", [128, 20 * 128], BF16), Va=P.inp("Va", [128, 4, 20, 128], BF16),
                    mask=P.inp("mask", [128, NT, 384], BF16), sinkp=P.inp("sinkp", [128, 4], F32), QTb=P.inp("QTb", [4, 128, 2, TOK], BF16),
                    KTb=P.inp("KTb", [4, 128, NKB * 128], BF16), Vb=P.inp("Vb", [4, 128, NKB, 128], BF16), lamv=P.inp("lamv", [4, 64], F32),
                    lami=P.inp("lami", [128, 2], F32), subg=P.inp("subg", [128, 1], F32))
    return dict(QT=P.inp("QT", [8, 128, TOK], BF16), KT=P.inp("KT", [2, 128, NKB * 128], BF16), V=P.inp("V", [2, 128, NKB, 128], BF16))


def emit_B(P, even, OT, b_OT, d):
    if even:
        phase_B_even(P, OT, b_OT, d["QTa"], d["KTa"], d["Va"], d["mask"], d["sinkp"], d["QTb"], d["KTb"], d["Vb"], d["lamv"], d["lami"], d["subg"])
    else:
        phase_B_odd(P, OT, b_OT, d["QT"], d["KT"], d["V"])


def build_Btest(even):
    key = "Bt%d" % even
    if key not in _PROG_CACHE:
        P = Prog()
        d = decl_B(P, even)
        out = P.outp("OT", [128, 8, TOK], BF16)
        OT = P.sb(P.es, [128, 8, TOK], BF16, "OT")
        b_OT = Buf("OT")
        emit_B(P, even, OT, b_OT, d)
        P.T.dma("pool", out[:, :, :], OT[:], reads=[b_OT])
        _PROG_CACHE[key] = P.close()
    return _PROG_CACHE[key]


def build_BCA(even, has_A):
    key = "BCA%d%d" % (even, has_A)
    if key in _PROG_CACHE:
        return _PROG_CACHE[key]
    P = Prog()
    nc = P.nc
    dB = decl_B(P, even)
    x = P.inp("x", [TOK, D], F32)
    wo = P.inp("w_out", [D, D], F32)
    gbc = P.inp("gbc", [2, 2, D], F32)
    mC = P.inp("mC", [128, 2, 2, 8], F32)
    lng = P.inp("lng", [2, D], F32)
    lnb = P.inp("lnb", [2, D], F32)
    wr = P.inp("wr", [D, NEXP], F32)
    br = P.inp("br", [1, NEXP], F32)
    wg = P.inp("wg", [NEXP, D, DEXP], F32)
    wu = P.inp("wu", [NEXP, D, DEXP], F32)
    wd = P.inp("wd", [NEXP, DEXP, D], F32)
    x1s = nc.dram_tensor("x1s", [TOK, D], F32, kind="Internal").ap()
    xo = P.outp("xo", [TOK, D], F32)
    x1b = [Buf("x1s%d" % t) for t in range(NT)]
    xob = [Buf("xo%d" % t) for t in range(NT)]
    if has_A:
        a_even = not even
        ncols = E_COLS if a_even else O_COLS
        hq = 32 if a_even else 64
        w_in = P.inp("w_in", [D, ncols], F32)
        mA = P.inp("mA", [128, 2, 2, 8], F32)
        cos = P.inp("cos", [TOK, hq], F32)
        sin = P.inp("sin", [TOK, hq], F32)
        qng = None if a_even else P.inp("qng", [2, 128], F32)
        qkv = P.outp("qkv", [TOK, ncols], BF16)
    with ExitStack() as es:
        OT = P.sb(es, [128, 8, TOK], BF16, "OT")
        b_OT = Buf("OT")
        emit_B(P, even, OT, b_OT, dB)
        phase_C1(P, OT, b_OT, x, wo, gbc, lng, lnb, x1s, x1b)
    if "noC2" not in os.environ.get("K_DBG", ""):
        phase_C2(P, x1s, x1b, mC, gbc, lng, lnb, wr, br, wg, wu, wd, xo, xob)
    if has_A and "noA" not in os.environ.get("K_DBG", ""):
        phase_A(P, a_even, xo, xob, w_in, mA, cos, sin, qng, qkv)
    print("BCA program: %d instructions, %d waits, %d dma sems" % (P.T.n_ins, P.T.n_wait, P.T.nsem), flush=True)
    _PROG_CACHE[key] = P.close()
    return _PROG_CACHE[key]


def a_inputs(inputs, modv, l, r, tabs):
    even = (l % 2 == 0)
    i = l // 2
    cos_c, sin_c = tabs[64 if even else 128]
    m = {"w_in": inputs["w_in_even"][i] if even else inputs["w_in_odd"][i], "mA": modT_layout(modv, l, [1, 0]),
         "cos": cos_c[r], "sin": sin_c[r]}
    if not even:
        m["qng"] = np.stack([inputs["q_norm_g"][i], inputs["k_norm_g"][i]]).astype(np.float32)
    return m


def kernel(**inputs):
    inputs = {k: np.asarray(v) for k, v in inputs.items()}
    x = inputs["x"][0]
    ctx = inputs["ctx"][0]
    tabs = {64: rope_tables(64), 128: rope_tables(128)}
    modv = run_mod(inputs["c"][0], inputs["c_ctx"], inputs["w_mod"], inputs["b_mod"])
    xres = [np.ascontiguousarray(np.concatenate([x[r * 2048:(r + 1) * 2048], ctx[(r % 2) * 128:(r % 2) * 128 + 128]], 0)) for r in range(NCORES)]
    maps = []
    for r in range(NCORES):
        m = a_inputs(inputs, modv, 0, r, tabs)
        m.update({"ident": IDENT, "x": xres[r]})
        maps.append(m)
    res = run(build_A(True), maps)
    qkv = [res[r]["qkv"] for r in range(NCORES)]
    for l in range(DEPTH):
        even = (l % 2 == 0)
        i = l // 2
        has_A = l < DEPTH - 1
        if even:
            lam_init = 0.8 - 0.6 * float(np.exp(-0.3 * l))
            lam4 = np.stack([inputs["lam_q1"][i], inputs["lam_k1"][i], inputs["lam_q2"][i], inputs["lam_k2"][i]])
            maps = layout_even(qkv, inputs["sink_logits"][i], lam4, lam_init, inputs["subln_g"][i])
        else:
            maps = layout_odd(qkv)
        gbc = np.ascontiguousarray(np.stack([np.stack([modv[l, who, 2048:3072], modv[l, who, 5120:6144]]) for who in range(2)]))
        mC = modT_layout(modv, l, [4, 3])
        for r in range(NCORES):
            m = maps[r]
            m.update({"ident": IDENT, "x": xres[r], "w_out": inputs["w_out_even"][i] if even else inputs["w_out_odd"][i], "gbc": gbc, "mC": mC,
                      "lng": inputs["ln_g"][l], "lnb": inputs["ln_b"][l], "wr": inputs["w_router"], "br": inputs["b_router"].reshape(1, NEXP),
                      "wg": inputs["w_gate"][l], "wu": inputs["w_up"][l], "wd": inputs["w_down"][l]})
            if has_A:
                m.update(a_inputs(inputs, modv, l + 1, r, tabs))
        res = run(build_BCA(even, has_A), maps)
        xres = [res[r]["xo"] for r in range(NCORES)]
        if has_A:
            qkv = [res[r]["qkv"] for r in range(NCORES)]
    out = np.concatenate([xres[r][:2048] for r in range(NCORES)], axis=0)
    return out.reshape(1, SEQ, D).astype(np.float32)
```

```python
import os
import numpy as np
import ml_dtypes
from contextlib import ExitStack
import concourse.bass as bass
import concourse.mybir as mybir
from concourse.bass_utils import run_bass_kernel_spmd

F32 = mybir.dt.float32
BF16 = mybir.dt.bfloat16
AF = mybir.ActivationFunctionType
ALU = mybir.AluOpType
AX = mybir.AxisListType
NPBF = ml_dtypes.bfloat16

NCORES = 8
D = 1024
SEQ = 16384
CTX = 256
NT = 17
TOK = NT * 128
NKB = (SEQ + CTX) // 128
DEPTH = 4
ALPHA = (2 * DEPTH) ** 0.25
LN_EPS = 1e-5
QK_EPS = 1e-6
SUBLN_EPS = 1e-5
E_COLS = 2304
O_COLS = 1536
NEXP = 16
DEXP = 512
CHUNKS = [(0, 512), (512, 512), (1024, 512), (1536, 512), (2048, 128)]


class Buf:
    __slots__ = ("name", "w", "rs", "dsem", "dcnt", "excl")

    def __init__(self, name, excl=False):
        self.name = name
        self.w = None
        self.rs = {}
        self.dsem = None
        self.dcnt = 0
        self.excl = excl


class Trk:
    def __init__(self, nc, es):
        self.nc = nc
        self.es = es
        self.eng = {"pe": nc.tensor, "act": nc.scalar, "dve": nc.vector, "pool": nc.gpsimd, "sp": nc.sync}
        self.sem = {}
        self.cnt = {}
        self.waited = {k: {} for k in self.eng}
        self.nsem = 0
        self.dbufs = []
        for k in self.eng:
            self.sem[k] = es.enter_context(nc.semaphore("e_" + k))
            self.cnt[k] = 0
        self.n_ins = 0
        self.n_wait = 0

    def _deps(self, reads, writes, e=None):
        deps = {}
        own = self.sem.get(e)

        def add(t):
            if t is None:
                return
            k = id(t[0])
            if k not in deps or deps[k][1] < t[1]:
                deps[k] = t
        for b in reads:
            add(b.w)
            if b.excl:
                for t in b.rs.values():
                    if t[0] is not own:
                        add(t)
        for b in writes:
            add(b.w)
            for t in b.rs.values():
                add(t)
        return deps

    def _emit_waits(self, e, deps):
        own = self.sem.get(e)
        eo = self.eng[e]
        wd = self.waited[e]
        for k, (s, v) in deps.items():
            if s is own and e in ("pe", "sp"):
                continue
            if wd.get(k, 0) >= v:
                continue
            eo.wait_ge(s, v)
            wd[k] = v
            self.n_wait += 1

    def _commit(self, t, reads, writes):
        k = id(t[0])
        for b in reads:
            b.rs[k] = t
        for b in writes:
            b.w = t
            b.rs = {}

    def op(self, e, fn, reads=(), writes=()):
        self._emit_waits(e, self._deps(reads, writes, e))
        ins = fn()
        self.cnt[e] += 1
        ins.then_inc(self.sem[e], 1)
        self._commit((self.sem[e], self.cnt[e]), reads, writes)
        self.n_ins += 1
        return ins

    def dma(self, q, out, in_, reads=(), writes=(), **kw):
        self._emit_waits(q, self._deps(reads, writes, q))
        owner = writes[0] if len(writes) else reads[0]
        if owner.dsem is None:
            owner.dsem = self.es.enter_context(self.nc.semaphore("d_%d" % self.nsem))
            self.nsem += 1
            self.dbufs.append(owner)
        ins = self.eng[q].dma_start(out=out, in_=in_, **kw)
        owner.dcnt += 16
        ins.then_inc(owner.dsem, 16)
        self._commit((owner.dsem, owner.dcnt), reads, writes)
        self.n_ins += 1
        return ins

    def barrier(self):
        deps = {}
        for k in self.eng:
            if self.cnt[k]:
                deps[id(self.sem[k])] = (self.sem[k], self.cnt[k])
        for b in self.dbufs:
            deps[id(b.dsem)] = (b.dsem, b.dcnt)
        for e in self.eng:
            own = self.sem[e]
            eo = self.eng[e]
            wd = self.waited[e]
            for k, (s, v) in deps.items():
                if s is own or wd.get(k, 0) >= v:
                    continue
                eo.wait_ge(s, v)
                wd[k] = v

    def finish(self, e="pool"):
        deps = {}
        for b in self.dbufs:
            deps[id(b.dsem)] = (b.dsem, b.dcnt)
        for k in self.eng:
            if self.cnt[k]:
                deps[id(self.sem[k])] = (self.sem[k], self.cnt[k])
        self._emit_waits(e, deps)


class Prog:
    def __init__(self):
        self.nc = bass.Bass("TRN2", target_bir_lowering=False)
        self.es = ExitStack()
        self.T = Trk(self.nc, self.es)
        self.pbig = [self.es.enter_context(self.nc.psum_tensor("pbig%d" % i, [128, 1024], F32)) for i in range(4)]
        self.banks = [self.pbig[i // 2][:, (i % 2) * 512:(i % 2 + 1) * 512] for i in range(8)]
        self.bb = [Buf("bank%d" % i, excl=True) for i in range(8)]
        self.uid = 0
        nc, T = self.nc, self.T
        self.ident_d = self.inp("ident", [128, 128], F32)
        self.ident = self.sb(self.es, [128, 128], F32)
        self.b_ident = Buf("ident")
        T.dma("sp", self.ident[:], self.ident_d[:, :], writes=[self.b_ident])
        self.ones = self.sb(self.es, [128, 128], BF16)
        self.b_ones = Buf("ones")
        T.op("pool", lambda: nc.gpsimd.memset(self.ones[:], 1.0), writes=[self.b_ones])
        self.onesd = self.sb(self.es, [128, 128], F32)
        self.b_onesd = Buf("onesd")
        T.op("pool", lambda: nc.gpsimd.memset(self.onesd[:], 1.0 / 128.0), writes=[self.b_onesd])

    def inp(self, name, shape, dt):
        return self.nc.dram_tensor(name, list(shape), dt, kind="ExternalInput").ap()

    def outp(self, name, shape, dt):
        return self.nc.dram_tensor(name, list(shape), dt, kind="ExternalOutput").ap()

    def sb(self, es, shape, dt, name=None):
        self.uid += 1
        return es.enter_context(self.nc.sbuf_tensor("%s_%d" % (name or "t", self.uid), list(shape), dt))

    def close(self):
        self.T.finish("pool")
        self.es.close()
        return self.nc


def bcast_rows(ap_row, n):
    return ap_row.partition_broadcast(n)


def phase_mod(P, cc_d, wm_d, bm_d, out_d, ncols):
    nc, T = P.nc, P.T
    with ExitStack() as es:
        cc = P.sb(es, [128, 8, 2], F32)
        b_cc = Buf("cc")
        T.dma("sp", cc[:], cc_d[:, :, :], writes=[b_cc])
        ccs = P.sb(es, [128, 8, 2], F32)
        b_ccs = Buf("ccs")
        T.op("act", lambda: nc.scalar.activation(out=ccs[:], in_=cc[:], func=AF.Silu), reads=[b_cc], writes=[b_ccs])
        CW = 384
        stg = [P.sb(es, [128, 8, CW], F32) for _ in range(2)]
        b_stg = [Buf("stg%d" % i) for i in range(2)]
        bt = [P.sb(es, [2, CW], F32) for _ in range(2)]
        b_bt = [Buf("bt%d" % i) for i in range(2)]
        rs = [P.sb(es, [2, CW], F32) for _ in range(2)]
        b_rs = [Buf("rs%d" % i) for i in range(2)]
        it = 0
        for l in range(DEPTH):
            for c0 in range(0, ncols, CW):
                i = it % 2
                it += 1
                T.dma("sp", stg[i][:], wm_d[l, :, c0:c0 + CW].rearrange("(p k) n -> p k n", k=8), writes=[b_stg[i]])
                T.dma("sp", bt[i][:], bm_d[l, :, c0:c0 + CW], writes=[b_bt[i]])
                ps = P.banks[i]
                for k in range(8):
                    T.op("pe", lambda: nc.tensor.matmul(ps[0:2, 0:CW], lhsT=ccs[:, k, :], rhs=stg[i][:, k, :], start=(k == 0), stop=(k == 7)),
                         reads=[b_ccs, b_stg[i]], writes=[P.bb[i]])
                T.op("dve", lambda: nc.vector.tensor_tensor(out=rs[i][:], in0=ps[0:2, 0:CW], in1=bt[i][:], op=ALU.add),
                     reads=[P.bb[i], b_bt[i]], writes=[b_rs[i]])
                T.dma("pool", out_d[l, :, c0:c0 + CW], rs[i][:], reads=[b_rs[i]])
    T.barrier()


def load_weight_bf16(P, es, src, K, ncols, name):
    nc, T = P.nc, P.T
    dst = P.sb(es, [128, K, ncols], BF16, name)
    CW = 4096 // K
    stg = [P.sb(es, [128, K, CW], F32, name + "s") for _ in range(2)]
    b_stg = [Buf(name + "s%d" % i) for i in range(2)]
    bufs = {}
    it = 0
    for c0 in range(0, ncols, CW):
        cw = min(CW, ncols - c0)
        i = it % 2
        it += 1
        T.dma("sp", stg[i][:, :, 0:cw], src[:, c0:c0 + cw].rearrange("(k p) n -> p k n", p=128), writes=[b_stg[i]])
        b = Buf(name + "_c%d" % c0)
        T.op("act", lambda: nc.scalar.copy(out=dst[:, :, c0:c0 + cw], in_=stg[i][:, :, 0:cw]), reads=[b_stg[i]], writes=[b])
        for c in range(c0, c0 + cw, 128):
            bufs[c // 128] = b
    return dst, bufs


def rope_tok(P, src, dst, nh, hd, cos, sin, b_src, b_dst, b_tab, tmp, b_tmp):
    nc, T = P.nc, P.T
    q = hd // 4
    sv = src.rearrange("p (h a j i) -> p h a j i", h=nh, a=2, j=2, i=q)
    dv = dst.rearrange("p (h a j i) -> p h a j i", h=nh, a=2, j=2, i=q)
    x1 = sv[:, :, :, 0, :]
    x2 = sv[:, :, :, 1, :]
    cb = cos.rearrange("p (a i) -> p a i", a=2).unsqueeze(1).to_broadcast([128, nh, 2, q])
    sbc = sin.rearrange("p (a i) -> p a i", a=2).unsqueeze(1).to_broadcast([128, nh, 2, q])
    n = nh * 2 * q
    t = [tmp[j][:, 0:n].rearrange("p (h a i) -> p h a i", h=nh, a=2, i=q) for j in range(4)]
    T.op("dve", lambda: nc.vector.tensor_tensor(out=t[0], in0=x1, in1=cb, op=ALU.mult), reads=[b_src, b_tab], writes=[b_tmp[0]])
    T.op("dve", lambda: nc.vector.tensor_tensor(out=t[1], in0=x2, in1=sbc, op=ALU.mult), reads=[b_src, b_tab], writes=[b_tmp[1]])
    T.op("dve", lambda: nc.vector.tensor_tensor(out=t[2], in0=x1, in1=sbc, op=ALU.mult), reads=[b_src, b_tab], writes=[b_tmp[2]])
    T.op("dve", lambda: nc.vector.tensor_tensor(out=t[3], in0=x2, in1=cb, op=ALU.mult), reads=[b_src, b_tab], writes=[b_tmp[3]])
    T.op("pool", lambda: nc.gpsimd.tensor_tensor(out=dv[:, :, :, 0, :], in0=t[0], in1=t[1], op=ALU.subtract),
         reads=[b_tmp[0], b_tmp[1]], writes=[b_dst])
    T.op("pool", lambda: nc.gpsimd.tensor_tensor(out=dv[:, :, :, 1, :], in0=t[2], in1=t[3], op=ALU.add),
         reads=[b_tmp[2], b_tmp[3]], writes=[b_dst])


def transpose_mod(P, xt, b_xt, scT, shT, b_mod, hT, b_hT, tb, hTf=None, b_hTf=None):
    nc, T = P.nc, P.T
    for k in range(8):
        bk = tb[k // 4]
        T.op("pe", lambda: nc.tensor.transpose(P.banks[bk][:, (k % 4) * 128:(k % 4 + 1) * 128], xt[:, k * 128:(k + 1) * 128], P.ident[:]),
             reads=[b_xt, P.b_ident], writes=[P.bb[bk]])
    for k in range(8):
        bk = tb[k // 4]
        src = P.banks[bk][:, (k % 4) * 128:(k % 4 + 1) * 128]
        if hTf is None:
            T.op("act", lambda: nc.scalar.activation(out=hT[:, k, :], in_=src, func=AF.Identity, bias=shT[:, k:k + 1], scale=scT[:, k:k + 1]),
                 reads=[P.bb[bk], b_mod], writes=[b_hT])
        else:
            T.op("act", lambda: nc.scalar.activation(out=hTf[:, k, :], in_=src, func=AF.Identity, bias=shT[:, k:k + 1], scale=scT[:, k:k + 1]),
                 reads=[P.bb[bk], b_mod], writes=[b_hTf])
    if hTf is not None:
        T.op("dve", lambda: nc.vector.tensor_copy(out=hT, in_=hTf[:]), reads=[b_hTf], writes=[b_hT])


def load_modT(P, es, m_d, name):
    nc, T = P.nc, P.T
    m = P.sb(es, [128, 2, 2, 8], F32, name)
    b = Buf(name)
    T.dma("sp", m[:], m_d[:, :, :, :], writes=[b])
    T.op("dve", lambda: nc.vector.tensor_scalar(out=m[:, :, 0, :], in0=m[:, :, 0, :], scalar1=1.0, scalar2=None, op0=ALU.add),
         reads=[b], writes=[b])
    return m, b


def phase_A(P, even, x_d, x_bufs, w_d, mA_d, cos_d, sin_d, qng_d, qkv_d):
    nc, T = P.nc, P.T
    ncols = E_COLS if even else O_COLS
    hd = 64 if even else 128
    hq = hd // 2
    with ExitStack() as es:
        W, wb = load_weight_bf16(P, es, w_d, 8, ncols, "win")
        m, b_m = load_modT(P, es, mA_d, "mA")
        xt = [P.sb(es, [128, D], F32, "xt") for _ in range(2)]
        b_xt = [Buf("xt%d" % i) for i in range(2)]
        hT = [P.sb(es, [128, 8, 128], BF16, "hT") for _ in range(2)]
        b_hT = [Buf("hT%d" % i) for i in range(2)]
        ot = [P.sb(es, [128, ncols], BF16, "ot") for _ in range(2)]
        b_ot = [Buf("ot%d" % i) for i in range(2)]
        cs = [P.sb(es, [128, 2, hq], F32, "cs") for _ in range(2)]
        b_cs = [Buf("cs%d" % i) for i in range(2)]
        tmp = [P.sb(es, [128, 512], F32, "rt") for _ in range(4)]
        b_tmp = [Buf("rt%d" % i) for i in range(4)]
        if not even:
            gq = P.sb(es, [128, 2, 128], F32, "gq")
            b_gq = Buf("gq")
            T.dma("sp", gq[:, 0, :], qng_d[0:1, :].partition_broadcast(128), writes=[b_gq])
            T.dma("sp", gq[:, 1, :], qng_d[1:2, :].partition_broadcast(128), writes=[b_gq])
            epsq = P.sb(es, [128, 1], F32, "epsq")
            b_epsq = Buf("epsq")
            T.op("pool", lambda: nc.gpsimd.memset(epsq[:], QK_EPS), writes=[b_epsq])
            sq = P.sb(es, [128, 512], F32, "sq")
            b_sq = Buf("sq")
            ssq = P.sb(es, [128, 4], F32, "ssq")
            b_ssq = Buf("ssq")
            xn = [P.sb(es, [128, 512], F32, "xn") for _ in range(2)]
            b_xn = [Buf("xn%d" % i) for i in range(2)]
        nbk = (ncols + 511) // 512
        for t in range(NT):
            i = t % 2
            who = 1 if t == NT - 1 else 0
            rd = [x_bufs[t]] if x_bufs is not None else []
            T.dma("sp", xt[i][:], x_d[t * 128:(t + 1) * 128, :], reads=rd, writes=[b_xt[i]])
            T.dma("sp", cs[i][:, 0, :], cos_d[t * 128:(t + 1) * 128, :], writes=[b_cs[i]])
            T.dma("sp", cs[i][:, 1, :], sin_d[t * 128:(t + 1) * 128, :], writes=[b_cs[i]])
            transpose_mod(P, xt[i], b_xt[i], m[:, who, 0, :], m[:, who, 1, :], b_m, hT[i], b_hT[i], (6, 7))
            for c in range(nbk):
                cw = min(512, ncols - c * 512)
                for k in range(8):
                    T.op("pe", lambda: nc.tensor.matmul(P.banks[c][:, 0:cw], lhsT=hT[i][:, k, :], rhs=W[:, k, c * 512:c * 512 + cw],
                                                        start=(k == 0), stop=(k == 7)),
                         reads=[b_hT[i], wb[c * 4]] + ([wb[c * 4 + 3]] if cw == 512 else []), writes=[P.bb[c]])
            o = ot[i]
            cosv, sinv = cs[i][:, 0, :], cs[i][:, 1, :]
            if even:
                def rp(bank, c0, c1):
                    rope_tok(P, P.banks[bank][:, c0 - bank * 512:c1 - bank * 512], o[:, c0:c1], (c1 - c0) // 64, 64, cosv, sinv,
                             P.bb[bank], b_ot[i], b_cs[i], tmp, b_tmp)
                rp(0, 0, 512)
                rp(1, 512, 640)
                rp(1, 768, 1024)
                rp(2, 1024, 1536)
                rp(3, 1536, 1792)
                T.op("act", lambda: nc.scalar.copy(out=o[:, 640:768], in_=P.banks[1][:, 128:256]), reads=[P.bb[1]], writes=[b_ot[i]])
                T.op("act", lambda: nc.scalar.copy(out=o[:, 1792:2048], in_=P.banks[3][:, 256:512]), reads=[P.bb[3]], writes=[b_ot[i]])
                T.op("act", lambda: nc.scalar.copy(out=o[:, 2048:2304], in_=P.banks[4][:, 0:256]), reads=[P.bb[4]], writes=[b_ot[i]])
            else:
                for bank, nh, gi in ((0, 4, 0), (1, 4, 0), (2, 2, 1)):
                    n = nh * 128
                    src = P.banks[bank][:, 0:n]
                    T.op("act", lambda: nc.scalar.activation(out=sq[:, 0:n], in_=src, func=AF.Square), reads=[P.bb[bank]], writes=[b_sq])
                    T.op("dve", lambda: nc.vector.tensor_reduce(out=ssq[:, 0:nh], in_=sq[:, 0:n].rearrange("p (h d) -> p h d", h=nh),
                                                                axis=AX.X, op=ALU.add), reads=[b_sq], writes=[b_ssq])
                    T.op("act", lambda: nc.scalar.activation(out=ssq[:, 0:nh], in_=ssq[:, 0:nh], func=AF.Sqrt, bias=epsq[:, 0:1], scale=1.0 / 128.0),
                         reads=[b_ssq, b_epsq], writes=[b_ssq])
                    T.op("dve", lambda: nc.vector.reciprocal(out=ssq[:, 0:nh], in_=ssq[:, 0:nh]), reads=[b_ssq], writes=[b_ssq])
                    j = bank % 2
                    T.op("dve", lambda: nc.vector.tensor_tensor(out=xn[j][:, 0:n].rearrange("p (h d) -> p h d", h=nh),
                                                                in0=src.rearrange("p (h d) -> p h d", h=nh),
                                                                in1=ssq[:, 0:nh].unsqueeze(2).to_broadcast([128, nh, 128]), op=ALU.mult),
                         reads=[P.bb[bank], b_ssq], writes=[b_xn[j]])
                    T.op("pool", lambda: nc.gpsimd.tensor_tensor(out=xn[j][:, 0:n].rearrange("p (h d) -> p h d", h=nh),
                                                                 in0=xn[j][:, 0:n].rearrange("p (h d) -> p h d", h=nh),
                                                                 in1=gq[:, gi, :].unsqueeze(1).to_broadcast([128, nh, 128]), op=ALU.mult),
                         reads=[b_xn[j], b_gq], writes=[b_xn[j]])
                    rope_tok(P, xn[j][:, 0:n], o[:, bank * 512:bank * 512 + n], nh, 128, cosv, sinv, b_xn[j], b_ot[i], b_cs[i], tmp, b_tmp)
                T.op("act", lambda: nc.scalar.copy(out=o[:, 1280:1536], in_=P.banks[2][:, 256:512]), reads=[P.bb[2]], writes=[b_ot[i]])
            T.dma("pool", qkv_d[t * 128:(t + 1) * 128, :], o[:], reads=[b_ot[i]])
    T.barrier()


class SweepCtx:
    def __init__(self, P, es, npt=5):
        self.P = P
        self.pT = [P.sb(es, [128, 2, 512], BF16, "pT") for _ in range(npt)]
        self.b_pT = [Buf("pT%d" % i) for i in range(npt)]
        self.n = 0
        self.ps = [P.sb(es, [128, 512], BF16, "psum2") for _ in range(4)]
        self.b_ps = [Buf("psum2_%d" % i) for i in range(4)]
        self.m = 0


def sweep(P, S, N, blocks, qT, b_q, kT_of, v_of, b_kv, M, scale):
    nc, T = P.nc, P.T
    SK = 2
    ob, sb_ = 6, 7
    nb = len(blocks)
    assert nb % 2 == 0
    npair = nb // 2
    idx = []
    pidx = []
    for u in range(npair + SK):
        if u < npair:
            n = S.n
            S.n += 1
            idx.append(n)
            sp = n % 3
            p = n % len(S.pT)
            for b in range(2):
                bk = 2 * sp + b
                T.op("pe", lambda: nc.tensor.matmul(P.banks[bk][:, 0:N], lhsT=kT_of(blocks[2 * u + b]), rhs=qT, start=True, stop=True),
                     reads=[b_kv, b_q], writes=[P.bb[bk]])
            src = P.pbig[sp][:, :].rearrange("p (b n) -> p b n", b=2)[:, :, 0:N]
            T.op("act", lambda: nc.scalar.activation(out=S.pT[p][:, :, 0:N], in_=src, func=AF.Exp, scale=scale),
                 reads=[P.bb[2 * sp], P.bb[2 * sp + 1]], writes=[S.b_pT[p]])
        if 1 <= u <= npair:
            p = idx[u - 1] % len(S.pT)
            q = S.m % len(S.ps)
            S.m += 1
            pidx.append(q)
            T.op("dve", lambda: nc.vector.tensor_tensor(out=S.ps[q][:, 0:N], in0=S.pT[p][:, 0, 0:N], in1=S.pT[p][:, 1, 0:N], op=ALU.add),
                 reads=[S.b_pT[p]], writes=[S.b_ps[q]])
        if u >= SK:
            v = u - SK
            p = idx[v] % len(S.pT)
            for b in range(2):
                j = 2 * v + b
                T.op("pe", lambda: nc.tensor.matmul(P.banks[ob][0:M, 0:N], lhsT=v_of(blocks[j]), rhs=S.pT[p][:, b, 0:N], start=(j == 0), stop=(j == nb - 1)),
                     reads=[b_kv, S.b_pT[p]], writes=[P.bb[ob]])
            q = pidx[v]
            T.op("pe", lambda: nc.tensor.matmul(P.banks[sb_][:, 0:N], lhsT=P.ones[:], rhs=S.ps[q][:, 0:N], start=(v == 0), stop=(v == npair - 1)),
                 reads=[P.b_ones, S.b_ps[q]], writes=[P.bb[sb_]])
    return ob, sb_


ALL_BLOCKS = list(range(NKB))
CTX_BLOCKS = [0, 1]


def phase_B_odd(P, OT, b_OT, QT_d, KT_d, V_d):
    nc, T = P.nc, P.T
    scale = 128.0 ** -0.5
    with ExitStack() as es:
        S = SweepCtx(P, es)
        kts = [P.sb(es, [128, NKB * 128], BF16, "kt") for _ in range(2)]
        vts = [P.sb(es, [128, NKB, 128], BF16, "vt") for _ in range(2)]
        b_kvs = [Buf("kv%d" % i) for i in range(2)]
        qt = [P.sb(es, [128, TOK], BF16, "qt") for _ in range(2)]
        b_qt = [Buf("qt%d" % i) for i in range(2)]
        rec = [P.sb(es, [128, 512], F32, "rec") for _ in range(2)]
        b_rec = [Buf("rec%d" % i) for i in range(2)]
        fi = 0
        for kvh in range(2):
            kt, vt, b_kv = kts[kvh], vts[kvh], b_kvs[kvh]
            T.dma("sp", kt[:], KT_d[kvh, :, :], writes=[b_kv])
            T.dma("sp", vt[:], V_d[kvh, :, :, :], writes=[b_kv])
        for kvh in range(2):
            kt, vt, b_kv = kts[kvh], vts[kvh], b_kvs[kvh]
            for g in range(4):
                h = kvh * 4 + g
                qi = h % 2
                T.dma("sp", qt[qi][:], QT_d[h, :, :], writes=[b_qt[qi]])
                for (c0, N) in CHUNKS:
                    blocks = ALL_BLOCKS if N == 512 else CTX_BLOCKS
                    ksl = slice(64, 128) if "k64" in os.environ.get("K_DBG", "") else slice(0, 128)
                    ob, sbk = sweep(P, S, N, blocks, qt[qi][ksl, c0:c0 + N], b_qt[qi],
                                    lambda b: kt[ksl, b * 128:(b + 1) * 128], lambda b: vt[:, b, :], b_kv, 128, scale)
                    r = fi % 2
                    fi += 1
                    T.op("dve", lambda: nc.vector.reciprocal(out=rec[r][:, 0:N], in_=P.banks[sbk][:, 0:N]), reads=[P.bb[sbk]], writes=[b_rec[r]])
                    T.op("dve", lambda: nc.vector.tensor_tensor(out=OT[:, h, c0:c0 + N], in0=P.banks[ob][:, 0:N], in1=rec[r][:, 0:N], op=ALU.mult),
                         reads=[P.bb[ob], b_rec[r]], writes=[b_OT])
    T.barrier()


def phase_B_even(P, OT, b_OT, QTa_d, KTa_d, Va_d, mask_d, sinkp_d, QTb_d, KTb_d, Vb_d, lamv_d, lami_d, subg_d):
    nc, T = P.nc, P.T
    sc = 64.0 ** -0.5
    with ExitStack() as es:
        if "nomA" in os.environ.get("K_DBG", ""):
            es.close()
            return phase_B_even_mixB(P, OT, b_OT, QTb_d, KTb_d, Vb_d, lamv_d, lami_d, subg_d)
        qta = P.sb(es, [128, 4, TOK], BF16, "qta")
        b_qta = Buf("qta")
        T.dma("sp", qta[:], QTa_d[:, :, :], writes=[b_qta])
        ktaw = P.sb(es, [128, 20 * 128], BF16, "ktaw")
        b_kta = Buf("ktaw")
        T.dma("sp", ktaw[:], KTa_d[:, :], writes=[b_kta])
        vaw = P.sb(es, [128, 4, 20, 128], BF16, "vaw")
        b_vaw = Buf("vaw")
        T.dma("sp", vaw[:], Va_d[:, :, :, :], writes=[b_vaw])
        msk = P.sb(es, [128, NT, 384], BF16, "msk")
        b_msk = Buf("msk")
        T.dma("sp", msk[:], mask_d[:, :, :], writes=[b_msk])
        esink = P.sb(es, [128, 4], F32, "esink")
        b_es = Buf("esink")
        T.dma("sp", esink[:], sinkp_d[:, :], writes=[b_es])
        T.op("act", lambda: nc.scalar.activation(out=esink[:], in_=esink[:], func=AF.Exp), reads=[b_es], writes=[b_es])
        oneslh = P.sb(es, [128, 2, 128], BF16, "oneslh")
        b_olh = Buf("oneslh")
        T.op("pool", lambda: nc.gpsimd.memset(oneslh[:], 0.0), writes=[b_olh])
        T.op("pool", lambda: nc.gpsimd.memset(oneslh[:, 0, 0:64], 1.0), reads=[b_olh], writes=[b_olh])
        T.op("pool", lambda: nc.gpsimd.memset(oneslh[:, 1, 64:128], 1.0), reads=[b_olh], writes=[b_olh])
        pTa = [P.sb(es, [128, 640], BF16, "pTa") for _ in range(4)]
        b_pTa = [Buf("pTa%d" % i) for i in range(4)]
        den = [P.sb(es, [128, 128], F32, "den") for _ in range(2)]
        b_den = [Buf("den%d" % i) for i in range(2)]
        n = 0
        fi = 0
        for c in range(4):
            kvh = c // 2
            ksl = slice(kvh * 64, kvh * 64 + 64)
            for t in range(NT):
                wl = (t + 2) if t < NT - 1 else 2
                blocks = [0, 1, wl, wl + 1, wl + 2]
                ob, sbk = (4, 5) if fi % 2 == 0 else (6, 7)
                for hh in range(2):
                    j = (2 * c + hh) % 4
                    p = n % 4
                    s0, s1 = (0, 1) if n % 2 == 0 else (2, 3)
                    n += 1
                    q_ap = qta[ksl, j, t * 128:(t + 1) * 128]
                    for bi, w in enumerate(blocks):
                        bk, col = (s0, bi * 128) if bi < 2 else (s1, (bi - 2) * 128)
                        T.op("pe", lambda: nc.tensor.matmul(P.banks[bk][:, col:col + 128], lhsT=ktaw[ksl, w * 128:(w + 1) * 128], rhs=q_ap,
                                                            start=True, stop=True), reads=[b_kta, b_qta], writes=[P.bb[bk]])
                    T.op("act", lambda: nc.scalar.activation(out=pTa[p][:, 0:256], in_=P.banks[s0][:, 0:256], func=AF.Exp, scale=sc),
                         reads=[P.bb[s0]], writes=[b_pTa[p]])
                    T.op("act", lambda: nc.scalar.activation(out=pTa[p][:, 256:640], in_=P.banks[s1][:, 0:384], func=AF.Exp, scale=sc),
                         reads=[P.bb[s1]], writes=[b_pTa[p]])
                    T.op("pool", lambda: nc.gpsimd.tensor_tensor(out=pTa[p][:, 256:640], in0=pTa[p][:, 256:640], in1=msk[:, t, :], op=ALU.mult),
                         reads=[b_pTa[p], b_msk], writes=[b_pTa[p]])
                    for bi, w in enumerate(blocks):
                        first = (hh == 0 and bi == 0)
                        last = (hh == 1 and bi == 4)
                        T.op("pe", lambda: nc.tensor.matmul(P.banks[ob][:, 0:128], lhsT=vaw[:, kvh * 2 + hh, w, :], rhs=pTa[p][:, bi * 128:(bi + 1) * 128],
                                                            start=first, stop=last), reads=[b_vaw, b_pTa[p]], writes=[P.bb[ob]])
                        T.op("pe", lambda: nc.tensor.matmul(P.banks[sbk][:, 0:128], lhsT=oneslh[:, hh, :], rhs=pTa[p][:, bi * 128:(bi + 1) * 128],
                                                            start=first, stop=last), reads=[b_olh, b_pTa[p]], writes=[P.bb[sbk]])
                r = fi % 2
                fi += 1
                T.op("dve", lambda: nc.vector.tensor_scalar(out=den[r][:], in0=P.banks[sbk][:, 0:128], scalar1=esink[:, c:c + 1], scalar2=None, op0=ALU.add),
                     reads=[P.bb[sbk], b_es], writes=[b_den[r]])
                T.op("dve", lambda: nc.vector.reciprocal(out=den[r][:], in_=den[r][:]), reads=[b_den[r]], writes=[b_den[r]])
                T.op("dve", lambda: nc.vector.tensor_tensor(out=OT[:, c, t * 128:(t + 1) * 128], in0=P.banks[ob][:, 0:128], in1=den[r][:], op=ALU.mult),
                     reads=[P.bb[ob], b_den[r]], writes=[b_OT])
    T.barrier()
    if "nomB" in os.environ.get("K_DBG", ""):
        return
    phase_B_even_mixB(P, OT, b_OT, QTb_d, KTb_d, Vb_d, lamv_d, lami_d, subg_d)


def phase_B_even_mixB(P, OT, b_OT, QTb_d, KTb_d, Vb_d, lamv_d, lami_d, subg_d):
    nc, T = P.nc, P.T
    sc = 64.0 ** -0.5
    with ExitStack() as es:
        S = SweepCtx(P, es)
        ktbs = [P.sb(es, [128, NKB * 128], BF16, "ktb")] * 2
        vtbs = [P.sb(es, [128, NKB, 128], BF16, "vtb")] * 2
        b_kvs = [Buf("kvb")] * 2
        qtb = [P.sb(es, [128, 2, TOK], BF16, "qtb") for _ in range(2)]
        b_qtb = [Buf("qtb%d" % i) for i in range(2)]
        lamv = P.sb(es, [128, 4, 64], F32, "lamv")
        b_lamv = Buf("lamv")
        for i in range(4):
            T.dma("sp", lamv[:, i, :], lamv_d[i:i + 1, :].partition_broadcast(128), writes=[b_lamv])
        lami = P.sb(es, [128, 2], F32, "lami")
        b_lami = Buf("lami")
        T.dma("sp", lami[:], lami_d[:, :], writes=[b_lami])
        lp = P.sb(es, [128, 2, 64], F32, "lp")
        b_lp = Buf("lp")
        ls = P.sb(es, [128, 4], F32, "ls")
        b_ls = Buf("ls")
        T.op("dve", lambda: nc.vector.tensor_tensor(out=lp[:, 0, :], in0=lamv[:, 0, :], in1=lamv[:, 1, :], op=ALU.mult), reads=[b_lamv], writes=[b_lp])
        T.op("dve", lambda: nc.vector.tensor_tensor(out=lp[:, 1, :], in0=lamv[:, 2, :], in1=lamv[:, 3, :], op=ALU.mult), reads=[b_lamv, b_lp], writes=[b_lp])
        T.op("dve", lambda: nc.vector.tensor_reduce(out=ls[:, 0:2], in_=lp[:], axis=AX.X, op=ALU.add), reads=[b_lp], writes=[b_ls])
        T.op("act", lambda: nc.scalar.activation(out=ls[:, 0:2], in_=ls[:, 0:2], func=AF.Exp), reads=[b_ls], writes=[b_ls])
        T.op("dve", lambda: nc.vector.tensor_tensor(out=ls[:, 2:3], in0=ls[:, 1:2], in1=ls[:, 0:1], op=ALU.subtract), reads=[b_ls], writes=[b_ls])
        T.op("dve", lambda: nc.vector.tensor_tensor(out=ls[:, 2:3], in0=ls[:, 2:3], in1=lami[:, 0:1], op=ALU.subtract), reads=[b_ls, b_lami], writes=[b_ls])
        gsc = P.sb(es, [128, 1], F32, "gsc")
        b_gsc = Buf("gsc")
        T.dma("sp", gsc[:], subg_d[:, :], writes=[b_gsc])
        T.op("dve", lambda: nc.vector.tensor_tensor(out=gsc[:], in0=gsc[:], in1=lami[:, 1:2], op=ALU.mult), reads=[b_gsc, b_lami], writes=[b_gsc])
        epss = P.sb(es, [128, 1], F32, "epss")
        b_epss = Buf("epss")
        T.op("pool", lambda: nc.gpsimd.memset(epss[:], SUBLN_EPS), writes=[b_epss])
        rec = P.sb(es, [128, 512], F32, "recb")
        b_rec = Buf("recb")
        am = [P.sb(es, [128, 512], F32, "am") for _ in range(2)]
        b_am = [Buf("am%d" % i) for i in range(2)]
        dm = P.sb(es, [128, 512], F32, "dm")
        b_dm = Buf("dm")
        sq = P.sb(es, [128, 512], F32, "sqb")
        b_sq = Buf("sqb")
        rstd = P.sb(es, [128, 512], F32, "rstd")
        b_rstd = Buf("rstd")
        def load_head(h):
            T.dma("sp", ktbs[h % 2][:], KTb_d[h, :, :], writes=[b_kvs[h % 2]])
            T.dma("sp", vtbs[h % 2][:], Vb_d[h, :, :, :], writes=[b_kvs[h % 2]])
            T.dma("sp", qtb[h % 2][:], QTb_d[h, :, :, :], writes=[b_qtb[h % 2]])
        load_head(0)
        for h in range(4):
            ktb, vtb, b_kv = ktbs[h % 2], vtbs[h % 2], b_kvs[h % 2]
            qi = h % 2
            if h > 0:
                load_head(h)
            for (c0, N) in CHUNKS:
                blocks = ALL_BLOCKS if N == 512 else CTX_BLOCKS
                for mm in range(2):
                    ob, sbk = sweep(P, S, N, blocks, qtb[qi][:, mm, c0:c0 + N], b_qtb[qi],
                                    lambda b: ktb[:, b * 128:(b + 1) * 128], lambda b: vtb[:, b, :], b_kv, 128, sc)
                    T.op("dve", lambda: nc.vector.reciprocal(out=rec[:, 0:N], in_=P.banks[sbk][:, 0:N]), reads=[P.bb[sbk]], writes=[b_rec])
                    T.op("dve", lambda: nc.vector.tensor_tensor(out=am[mm][:, 0:N], in0=P.banks[ob][:, 0:N], in1=rec[:, 0:N], op=ALU.mult),
                         reads=[P.bb[ob], b_rec], writes=[b_am[mm]])
                T.op("dve", lambda: nc.vector.scalar_tensor_tensor(out=dm[:, 0:N], in0=am[1][:, 0:N], scalar=ls[:, 2:3], in1=am[0][:, 0:N],
                                                                   op0=ALU.mult, op1=ALU.add), reads=[b_am[0], b_am[1], b_ls], writes=[b_dm])
                T.op("act", lambda: nc.scalar.activation(out=sq[:, 0:N], in_=dm[:, 0:N], func=AF.Square), reads=[b_dm], writes=[b_sq])
                T.op("pe", lambda: nc.tensor.matmul(P.banks[sbk][:, 0:N], lhsT=P.onesd[:], rhs=sq[:, 0:N], start=True, stop=True),
                     reads=[P.b_onesd, b_sq], writes=[P.bb[sbk]])
                T.op("act", lambda: nc.scalar.activation(out=rstd[:, 0:N], in_=P.banks[sbk][:, 0:N], func=AF.Sqrt, bias=epss[:, 0:1], scale=1.0),
                     reads=[P.bb[sbk], b_epss], writes=[b_rstd])
                T.op("dve", lambda: nc.vector.reciprocal(out=rstd[:, 0:N], in_=rstd[:, 0:N]), reads=[b_rstd], writes=[b_rstd])
                T.op("dve", lambda: nc.vector.scalar_tensor_tensor(out=OT[:, 4 + h, c0:c0 + N], in0=dm[:, 0:N], scalar=gsc[:, 0:1], in1=rstd[:, 0:N],
                                                                   op0=ALU.mult, op1=ALU.mult), reads=[b_dm, b_gsc, b_rstd], writes=[b_OT])
    T.barrier()


def layer_norm_tile(P, L, src, b_src, dst, b_dst, gam, bet, b_gb):
    nc, T = P.nc, P.T
    st, b_st, mv, b_mv, xn, b_xn = L
    T.op("dve", lambda: nc.vector.bn_stats(out=st[:, 0, :], in_=src[:, 0:512]), reads=[b_src], writes=[b_st])
    T.op("dve", lambda: nc.vector.bn_stats(out=st[:, 1, :], in_=src[:, 512:1024]), reads=[b_src, b_st], writes=[b_st])
    T.op("dve", lambda: nc.vector.bn_aggr(out=mv[:, 0:2], in_=st[:].rearrange("p a s -> p (a s)")), reads=[b_st], writes=[b_mv])
    T.op("act", lambda: nc.scalar.activation(out=mv[:, 2:3], in_=mv[:, 1:2], func=AF.Sqrt, bias=mv[:, 3:4], scale=1.0), reads=[b_mv], writes=[b_mv])
    T.op("dve", lambda: nc.vector.reciprocal(out=mv[:, 2:3], in_=mv[:, 2:3]), reads=[b_mv], writes=[b_mv])
    T.op("dve", lambda: nc.vector.tensor_scalar(out=xn[:], in0=src, scalar1=mv[:, 0:1], scalar2=mv[:, 2:3], op0=ALU.subtract, op1=ALU.mult),
         reads=[b_src, b_mv], writes=[b_xn])
    T.op("pool", lambda: nc.gpsimd.tensor_tensor(out=xn[:], in0=xn[:], in1=gam, op=ALU.mult), reads=[b_xn, b_gb], writes=[b_xn])
    T.op("pool", lambda: nc.gpsimd.tensor_tensor(out=dst, in0=xn[:], in1=bet, op=ALU.add), reads=[b_xn, b_gb], writes=[b_dst])


def ln_scratch(P, es, tag):
    nc, T = P.nc, P.T
    st = P.sb(es, [128, 2, 6], F32, "lnst")
    mv = P.sb(es, [128, 4], F32, "lnmv")
    xn = P.sb(es, [128, D], F32, "lnxn")
    b_mv = Buf("lnmv" + tag)
    T.op("pool", lambda: nc.gpsimd.memset(mv[:, 3:4], LN_EPS), writes=[b_mv])
    return (st, Buf("lnst" + tag), mv, b_mv, xn, Buf("lnxn" + tag))


def phase_C1(P, OT, b_OT, x_d, wo_d, gbc_d, lng_d, lnb_d, x1_d, x1b):
    nc, T = P.nc, P.T
    with ExitStack() as es:
        wo, wob = load_weight_bf16(P, es, wo_d, 8, D, "wo")
        gb = P.sb(es, [128, 2, D], F32, "g1bc")
        b_gb = Buf("g1bc")
        for who in range(2):
            T.dma("sp", gb[:, who, :], gbc_d[who, 0:1, :].partition_broadcast(128), writes=[b_gb])
        ln = P.sb(es, [128, 2, D], F32, "ln1")
        b_ln = Buf("ln1")
        T.dma("sp", ln[:, 0, :], lng_d[0:1, :].partition_broadcast(128), writes=[b_ln])
        T.dma("sp", ln[:, 1, :], lnb_d[0:1, :].partition_broadcast(128), writes=[b_ln])
        L = ln_scratch(P, es, "1")
        xt = [P.sb(es, [128, D], F32, "cxt") for _ in range(2)]
        b_xt = [Buf("cxt%d" % i) for i in range(2)]
        rr = [P.sb(es, [128, D], F32, "crr") for _ in range(2)]
        b_rr = [Buf("crr%d" % i) for i in range(2)]
        x1t = [P.sb(es, [128, D], F32, "x1t") for _ in range(2)]
        b_x1t = [Buf("x1t%d" % i) for i in range(2)]
        for t in range(NT):
            i = t % 2
            who = 1 if t == NT - 1 else 0
            yb = (0, 1) if i == 0 else (2, 3)
            T.dma("sp", xt[i][:], x_d[t * 128:(t + 1) * 128, :], writes=[b_xt[i]])
            for half in range(2):
                for k in range(8):
                    T.op("pe", lambda: nc.tensor.matmul(P.banks[yb[half]][:, :], lhsT=OT[:, k, t * 128:(t + 1) * 128],
                                                        rhs=wo[:, k, half * 512:(half + 1) * 512], start=(k == 0), stop=(k == 7)),
                         reads=[b_OT, wob[half * 4]], writes=[P.bb[yb[half]]])
                T.op("dve", lambda: nc.vector.tensor_tensor(out=rr[i][:, half * 512:(half + 1) * 512], in0=P.banks[yb[half]][:, :],
                                                            in1=gb[:, who, half * 512:(half + 1) * 512], op=ALU.mult),
                     reads=[P.bb[yb[half]], b_gb], writes=[b_rr[i]])
            T.op("dve", lambda: nc.vector.scalar_tensor_tensor(out=rr[i][:], in0=xt[i][:], scalar=ALPHA, in1=rr[i][:], op0=ALU.mult, op1=ALU.add),
                 reads=[b_xt[i], b_rr[i]], writes=[b_rr[i]])
            layer_norm_tile(P, L, rr[i][:], b_rr[i], x1t[i][:], b_x1t[i], ln[:, 0, :], ln[:, 1, :], b_ln)
            T.dma("pool", x1_d[t * 128:(t + 1) * 128, :], x1t[i][:], reads=[b_x1t[i]], writes=[x1b[t]])
    T.barrier()


def phase_C2(P, x1_d, x1b, mC_d, gbc_d, lng_d, lnb_d, wr_d, br_d, wg_d, wu_d, wd_d, xo_d, xob):
    nc, T = P.nc, P.T
    with ExitStack() as es:
        acc = P.sb(es, [128, NT, D], F32, "acc")
        accb = [Buf("acc%d" % t) for t in range(NT)]
        h2T = P.sb(es, [128, 8, TOK], BF16, "h2T")
        h2b = [Buf("h2T%d" % c) for c in range(len(CHUNKS))]
        m, b_m = load_modT(P, es, mC_d, "mC")
        gb = P.sb(es, [128, 2, D], F32, "g2bc")
        b_gb = Buf("g2bc")
        for who in range(2):
            T.dma("sp", gb[:, who, :], gbc_d[who, 1:2, :].partition_broadcast(128), writes=[b_gb])
        wr = P.sb(es, [128, 8, NEXP], F32, "wr")
        b_wr = Buf("wr")
        T.dma("sp", wr[:], wr_d.rearrange("(k p) e -> p k e", p=128), writes=[b_wr])
        brt = P.sb(es, [128, NEXP], F32, "brt")
        b_brt = Buf("brt")
        T.dma("sp", brt[:], br_d[0:1, :].partition_broadcast(128), writes=[b_brt])
        scs = P.sb(es, [128, NT, NEXP], F32, "scs")
        b_scs = Buf("scs")
        gates = P.sb(es, [128, NT, NEXP], F32, "gates")
        b_gates = Buf("gates")
        with ExitStack() as es2:
            hTf = [P.sb(es2, [128, 8, 128], F32, "hTf") for _ in range(2)]
            b_hTf = [Buf("hTf%d" % i) for i in range(2)]
            for t in range(int(os.environ.get("K_NT", NT))):
                i = t % 2
                who = 1 if t == NT - 1 else 0
                ch = min(t // 4, 4)
                T.dma("sp", acc[:, t, :], x1_d[t * 128:(t + 1) * 128, :], reads=[x1b[t]], writes=[accb[t]])
                transpose_mod(P, acc[:, t, :], accb[t], m[:, who, 0, :], m[:, who, 1, :], b_m, h2T[:, :, t * 128:(t + 1) * 128], h2b[ch],
                              (0, 1) if i == 0 else (2, 3), None if "nohtf" in os.environ.get("K_DBG", "") else hTf[i], b_hTf[i])
                lb = 4 + i
                DBG = os.environ.get("K_DBG", "")
                if "nologit" not in DBG:
                    for k in range(8):
                        T.op("pe", lambda: nc.tensor.matmul(P.banks[lb][:, 0:NEXP], lhsT=hTf[i][:, k, :], rhs=wr[:, k, :], start=(k == 0), stop=(k == 7)),
                             reads=[b_hTf[i], b_wr], writes=[P.bb[lb]])
                    if "nosig" not in DBG:
                        T.op("act", lambda: nc.scalar.activation(out=scs[:, t, :], in_=P.banks[lb][:, 0:NEXP], func=AF.Sigmoid), reads=[P.bb[lb]], writes=[b_scs])
                if "nopool" not in DBG:
                    T.op("pool", lambda: nc.gpsimd.tensor_scalar(out=acc[:, t, :], in0=acc[:, t, :], scalar1=ALPHA, scalar2=None, op0=ALU.mult),
                         reads=[accb[t]], writes=[accb[t]])
            if "noroute" in os.environ.get("K_DBG", ""):
                return
            G = NT * 4
            sel = P.sb(es2, [128, NT, NEXP], F32, "sel")
            sel2 = P.sb(es2, [128, NT, NEXP], F32, "sel2")
            eq = P.sb(es2, [128, NT, NEXP], F32, "eq")
            m1 = P.sb(es2, [128, G], F32, "m1")
            m2 = P.sb(es2, [128, G], F32, "m2")
            gs = P.sb(es2, [128, G], F32, "gs")
            gmx = P.sb(es2, [128, NT], F32, "gmx")
            b_r = Buf("route")
            g4 = lambda a: a[:].rearrange("p t (g j) -> p (t g) j", j=4)
            bc4 = lambda a: a[:].unsqueeze(2).to_broadcast([128, G, 4])
            R = dict(reads=[b_r, b_scs, b_brt], writes=[b_r])
            T.op("dve", lambda: nc.vector.tensor_tensor(out=sel[:], in0=scs[:], in1=brt[:].unsqueeze(1).to_broadcast([128, NT, NEXP]), op=ALU.add), **R)
            T.op("dve", lambda: nc.vector.tensor_reduce(out=m1[:], in_=g4(sel), axis=AX.X, op=ALU.max), **R)
            T.op("dve", lambda: nc.vector.tensor_tensor(out=g4(eq), in0=g4(sel), in1=bc4(m1), op=ALU.is_equal), **R)
            T.op("dve", lambda: nc.vector.scalar_tensor_tensor(out=sel2[:], in0=eq[:], scalar=-1.0e9, in1=sel[:], op0=ALU.mult, op1=ALU.add), **R)
            T.op("dve", lambda: nc.vector.tensor_reduce(out=m2[:], in_=g4(sel2), axis=AX.X, op=ALU.max), **R)
            T.op("dve", lambda: nc.vector.tensor_tensor(out=gs[:], in0=m1[:], in1=m2[:], op=ALU.add), **R)
            T.op("dve", lambda: nc.vector.tensor_reduce(out=gmx[:], in_=gs[:].rearrange("p (t g) -> p t g", g=4), axis=AX.X, op=ALU.max), **R)
            T.op("dve", lambda: nc.vector.tensor_tensor(out=gs[:].rearrange("p (t g) -> p t g", g=4), in0=gs[:].rearrange("p (t g) -> p t g", g=4),
                                                        in1=gmx[:].unsqueeze(2).to_broadcast([128, NT, 4]), op=ALU.is_equal), **R)
            T.op("dve", lambda: nc.vector.tensor_tensor(out=g4(eq), in0=g4(sel), in1=bc4(m2), op=ALU.is_ge), **R)
            T.op("dve", lambda: nc.vector.tensor_tensor(out=g4(eq), in0=g4(eq), in1=bc4(gs), op=ALU.mult), **R)
            T.op("dve", lambda: nc.vector.tensor_tensor(out=sel[:], in0=scs[:], in1=eq[:], op=ALU.mult), **R)
            T.op("dve", lambda: nc.vector.tensor_reduce(out=gmx[:], in_=sel[:], axis=AX.X, op=ALU.add), **R)
            T.op("dve", lambda: nc.vector.reciprocal(out=gmx[:], in_=gmx[:]), **R)
            T.op("dve", lambda: nc.vector.tensor_tensor(out=gates[:], in0=sel[:], in1=gmx[:].unsqueeze(2).to_broadcast([128, NT, NEXP]), op=ALU.mult),
                 reads=[b_r], writes=[b_gates])
        T.barrier()
        DBG = os.environ.get("K_DBG", "")
        if "noexp" in DBG:
            return
        with ExitStack() as es3:
            wg = [P.sb(es3, [128, 8, DEXP], BF16, "wg") for _ in range(2)]
            wu = [P.sb(es3, [128, 8, DEXP], BF16, "wu") for _ in range(2)]
            wd = [P.sb(es3, [128, 4, D], BF16, "wd") for _ in range(2)]
            wbuf = [[[Buf("w%d_%d_%d" % (s_, m_, h_)) for h_ in range(2)] for m_ in range(3)] for s_ in range(2)]
            NSTG = 2
            stg = [P.sb(es3, [128, 2048], F32, "wstg") for _ in range(NSTG)]
            b_stg = [Buf("wstg%d" % i) for i in range(NSTG)]
            sg = [P.sb(es3, [128, 512], F32, "sg") for _ in range(2)]
            b_sg = [Buf("sg%d" % i) for i in range(2)]
            aT = [P.sb(es3, [128, 4, 512], BF16, "aT") for _ in range(2)]
            b_aT = [Buf("aT%d" % i) for i in range(2)]
            tmp = [P.sb(es3, [128, D], F32, "mtmp") for _ in range(2)]
            b_tmp = [Buf("mtmp%d" % i) for i in range(2)]
            si = 0
            fi = 0
            ti = 0

            def load_expert(e):
                nonlocal si
                s_ = e % 2
                for m_, (src, dst, K) in enumerate(((wg_d, wg[s_], 8), (wu_d, wu[s_], 8), (wd_d, wd[s_], 4))):
                    for h_ in range(2):
                        st = stg[si % NSTG]
                        bs = b_stg[si % NSTG]
                        si += 1
                        kh = K // 2
                        T.dma("sp", st[:].rearrange("p (k n) -> p k n", k=kh),
                              src[e, h_ * kh * 128:(h_ + 1) * kh * 128, :].rearrange("(k p) n -> p k n", p=128), writes=[bs])
                        T.op("act", lambda: nc.scalar.copy(out=dst[:, h_ * kh:(h_ + 1) * kh, :], in_=st[:].rearrange("p (k n) -> p k n", k=kh)),
                             reads=[bs], writes=[wbuf[s_][m_][h_]])

            def gate_up(e, chn, a):
                nonlocal fi
                s_ = e % 2
                c0, N = CHUNKS[chn]
                for f in range(4):
                    gbk = fi % 2
                    ubk = 2 + fi % 2
                    fi += 1
                    for k in range(8):
                        T.op("pe", lambda: nc.tensor.matmul(P.banks[gbk][:, 0:N], lhsT=wg[s_][:, k, f * 128:(f + 1) * 128], rhs=h2T[:, k, c0:c0 + N],
                                                            start=(k == 0), stop=(k == 7)), reads=[wbuf[s_][0][k // 4], h2b[chn]], writes=[P.bb[gbk]])
                    for k in range(8):
                        T.op("pe", lambda: nc.tensor.matmul(P.banks[ubk][:, 0:N], lhsT=wu[s_][:, k, f * 128:(f + 1) * 128], rhs=h2T[:, k, c0:c0 + N],
                                                            start=(k == 0), stop=(k == 7)), reads=[wbuf[s_][1][k // 4], h2b[chn]], writes=[P.bb[ubk]])
                    T.op("act", lambda: nc.scalar.activation(out=sg[gbk][:, 0:N], in_=P.banks[gbk][:, 0:N], func=AF.Silu),
                         reads=[P.bb[gbk]], writes=[b_sg[gbk]])
                    T.op("dve", lambda: nc.vector.tensor_tensor(out=aT[a][:, f, 0:N], in0=sg[gbk][:, 0:N], in1=P.banks[ubk][:, 0:N], op=ALU.mult),
                         reads=[b_sg[gbk], P.bb[ubk]], writes=[b_aT[a]])

            def down(e, chn, a):
                nonlocal ti
                s_ = e % 2
                c0, N = CHUNKS[chn]
                for tt in range(N // 128):
                    t = c0 // 128 + tt
                    who = 1 if t == NT - 1 else 0
                    yb = (4, 5) if ti % 2 == 0 else (6, 7)
                    tm = ti % 2
                    ti += 1
                    for half in range(2):
                        for f in range(4):
                            T.op("pe", lambda: nc.tensor.matmul(P.banks[yb[half]][:, :], lhsT=aT[a][:, f, tt * 128:(tt + 1) * 128],
                                                                rhs=wd[s_][:, f, half * 512:(half + 1) * 512], start=(f == 0), stop=(f == 3)),
                                 reads=[b_aT[a], wbuf[s_][2][f // 2]], writes=[P.bb[yb[half]]])
                        T.op("dve", lambda: nc.vector.scalar_tensor_tensor(out=tmp[tm][:, half * 512:(half + 1) * 512], in0=P.banks[yb[half]][:, :],
                                                                           scalar=gates[:, t, e:e + 1], in1=gb[:, who, half * 512:(half + 1) * 512],
                                                                           op0=ALU.mult, op1=ALU.mult),
                             reads=[P.bb[yb[half]], b_gates, b_gb], writes=[b_tmp[tm]])
                    T.op("pool", lambda: nc.gpsimd.tensor_tensor(out=acc[:, t, :], in0=acc[:, t, :], in1=tmp[tm][:], op=ALU.add),
                         reads=[accb[t], b_tmp[tm]], writes=[accb[t]])

            items = [(e, chn) for e in range(NEXP) for chn in range(len(CHUNKS))]
            load_expert(0)
            for i in range(len(items) + 1):
                if i < len(items):
                    e, chn = items[i]
                    gate_up(e, chn, i % 2)
                if i >= 1:
                    e0, chn0 = items[i - 1]
                    down(e0, chn0, (i - 1) % 2)
                if i < len(items):
                    e, chn = items[i]
                    if chn == 0 and e + 1 < NEXP:
                        load_expert(e + 1)
        T.barrier()
        with ExitStack() as es4:
            L = ln_scratch(P, es4, "2")
            ln = P.sb(es4, [128, 2, D], F32, "ln2")
            b_ln = Buf("ln2")
            T.dma("sp", ln[:, 0, :], lng_d[1:2, :].partition_broadcast(128), writes=[b_ln])
            T.dma("sp", ln[:, 1, :], lnb_d[1:2, :].partition_broadcast(128), writes=[b_ln])
            xo = [P.sb(es4, [128, D], F32, "xo") for _ in range(2)]
            b_xo = [Buf("xo%d" % i) for i in range(2)]
            for t in range(NT):
                i = t % 2
                layer_norm_tile(P, L, acc[:, t, :], accb[t], xo[i][:], b_xo[i], ln[:, 0, :], ln[:, 1, :], b_ln)
                T.dma("pool", xo_d[t * 128:(t + 1) * 128, :], xo[i][:], reads=[b_xo[i]], writes=[xob[t]])
    T.barrier()


def rope_tables(hd):
    q = hd // 4
    inv = (10000.0 ** (-np.arange(q, dtype=np.float32) / np.float32(q))).astype(np.float32)
    tpos = np.arange(SEQ)
    ang_r = (tpos // 64).astype(np.float32)[:, None] * inv[None, :]
    ang_c = (tpos % 64).astype(np.float32)[:, None] * inv[None, :]
    cos = np.concatenate([np.cos(ang_r), np.cos(ang_c)], axis=1).astype(np.float32)
    sin = np.concatenate([np.sin(ang_r), np.sin(ang_c)], axis=1).astype(np.float32)
    cos_c = np.ones((NCORES, TOK, 2 * q), np.float32)
    sin_c = np.zeros((NCORES, TOK, 2 * q), np.float32)
    for r in range(NCORES):
        cos_c[r, :2048] = cos[r * 2048:(r + 1) * 2048]
        sin_c[r, :2048] = sin[r * 2048:(r + 1) * 2048]
    return cos_c, sin_c


IDENT = np.eye(128, dtype=np.float32)
_PROG_CACHE = {}


def run(nc, in_maps):
    return run_bass_kernel_spmd(nc, in_maps, core_ids=list(range(NCORES))).results


def modT_layout(modv, l, cols):
    out = np.empty((128, 2, len(cols), 8), np.float32)
    for who in range(2):
        for j, c in enumerate(cols):
            out[:, who, j, :] = modv[l, who, c * 1024:(c + 1) * 1024].reshape(8, 128).T
    return out


def build_mod():
    if "mod" not in _PROG_CACHE:
        P = Prog()
        cc = P.inp("cc", [128, 8, 2], F32)
        wm = P.inp("wm", [DEPTH, D, 768], F32)
        bm = P.inp("bm", [DEPTH, 2, 768], F32)
        out = P.outp("modp", [DEPTH, 2, 768], F32)
        phase_mod(P, cc, wm, bm, out, 768)
        _PROG_CACHE["mod"] = P.close()
    return _PROG_CACHE["mod"]


def run_mod(c, c_ctx, w_mod, b_mod):
    nc = build_mod()
    cc = np.stack([c.reshape(128, 8), c_ctx.reshape(128, 8)], axis=-1).astype(np.float32)
    maps = []
    for r in range(NCORES):
        sl = slice(r * 768, (r + 1) * 768)
        maps.append({"ident": IDENT, "cc": cc, "wm": np.ascontiguousarray(w_mod[:, :, sl]),
                     "bm": np.ascontiguousarray(np.repeat(b_mod[:, None, sl], 2, axis=1))})
    res = run(nc, maps)
    return np.concatenate([res[r]["modp"] for r in range(NCORES)], axis=2)


def build_A(even):
    key = "A%d" % even
    if key not in _PROG_CACHE:
        P = Prog()
        ncols = E_COLS if even else O_COLS
        hq = 32 if even else 64
        x = P.inp("x", [TOK, D], F32)
        w = P.inp("w_in", [D, ncols], F32)
        mA = P.inp("mA", [128, 2, 2, 8], F32)
        cos = P.inp("cos", [TOK, hq], F32)
        sin = P.inp("sin", [TOK, hq], F32)
        qng = None if even else P.inp("qng", [2, 128], F32)
        qkv = P.outp("qkv", [TOK, ncols], BF16)
        phase_A(P, even, x, None, w, mA, cos, sin, qng, qkv)
        _PROG_CACHE[key] = P.close()
    return _PROG_CACHE[key]


def gather_tokens(parts, c0, c1):
    ctx = np.concatenate([parts[0][2048:, c0:c1], parts[1][2048:, c0:c1]], axis=0)
    lat = np.concatenate([p[:2048, c0:c1] for p in parts], axis=0)
    return np.concatenate([ctx, lat], axis=0)


def layout_odd(qkv):
    k_all = gather_tokens(qkv, 1024, 1280)
    v_all = gather_tokens(qkv, 1280, 1536)
    KT = np.ascontiguousarray(k_all.reshape(NKB * 128, 2, 128).transpose(1, 2, 0))
    V = np.ascontiguousarray(v_all.reshape(NKB, 128, 2, 128).transpose(2, 1, 0, 3))
    maps = []
    for r in range(NCORES):
        QT = np.ascontiguousarray(qkv[r][:, 0:1024].reshape(TOK, 8, 128).transpose(1, 2, 0))
        maps.append({"QT": QT, "KT": KT, "V": V})
    return maps


def window_masks():
    m = np.zeros((NCORES, 128, NT, 384), np.float32)
    jj = np.arange(128)[:, None]
    ii = np.arange(128)[None, :]
    left = (ii <= jj).astype(np.float32)
    right = (jj <= ii).astype(np.float32)
    for r in range(NCORES):
        for t in range(16):
            n = 16 * r + t
            if n - 1 >= 0:
                m[r, :, t, 0:128] = left
            m[r, :, t, 128:256] = 1.0
            if n + 1 < SEQ // 128:
                m[r, :, t, 256:384] = right
    return m.astype(NPBF)


def layout_even(qkv, sink, lam4, lam_init, subln_g):
    ak = gather_tokens(qkv, 512, 640)
    av = gather_tokens(qkv, 640, 768)
    bk = gather_tokens(qkv, 1280, 1792)
    bv = gather_tokens(qkv, 1792, 2304)
    KTb = np.ascontiguousarray(bk.reshape(NKB * 128, 4, 128).transpose(1, 2, 0))
    Vb = np.ascontiguousarray(bv.reshape(NKB, 128, 4, 128).transpose(2, 1, 0, 3))
    masks = window_masks()
    sinkp = np.empty((128, 4), np.float32)
    for c in range(4):
        sinkp[:64, c] = sink[2 * c]
        sinkp[64:, c] = sink[2 * c + 1]
    lami = np.empty((128, 2), np.float32)
    lami[:, 0] = lam_init
    lami[:, 1] = 1.0 - lam_init
    akb = ak.reshape(NKB, 128, 128)
    avb = av.reshape(NKB, 128, 2, 64)
    maps = []
    for r in range(NCORES):
        q = qkv[r]
        qb = q[:, 768:1280].reshape(TOK, 4, 2, 64).transpose(1, 2, 3, 0)
        QTb = np.zeros((4, 128, 2, TOK), q.dtype)
        QTb[:, 0:64, 0, :] = qb[:, 0]
        QTb[:, 64:128, 1, :] = qb[:, 1]
        QTa = np.ascontiguousarray(q[:, 0:512].reshape(TOK, 2, 4, 64).transpose(1, 3, 2, 0).reshape(128, 4, TOK))
        kw = np.zeros((20, 128, 128), ak.dtype)
        vw = np.zeros((20, 128, 2, 64), av.dtype)
        kw[0:2] = akb[0:2]
        vw[0:2] = avb[0:2]
        for w in range(2, 20):
            n = 16 * r - 1 + (w - 2)
            if 0 <= n < SEQ // 128:
                kw[w] = akb[2 + n]
                vw[w] = avb[2 + n]
        KTa = np.ascontiguousarray(kw.transpose(2, 0, 1).reshape(128, 20 * 128))
        Va = np.zeros((128, 4, 20, 128), av.dtype)
        for kvh in range(2):
            for lohi in range(2):
                Va[:, kvh * 2 + lohi, :, lohi * 64:lohi * 64 + 64] = vw[:, :, kvh, :].transpose(1, 0, 2)
        maps.append({"QTa": QTa, "KTa": KTa, "Va": Va, "mask": masks[r], "sinkp": sinkp, "QTb": QTb, "KTb": KTb, "Vb": Vb,
                     "lamv": np.ascontiguousarray(lam4.astype(np.float32)), "lami": lami,
                     "subg": np.ascontiguousarray(subln_g.reshape(128, 1).astype(np.float32))})
    return maps


def decl_B(P, even):
    if even:
        return dict(QTa=P.inp("QTa", [128, 4, TOK], BF16), KTa=P.inp("KTa", [128, 20 * 128], BF16), Va=P.inp("Va", [128, 4, 20, 128], BF16),
                    mask=P.inp("mask", [128, NT, 384], BF16), sinkp=P.inp("sinkp", [128, 4], F32), QTb=P.inp("QTb", [4, 128, 2, TOK], BF16),
                    KTb=P.inp("KTb", [4, 128, NKB * 128], BF16), Vb=P.inp("Vb", [4, 128, NKB, 128], BF16), lamv=P.inp("lamv", [4, 64], F32),
                    lami=P.inp("lami", [128, 2], F32), subg=P.inp("subg", [128, 1], F32))
    return dict(QT=P.inp("QT", [8, 128, TOK], BF16), KT=P.inp("KT", [2, 128, NKB * 128], BF16), V=P.inp("V", [2, 128, NKB, 128], BF16))


def emit_B(P, even, OT, b_OT, d):
    if even:
        phase_B_even(P, OT, b_OT, d["QTa"], d["KTa"], d["Va"], d["mask"], d["sinkp"], d["QTb"], d["KTb"], d["Vb"], d["lamv"], d["lami"], d["subg"])
    else:
        phase_B_odd(P, OT, b_OT, d["QT"], d["KT"], d["V"])


def build_Btest(even):
    key = "Bt%d" % even
    if key not in _PROG_CACHE:
        P = Prog()
        d = decl_B(P, even)
        out = P.outp("OT", [128, 8, TOK], BF16)
        OT = P.sb(P.es, [128, 8, TOK], BF16, "OT")
        b_OT = Buf("OT")
        emit_B(P, even, OT, b_OT, d)
        P.T.dma("pool", out[:, :, :], OT[:], reads=[b_OT])
        _PROG_CACHE[key] = P.close()
    return _PROG_CACHE[key]


def build_BCA(even, has_A):
    key = "BCA%d%d" % (even, has_A)
    if key in _PROG_CACHE:
        return _PROG_CACHE[key]
    P = Prog()
    nc = P.nc
    dB = decl_B(P, even)
    x = P.inp("x", [TOK, D], F32)
    wo = P.inp("w_out", [D, D], F32)
    gbc = P.inp("gbc", [2, 2, D], F32)
    mC = P.inp("mC", [128, 2, 2, 8], F32)
    lng = P.inp("lng", [2, D], F32)
    lnb = P.inp("lnb", [2, D], F32)
    wr = P.inp("wr", [D, NEXP], F32)
    br = P.inp("br", [1, NEXP], F32)
    wg = P.inp("wg", [NEXP, D, DEXP], F32)
    wu = P.inp("wu", [NEXP, D, DEXP], F32)
    wd = P.inp("wd", [NEXP, DEXP, D], F32)
    x1s = nc.dram_tensor("x1s", [TOK, D], F32, kind="Internal").ap()
    xo = P.outp("xo", [TOK, D], F32)
    x1b = [Buf("x1s%d" % t) for t in range(NT)]
    xob = [Buf("xo%d" % t) for t in range(NT)]
    if has_A:
        a_even = not even
        ncols = E_COLS if a_even else O_COLS
        hq = 32 if a_even else 64
        w_in = P.inp("w_in", [D, ncols], F32)
        mA = P.inp("mA", [128, 2, 2, 8], F32)
        cos = P.inp("cos", [TOK, hq], F32)
        sin = P.inp("sin", [TOK, hq], F32)
        qng = None if a_even else P.inp("qng", [2, 128], F32)
        qkv = P.outp("qkv", [TOK, ncols], BF16)
    with ExitStack() as es:
        OT = P.sb(es, [128, 8, TOK], BF16, "OT")
        b_OT = Buf("OT")
        emit_B(P, even, OT, b_OT, dB)
        phase_C1(P, OT, b_OT, x, wo, gbc, lng, lnb, x1s, x1b)
    if "noC2" not in os.environ.get("K_DBG", ""):
        phase_C2(P, x1s, x1b, mC, gbc, lng, lnb, wr, br, wg, wu, wd, xo, xob)
    if has_A and "noA" not in os.environ.get("K_DBG", ""):
        phase_A(P, a_even, xo, xob, w_in, mA, cos, sin, qng, qkv)
    print("BCA program: %d instructions, %d waits, %d dma sems" % (P.T.n_ins, P.T.n_wait, P.T.nsem), flush=True)
    _PROG_CACHE[key] = P.close()
    return _PROG_CACHE[key]


def a_inputs(inputs, modv, l, r, tabs):
    even = (l % 2 == 0)
    i = l // 2
    cos_c, sin_c = tabs[64 if even else 128]
    m = {"w_in": inputs["w_in_even"][i] if even else inputs["w_in_odd"][i], "mA": modT_layout(modv, l, [1, 0]),
         "cos": cos_c[r], "sin": sin_c[r]}
    if not even:
        m["qng"] = np.stack([inputs["q_norm_g"][i], inputs["k_norm_g"][i]]).astype(np.float32)
    return m


def kernel(**inputs):
    inputs = {k: np.asarray(v) for k, v in inputs.items()}
    x = inputs["x"][0]
    ctx = inputs["ctx"][0]
    tabs = {64: rope_tables(64), 128: rope_tables(128)}
    modv = run_mod(inputs["c"][0], inputs["c_ctx"], inputs["w_mod"], inputs["b_mod"])
    xres = [np.ascontiguousarray(np.concatenate([x[r * 2048:(r + 1) * 2048], ctx[(r % 2) * 128:(r % 2) * 128 + 128]], 0)) for r in range(NCORES)]
    maps = []
    for r in range(NCORES):
        m = a_inputs(inputs, modv, 0, r, tabs)
        m.update({"ident": IDENT, "x": xres[r]})
        maps.append(m)
    res = run(build_A(True), maps)
    qkv = [res[r]["qkv"] for r in range(NCORES)]
    for l in range(DEPTH):
        even = (l % 2 == 0)
        i = l // 2
        has_A = l < DEPTH - 1
        if even:
            lam_init = 0.8 - 0.6 * float(np.exp(-0.3 * l))
            lam4 = np.stack([inputs["lam_q1"][i], inputs["lam_k1"][i], inputs["lam_q2"][i], inputs["lam_k2"][i]])
            maps = layout_even(qkv, inputs["sink_logits"][i], lam4, lam_init, inputs["subln_g"][i])
        else:
            maps = layout_odd(qkv)
        gbc = np.ascontiguousarray(np.stack([np.stack([modv[l, who, 2048:3072], modv[l, who, 5120:6144]]) for who in range(2)]))
        mC = modT_layout(modv, l, [4, 3])
        for r in range(NCORES):
            m = maps[r]
            m.update({"ident": IDENT, "x": xres[r], "w_out": inputs["w_out_even"][i] if even else inputs["w_out_odd"][i], "gbc": gbc, "mC": mC,
                      "lng": inputs["ln_g"][l], "lnb": inputs["ln_b"][l], "wr": inputs["w_router"], "br": inputs["b_router"].reshape(1, NEXP),
                      "wg": inputs["w_gate"][l], "wu": inputs["w_up"][l], "wd": inputs["w_down"][l]})
            if has_A:
                m.update(a_inputs(inputs, modv, l + 1, r, tabs))
        res = run(build_BCA(even, has_A), maps)
        xres = [res[r]["xo"] for r in range(NCORES)]
        if has_A:
            qkv = [res[r]["qkv"] for r in range(NCORES)]
    out = np.concatenate([xres[r][:2048] for r in range(NCORES)], axis=0)
    return out.reshape(1, SEQ, D).astype(np.float32)
```

```python
import os
import numpy as np
import ml_dtypes
from contextlib import ExitStack
import concourse.bass as bass
import concourse.mybir as mybir
from concourse.bass_utils import run_bass_kernel_spmd

F32 = mybir.dt.float32
BF16 = mybir.dt.bfloat16
AF = mybir.ActivationFunctionType
ALU = mybir.AluOpType
AX = mybir.AxisListType
NPBF = ml_dtypes.bfloat16

NCORES = 8
D = 1024
SEQ = 16384
CTX = 256
NT = 17
TOK = NT * 128
NKB = (SEQ + CTX) // 128
DEPTH = 4
ALPHA = (2 * DEPTH) ** 0.25
LN_EPS = 1e-5
QK_EPS = 1e-6
SUBLN_EPS = 1e-5
E_COLS = 2304
O_COLS = 1536
NEXP = 16
DEXP = 512
CHUNKS = [(0, 512), (512, 512), (1024, 512), (1536, 512), (2048, 128)]


class Buf:
    __slots__ = ("name", "w", "rs", "dsem", "dcnt", "excl")

    def __init__(self, name, excl=False):
        self.name = name
        self.w = None
        self.rs = {}
        self.dsem = None
        self.dcnt = 0
        self.excl = excl


class Trk:
    def __init__(self, nc, es):
        self.nc = nc
        self.es = es
        self.eng = {"pe": nc.tensor, "act": nc.scalar, "dve": nc.vector, "pool": nc.gpsimd, "sp": nc.sync}
        self.sem = {}
        self.cnt = {}
        self.waited = {k: {} for k in self.eng}
        self.nsem = 0
        self.dbufs = []
        for k in self.eng:
            self.sem[k] = es.enter_context(nc.semaphore("e_" + k))
            self.cnt[k] = 0
        self.n_ins = 0
        self.n_wait = 0

    def _deps(self, reads, writes, e=None):
        deps = {}
        own = self.sem.get(e)

        def add(t):
            if t is None:
                return
            k = id(t[0])
            if k not in deps or deps[k][1] < t[1]:
                deps[k] = t
        for b in reads:
            add(b.w)
            if b.excl:
                for t in b.rs.values():
                    if t[0] is not own:
                        add(t)
        for b in writes:
            add(b.w)
            for t in b.rs.values():
                add(t)
        return deps

    def _emit_waits(self, e, deps):
        own = self.sem.get(e)
        eo = self.eng[e]
        wd = self.waited[e]
        for k, (s, v) in deps.items():
            if s is own and e in ("pe", "sp"):
                continue
            if wd.get(k, 0) >= v:
                continue
            eo.wait_ge(s, v)
            wd[k] = v
            self.n_wait += 1

    def _commit(self, t, reads, writes):
        k = id(t[0])
        for b in reads:
            b.rs[k] = t
        for b in writes:
            b.w = t
            b.rs = {}

    def op(self, e, fn, reads=(), writes=()):
        self._emit_waits(e, self._deps(reads, writes, e))
        ins = fn()
        self.cnt[e] += 1
        ins.then_inc(self.sem[e], 1)
        self._commit((self.sem[e], self.cnt[e]), reads, writes)
        self.n_ins += 1
        return ins

    def dma(self, q, out, in_, reads=(), writes=(), **kw):
        self._emit_waits(q, self._deps(reads, writes, q))
        owner = writes[0] if len(writes) else reads[0]
        if owner.dsem is None:
            owner.dsem = self.es.enter_context(self.nc.semaphore("d_%d" % self.nsem))
            self.nsem += 1
            self.dbufs.append(owner)
        ins = self.eng[q].dma_start(out=out, in_=in_, **kw)
        owner.dcnt += 16
        ins.then_inc(owner.dsem, 16)
        self._commit((owner.dsem, owner.dcnt), reads, writes)
        self.n_ins += 1
        return ins

    def barrier(self):
        deps = {}
        for k in self.eng:
            if self.cnt[k]:
                deps[id(self.sem[k])] = (self.sem[k], self.cnt[k])
        for b in self.dbufs:
            deps[id(b.dsem)] = (b.dsem, b.dcnt)
        for e in self.eng:
            own = self.sem[e]
            eo = self.eng[e]
            wd = self.waited[e]
            for k, (s, v) in deps.items():
                if s is own or wd.get(k, 0) >= v:
                    continue
                eo.wait_ge(s, v)
                wd[k] = v

    def finish(self, e="pool"):
        deps = {}
        for b in self.dbufs:
            deps[id(b.dsem)] = (b.dsem, b.dcnt)
        for k in self.eng:
            if self.cnt[k]:
                deps[id(self.sem[k])] = (self.sem[k], self.cnt[k])
        self._emit_waits(e, deps)


class Prog:
    def __init__(self):
        self.nc = bass.Bass("TRN2", target_bir_lowering=False)
        self.es = ExitStack()
        self.T = Trk(self.nc, self.es)
        self.pbig = [self.es.enter_context(self.nc.psum_tensor("pbig%d" % i, [128, 1024], F32)) for i in range(4)]
        self.banks = [self.pbig[i // 2][:, (i % 2) * 512:(i % 2 + 1) * 512] for i in range(8)]
        self.bb = [Buf("bank%d" % i, excl=True) for i in range(8)]
        self.uid = 0
        nc, T = self.nc, self.T
        self.ident_d = self.inp("ident", [128, 128], F32)
        self.ident = self.sb(self.es, [128, 128], F32)
        self.b_ident = Buf("ident")
        T.dma("sp", self.ident[:], self.ident_d[:, :], writes=[self.b_ident])
        self.ones = self.sb(self.es, [128, 128], BF16)
        self.b_ones = Buf("ones")
        T.op("pool", lambda: nc.gpsimd.memset(self.ones[:], 1.0), writes=[self.b_ones])
        self.onesd = self.sb(self.es, [128, 128], F32)
        self.b_onesd = Buf("onesd")
        T.op("pool", lambda: nc.gpsimd.memset(self.onesd[:], 1.0 / 128.0), writes=[self.b_onesd])

    def inp(self, name, shape, dt):
        return self.nc.dram_tensor(name, list(shape), dt, kind="ExternalInput").ap()

    def outp(self, name, shape, dt):
        return self.nc.dram_tensor(name, list(shape), dt, kind="ExternalOutput").ap()

    def sb(self, es, shape, dt, name=None):
        self.uid += 1
        return es.enter_context(self.nc.sbuf_tensor("%s_%d" % (name or "t", self.uid), list(shape), dt))

    def close(self):
        self.T.finish("pool")
        self.es.close()
        return self.nc


def bcast_rows(ap_row, n):
    return ap_row.partition_broadcast(n)


def phase_mod(P, cc_d, wm_d, bm_d, out_d, ncols):
    nc, T = P.nc, P.T
    with ExitStack() as es:
        cc = P.sb(es, [128, 8, 2], F32)
        b_cc = Buf("cc")
        T.dma("sp", cc[:], cc_d[:, :, :], writes=[b_cc])
        ccs = P.sb(es, [128, 8, 2], F32)
        b_ccs = Buf("ccs")
        T.op("act", lambda: nc.scalar.activation(out=ccs[:], in_=cc[:], func=AF.Silu), reads=[b_cc], writes=[b_ccs])
        CW = 384
        stg = [P.sb(es, [128, 8, CW], F32) for _ in range(2)]
        b_stg = [Buf("stg%d" % i) for i in range(2)]
        bt = [P.sb(es, [2, CW], F32) for _ in range(2)]
        b_bt = [Buf("bt%d" % i) for i in range(2)]
        rs = [P.sb(es, [2, CW], F32) for _ in range(2)]
        b_rs = [Buf("rs%d" % i) for i in range(2)]
        it = 0
        for l in range(DEPTH):
            for c0 in range(0, ncols, CW):
                i = it % 2
                it += 1
                T.dma("sp", stg[i][:], wm_d[l, :, c0:c0 + CW].rearrange("(p k) n -> p k n", k=8), writes=[b_stg[i]])
                T.dma("sp", bt[i][:], bm_d[l, :, c0:c0 + CW], writes=[b_bt[i]])
                ps = P.banks[i]
                for k in range(8):
                    T.op("pe", lambda: nc.tensor.matmul(ps[0:2, 0:CW], lhsT=ccs[:, k, :], rhs=stg[i][:, k, :], start=(k == 0), stop=(k == 7)),
                         reads=[b_ccs, b_stg[i]], writes=[P.bb[i]])
                T.op("dve", lambda: nc.vector.tensor_tensor(out=rs[i][:], in0=ps[0:2, 0:CW], in1=bt[i][:], op=ALU.add),
                     reads=[P.bb[i], b_bt[i]], writes=[b_rs[i]])
                T.dma("pool", out_d[l, :, c0:c0 + CW], rs[i][:], reads=[b_rs[i]])
    T.barrier()


def load_weight_bf16(P, es, src, K, ncols, name):
    nc, T = P.nc, P.T
    dst = P.sb(es, [128, K, ncols], BF16, name)
    CW = 4096 // K
    stg = [P.sb(es, [128, K, CW], F32, name + "s") for _ in range(2)]
    b_stg = [Buf(name + "s%d" % i) for i in range(2)]
    bufs = {}
    it = 0
    for c0 in range(0, ncols, CW):
        cw = min(CW, ncols - c0)
        i = it % 2
        it += 1
        T.dma("sp", stg[i][:, :, 0:cw], src[:, c0:c0 + cw].rearrange("(k p) n -> p k n", p=128), writes=[b_stg[i]])
        b = Buf(name + "_c%d" % c0)
        T.op("act", lambda: nc.scalar.copy(out=dst[:, :, c0:c0 + cw], in_=stg[i][:, :, 0:cw]), reads=[b_stg[i]], writes=[b])
        for c in range(c0, c0 + cw, 128):
            bufs[c // 128] = b
    return dst, bufs


def rope_tok(P, src, dst, nh, hd, cos, sin, b_src, b_dst, b_tab, tmp, b_tmp):
    nc, T = P.nc, P.T
    q = hd // 4
    sv = src.rearrange("p (h a j i) -> p h a j i", h=nh, a=2, j=2, i=q)
    dv = dst.rearrange("p (h a j i) -> p h a j i", h=nh, a=2, j=2, i=q)
    x1 = sv[:, :, :, 0, :]
    x2 = sv[:, :, :, 1, :]
    cb = cos.rearrange("p (a i) -> p a i", a=2).unsqueeze(1).to_broadcast([128, nh, 2, q])
    sbc = sin.rearrange("p (a i) -> p a i", a=2).unsqueeze(1).to_broadcast([128, nh, 2, q])
    n = nh * 2 * q
    t = [tmp[j][:, 0:n].rearrange("p (h a i) -> p h a i", h=nh, a=2, i=q) for j in range(4)]
    T.op("dve", lambda: nc.vector.tensor_tensor(out=t[0], in0=x1, in1=cb, op=ALU.mult), reads=[b_src, b_tab], writes=[b_tmp[0]])
    T.op("dve", lambda: nc.vector.tensor_tensor(out=t[1], in0=x2, in1=sbc, op=ALU.mult), reads=[b_src, b_tab], writes=[b_tmp[1]])
    T.op("dve", lambda: nc.vector.tensor_tensor(out=t[2], in0=x1, in1=sbc, op=ALU.mult), reads=[b_src, b_tab], writes=[b_tmp[2]])
    T.op("dve", lambda: nc.vector.tensor_tensor(out=t[3], in0=x2, in1=cb, op=ALU.mult), reads=[b_src, b_tab], writes=[b_tmp[3]])
    T.op("pool", lambda: nc.gpsimd.tensor_tensor(out=dv[:, :, :, 0, :], in0=t[0], in1=t[1], op=ALU.subtract),
         reads=[b_tmp[0], b_tmp[1]], writes=[b_dst])
    T.op("pool", lambda: nc.gpsimd.tensor_tensor(out=dv[:, :, :, 1, :], in0=t[2], in1=t[3], op=ALU.add),
         reads=[b_tmp[2], b_tmp[3]], writes=[b_dst])


def transpose_mod(P, xt, b_xt, scT, shT, b_mod, hT, b_hT, tb, hTf=None, b_hTf=None):
    nc, T = P.nc, P.T
    for k in range(8):
        bk = tb[k // 4]
        T.op("pe", lambda: nc.tensor.transpose(P.banks[bk][:, (k % 4) * 128:(k % 4 + 1) * 128], xt[:, k * 128:(k + 1) * 128], P.ident[:]),
             reads=[b_xt, P.b_ident], writes=[P.bb[bk]])
    for k in range(8):
        bk = tb[k // 4]
        src = P.banks[bk][:, (k % 4) * 128:(k % 4 + 1) * 128]
        if hTf is None:
            T.op("act", lambda: nc.scalar.activation(out=hT[:, k, :], in_=src, func=AF.Identity, bias=shT[:, k:k + 1], scale=scT[:, k:k + 1]),
                 reads=[P.bb[bk], b_mod], writes=[b_hT])
        else:
            T.op("act", lambda: nc.scalar.activation(out=hTf[:, k, :], in_=src, func=AF.Identity, bias=shT[:, k:k + 1], scale=scT[:, k:k + 1]),
                 reads=[P.bb[bk], b_mod], writes=[b_hTf])
    if hTf is not None:
        T.op("dve", lambda: nc.vector.tensor_copy(out=hT, in_=hTf[:]), reads=[b_hTf], writes=[b_hT])


def load_modT(P, es, m_d, name):
    nc, T = P.nc, P.T
    m = P.sb(es, [128, 2, 2, 8], F32, name)
    b = Buf(name)
    T.dma("sp", m[:], m_d[:, :, :, :], writes=[b])
    T.op("dve", lambda: nc.vector.tensor_scalar(out=m[:, :, 0, :], in0=m[:, :, 0, :], scalar1=1.0, scalar2=None, op0=ALU.add),
         reads=[b], writes=[b])
    return m, b


def phase_A(P, even, x_d, x_bufs, w_d, mA_d, cos_d, sin_d, qng_d, qkv_d):
    nc, T = P.nc, P.T
    ncols = E_COLS if even else O_COLS
    hd = 64 if even else 128
    hq = hd // 2
    with ExitStack() as es:
        W, wb = load_weight_bf16(P, es, w_d, 8, ncols, "win")
        m, b_m = load_modT(P, es, mA_d, "mA")
        xt = [P.sb(es, [128, D], F32, "xt") for _ in range(2)]
        b_xt = [Buf("xt%d" % i) for i in range(2)]
        hT = [P.sb(es, [128, 8, 128], BF16, "hT") for _ in range(2)]
        b_hT = [Buf("hT%d" % i) for i in range(2)]
        ot = [P.sb(es, [128, ncols], BF16, "ot") for _ in range(2)]
        b_ot = [Buf("ot%d" % i) for i in range(2)]
        cs = [P.sb(es, [128, 2, hq], F32, "cs") for _ in range(2)]
        b_cs = [Buf("cs%d" % i) for i in range(2)]
        tmp = [P.sb(es, [128, 512], F32, "rt") for _ in range(4)]
        b_tmp = [Buf("rt%d" % i) for i in range(4)]
        if not even:
            gq = P.sb(es, [128, 2, 128], F32, "gq")
            b_gq = Buf("gq")
            T.dma("sp", gq[:, 0, :], qng_d[0:1, :].partition_broadcast(128), writes=[b_gq])
            T.dma("sp", gq[:, 1, :], qng_d[1:2, :].partition_broadcast(128), writes=[b_gq])
            epsq = P.sb(es, [128, 1], F32, "epsq")
            b_epsq = Buf("epsq")
            T.op("pool", lambda: nc.gpsimd.memset(epsq[:], QK_EPS), writes=[b_epsq])
            sq = P.sb(es, [128, 512], F32, "sq")
            b_sq = Buf("sq")
            ssq = P.sb(es, [128, 4], F32, "ssq")
            b_ssq = Buf("ssq")
            xn = [P.sb(es, [128, 512], F32, "xn") for _ in range(2)]
            b_xn = [Buf("xn%d" % i) for i in range(2)]
        nbk = (ncols + 511) // 512

        def stage1(t):
            i = t % 2
            who = 1 if t == NT - 1 else 0
            rd = [x_bufs[t]] if x_bufs is not None else []
            T.dma("sp", xt[i][:], x_d[t * 128:(t + 1) * 128, :], reads=rd, writes=[b_xt[i]])
            transpose_mod(P, xt[i], b_xt[i], m[:, who, 0, :], m[:, who, 1, :], b_m, hT[i], b_hT[i], (6, 7))

        stage1(0)
        for t in range(NT):
            i = t % 2
            T.dma("sp", cs[i][:, 0, :], cos_d[t * 128:(t + 1) * 128, :], writes=[b_cs[i]])
            T.dma("sp", cs[i][:, 1, :], sin_d[t * 128:(t + 1) * 128, :], writes=[b_cs[i]])
            if t + 1 < NT:
                stage1(t + 1)
            for c in range(nbk):
                cw = min(512, ncols - c * 512)
                for k in range(8):
                    T.op("pe", lambda: nc.tensor.matmul(P.banks[c][:, 0:cw], lhsT=hT[i][:, k, :], rhs=W[:, k, c * 512:c * 512 + cw],
                                                        start=(k == 0), stop=(k == 7)),
                         reads=[b_hT[i], wb[c * 4]] + ([wb[c * 4 + 3]] if cw == 512 else []), writes=[P.bb[c]])
            o = ot[i]
            cosv, sinv = cs[i][:, 0, :], cs[i][:, 1, :]
            if even:
                def rp(bank, c0, c1):
                    rope_tok(P, P.banks[bank][:, c0 - bank * 512:c1 - bank * 512], o[:, c0:c1], (c1 - c0) // 64, 64, cosv, sinv,
                             P.bb[bank], b_ot[i], b_cs[i], tmp, b_tmp)
                rp(0, 0, 512)
                rp(1, 512, 640)
                rp(1, 768, 1024)
                rp(2, 1024, 1536)
                rp(3, 1536, 1792)
                T.op("act", lambda: nc.scalar.copy(out=o[:, 640:768], in_=P.banks[1][:, 128:256]), reads=[P.bb[1]], writes=[b_ot[i]])
                T.op("act", lambda: nc.scalar.copy(out=o[:, 1792:2048], in_=P.banks[3][:, 256:512]), reads=[P.bb[3]], writes=[b_ot[i]])
                T.op("act", lambda: nc.scalar.copy(out=o[:, 2048:2304], in_=P.banks[4][:, 0:256]), reads=[P.bb[4]], writes=[b_ot[i]])
            else:
                for bank, nh, gi in ((0, 4, 0), (1, 4, 0), (2, 2, 1)):
                    n = nh * 128
                    src = P.banks[bank][:, 0:n]
                    T.op("act", lambda: nc.scalar.activation(out=sq[:, 0:n], in_=src, func=AF.Square), reads=[P.bb[bank]], writes=[b_sq])
                    T.op("dve", lambda: nc.vector.tensor_reduce(out=ssq[:, 0:nh], in_=sq[:, 0:n].rearrange("p (h d) -> p h d", h=nh),
                                                                axis=AX.X, op=ALU.add), reads=[b_sq], writes=[b_ssq])
                    T.op("act", lambda: nc.scalar.activation(out=ssq[:, 0:nh], in_=ssq[:, 0:nh], func=AF.Sqrt, bias=epsq[:, 0:1], scale=1.0 / 128.0),
                         reads=[b_ssq, b_epsq], writes=[b_ssq])
                    T.op("dve", lambda: nc.vector.reciprocal(out=ssq[:, 0:nh], in_=ssq[:, 0:nh]), reads=[b_ssq], writes=[b_ssq])
                    j = bank % 2
                    T.op("dve", lambda: nc.vector.tensor_tensor(out=xn[j][:, 0:n].rearrange("p (h d) -> p h d", h=nh),
                                                                in0=src.rearrange("p (h d) -> p h d", h=nh),
                                                                in1=ssq[:, 0:nh].unsqueeze(2).to_broadcast([128, nh, 128]), op=ALU.mult),
                         reads=[P.bb[bank], b_ssq], writes=[b_xn[j]])
                    T.op("pool", lambda: nc.gpsimd.tensor_tensor(out=xn[j][:, 0:n].rearrange("p (h d) -> p h d", h=nh),
                                                                 in0=xn[j][:, 0:n].rearrange("p (h d) -> p h d", h=nh),
                                                                 in1=gq[:, gi, :].unsqueeze(1).to_broadcast([128, nh, 128]), op=ALU.mult),
                         reads=[b_xn[j], b_gq], writes=[b_xn[j]])
                    rope_tok(P, xn[j][:, 0:n], o[:, bank * 512:bank * 512 + n], nh, 128, cosv, sinv, b_xn[j], b_ot[i], b_cs[i], tmp, b_tmp)
                T.op("act", lambda: nc.scalar.copy(out=o[:, 1280:1536], in_=P.banks[2][:, 256:512]), reads=[P.bb[2]], writes=[b_ot[i]])
            T.dma("pool", qkv_d[t * 128:(t + 1) * 128, :], o[:], reads=[b_ot[i]])
    T.barrier()


class SweepCtx:
    def __init__(self, P, es, npt=5):
        self.P = P
        self.pT = [P.sb(es, [128, 2, 512], BF16, "pT") for _ in range(npt)]
        self.b_pT = [Buf("pT%d" % i) for i in range(npt)]
        self.n = 0
        self.ps = [P.sb(es, [128, 512], BF16, "psum2") for _ in range(4)]
        self.b_ps = [Buf("psum2_%d" % i) for i in range(4)]
        self.m = 0


def sweep(P, S, N, blocks, qT, b_q, kT_of, v_of, b_kv, M, scale):
    nc, T = P.nc, P.T
    SK = 2
    ob, sb_ = 6, 7
    nb = len(blocks)
    assert nb % 2 == 0
    npair = nb // 2
    idx = []
    pidx = []
    for u in range(npair + SK):
        if u < npair:
            n = S.n
            S.n += 1
            idx.append(n)
            sp = n % 3
            p = n % len(S.pT)
            for b in range(2):
                bk = 2 * sp + b
                T.op("pe", lambda: nc.tensor.matmul(P.banks[bk][:, 0:N], lhsT=kT_of(blocks[2 * u + b]), rhs=qT, start=True, stop=True),
                     reads=[b_kv, b_q], writes=[P.bb[bk]])
            src = P.pbig[sp][:, :].rearrange("p (b n) -> p b n", b=2)[:, :, 0:N]
            T.op("act", lambda: nc.scalar.activation(out=S.pT[p][:, :, 0:N], in_=src, func=AF.Exp, scale=scale),
                 reads=[P.bb[2 * sp], P.bb[2 * sp + 1]], writes=[S.b_pT[p]])
        if 1 <= u <= npair:
            p = idx[u - 1] % len(S.pT)
            q = S.m % len(S.ps)
            S.m += 1
            pidx.append(q)
            T.op("dve", lambda: nc.vector.tensor_tensor(out=S.ps[q][:, 0:N], in0=S.pT[p][:, 0, 0:N], in1=S.pT[p][:, 1, 0:N], op=ALU.add),
                 reads=[S.b_pT[p]], writes=[S.b_ps[q]])
        if u >= SK:
            v = u - SK
            p = idx[v] % len(S.pT)
            for b in range(2):
                j = 2 * v + b
                T.op("pe", lambda: nc.tensor.matmul(P.banks[ob][0:M, 0:N], lhsT=v_of(blocks[j]), rhs=S.pT[p][:, b, 0:N], start=(j == 0), stop=(j == nb - 1)),
                     reads=[b_kv, S.b_pT[p]], writes=[P.bb[ob]])
            q = pidx[v]
            T.op("pe", lambda: nc.tensor.matmul(P.banks[sb_][:, 0:N], lhsT=P.ones[:], rhs=S.ps[q][:, 0:N], start=(v == 0), stop=(v == npair - 1)),
                 reads=[P.b_ones, S.b_ps[q]], writes=[P.bb[sb_]])
    return ob, sb_


ALL_BLOCKS = list(range(NKB))
CTX_BLOCKS = [0, 1]


def phase_B_odd(P, OT, b_OT, QT_d, KT_d, V_d):
    nc, T = P.nc, P.T
    scale = 128.0 ** -0.5
    with ExitStack() as es:
        S = SweepCtx(P, es)
        kts = [P.sb(es, [128, NKB * 128], BF16, "kt") for _ in range(2)]
        vts = [P.sb(es, [128, NKB, 128], BF16, "vt") for _ in range(2)]
        b_kvs = [Buf("kv%d" % i) for i in range(2)]
        qt = [P.sb(es, [128, TOK], BF16, "qt") for _ in range(2)]
        b_qt = [Buf("qt%d" % i) for i in range(2)]
        rec = [P.sb(es, [128, 512], F32, "rec") for _ in range(2)]
        b_rec = [Buf("rec%d" % i) for i in range(2)]
        fi = 0
        for kvh in range(2):
            kt, vt, b_kv = kts[kvh], vts[kvh], b_kvs[kvh]
            T.dma("sp", kt[:], KT_d[kvh, :, :], writes=[b_kv])
            T.dma("sp", vt[:], V_d[kvh, :, :, :], writes=[b_kv])
        for kvh in range(2):
            kt, vt, b_kv = kts[kvh], vts[kvh], b_kvs[kvh]
            for g in range(4):
                h = kvh * 4 + g
                qi = h % 2
                T.dma("sp", qt[qi][:], QT_d[h, :, :], writes=[b_qt[qi]])
                for (c0, N) in CHUNKS:
                    blocks = ALL_BLOCKS if N == 512 else CTX_BLOCKS
                    ksl = slice(64, 128) if "k64" in os.environ.get("K_DBG", "") else slice(0, 128)
                    ob, sbk = sweep(P, S, N, blocks, qt[qi][ksl, c0:c0 + N], b_qt[qi],
                                    lambda b: kt[ksl, b * 128:(b + 1) * 128], lambda b: vt[:, b, :], b_kv, 128, scale)
                    r = fi % 2
                    fi += 1
                    T.op("dve", lambda: nc.vector.reciprocal(out=rec[r][:, 0:N], in_=P.banks[sbk][:, 0:N]), reads=[P.bb[sbk]], writes=[b_rec[r]])
                    T.op("dve", lambda: nc.vector.tensor_tensor(out=OT[:, h, c0:c0 + N], in0=P.banks[ob][:, 0:N], in1=rec[r][:, 0:N], op=ALU.mult),
                         reads=[P.bb[ob], b_rec[r]], writes=[b_OT])
    T.barrier()


def phase_B_even(P, OT, b_OT, QTa_d, KTa_d, Va_d, mask_d, sinkp_d, QTb_d, KTb_d, Vb_d, lamv_d, lami_d, subg_d):
    nc, T = P.nc, P.T
    sc = 64.0 ** -0.5
    with ExitStack() as es:
        if "nomA" in os.environ.get("K_DBG", ""):
            es.close()
            return phase_B_even_mixB(P, OT, b_OT, QTb_d, KTb_d, Vb_d, lamv_d, lami_d, subg_d)
        qta = P.sb(es, [128, 4, TOK], BF16, "qta")
        b_qta = Buf("qta")
        T.dma("sp", qta[:], QTa_d[:, :, :], writes=[b_qta])
        ktaw = P.sb(es, [128, 20 * 128], BF16, "ktaw")
        b_kta = Buf("ktaw")
        T.dma("sp", ktaw[:], KTa_d[:, :], writes=[b_kta])
        vaw = P.sb(es, [128, 4, 20, 128], BF16, "vaw")
        b_vaw = Buf("vaw")
        T.dma("sp", vaw[:], Va_d[:, :, :, :], writes=[b_vaw])
        msk = P.sb(es, [128, NT, 384], BF16, "msk")
        b_msk = Buf("msk")
        T.dma("sp", msk[:], mask_d[:, :, :], writes=[b_msk])
        esink = P.sb(es, [128, 4], F32, "esink")
        b_es = Buf("esink")
        T.dma("sp", esink[:], sinkp_d[:, :], writes=[b_es])
        T.op("act", lambda: nc.scalar.activation(out=esink[:], in_=esink[:], func=AF.Exp), reads=[b_es], writes=[b_es])
        oneslh = P.sb(es, [128, 2, 128], BF16, "oneslh")
        b_olh = Buf("oneslh")
        T.op("pool", lambda: nc.gpsimd.memset(oneslh[:], 0.0), writes=[b_olh])
        T.op("pool", lambda: nc.gpsimd.memset(oneslh[:, 0, 0:64], 1.0), reads=[b_olh], writes=[b_olh])
        T.op("pool", lambda: nc.gpsimd.memset(oneslh[:, 1, 64:128], 1.0), reads=[b_olh], writes=[b_olh])
        pTa = [P.sb(es, [128, 640], BF16, "pTa") for _ in range(4)]
        b_pTa = [Buf("pTa%d" % i) for i in range(4)]
        den = [P.sb(es, [128, 128], F32, "den") for _ in range(2)]
        b_den = [Buf("den%d" % i) for i in range(2)]
        items = [(c, t, hh) for c in range(4) for t in range(NT) for hh in range(2)]

        def blocks_of(t):
            wl = (t + 2) if t < NT - 1 else 2
            return [0, 1, wl, wl + 1, wl + 2]

        def front(i):
            c, t, hh = items[i]
            kvh = c // 2
            ksl = slice(kvh * 64, kvh * 64 + 64)
            j = (2 * c + hh) % 4
            p = i % 4
            s0, s1 = (0, 1) if i % 2 == 0 else (2, 3)
            q_ap = qta[ksl, j, t * 128:(t + 1) * 128]
            for bi, w in enumerate(blocks_of(t)):
                bk, col = (s0, bi * 128) if bi < 2 else (s1, (bi - 2) * 128)
                T.op("pe", lambda: nc.tensor.matmul(P.banks[bk][:, col:col + 128], lhsT=ktaw[ksl, w * 128:(w + 1) * 128], rhs=q_ap,
                                                    start=True, stop=True), reads=[b_kta, b_qta], writes=[P.bb[bk]])
            T.op("act", lambda: nc.scalar.activation(out=pTa[p][:, 0:256], in_=P.banks[s0][:, 0:256], func=AF.Exp, scale=sc),
                 reads=[P.bb[s0]], writes=[b_pTa[p]])
            T.op("act", lambda: nc.scalar.activation(out=pTa[p][:, 256:640], in_=P.banks[s1][:, 0:384], func=AF.Exp, scale=sc),
                 reads=[P.bb[s1]], writes=[b_pTa[p]])
            T.op("pool", lambda: nc.gpsimd.tensor_tensor(out=pTa[p][:, 256:640], in0=pTa[p][:, 256:640], in1=msk[:, t, :], op=ALU.mult),
                 reads=[b_pTa[p], b_msk], writes=[b_pTa[p]])

        def back(i):
            c, t, hh = items[i]
            kvh = c // 2
            p = i % 4
            fi = i // 2
            ob, sbk = (4, 5) if fi % 2 == 0 else (6, 7)
            for bi, w in enumerate(blocks_of(t)):
                first = (hh == 0 and bi == 0)
                last = (hh == 1 and bi == 4)
                T.op("pe", lambda: nc.tensor.matmul(P.banks[ob][:, 0:128], lhsT=vaw[:, kvh * 2 + hh, w, :], rhs=pTa[p][:, bi * 128:(bi + 1) * 128],
                                                    start=first, stop=last), reads=[b_vaw, b_pTa[p]], writes=[P.bb[ob]])
                T.op("pe", lambda: nc.tensor.matmul(P.banks[sbk][:, 0:128], lhsT=oneslh[:, hh, :], rhs=pTa[p][:, bi * 128:(bi + 1) * 128],
                                                    start=first, stop=last), reads=[b_olh, b_pTa[p]], writes=[P.bb[sbk]])
            if hh == 1:
                r = fi % 2
                T.op("dve", lambda: nc.vector.tensor_scalar(out=den[r][:], in0=P.banks[sbk][:, 0:128], scalar1=esink[:, c:c + 1], scalar2=None, op0=ALU.add),
                     reads=[P.bb[sbk], b_es], writes=[b_den[r]])
                T.op("dve", lambda: nc.vector.reciprocal(out=den[r][:], in_=den[r][:]), reads=[b_den[r]], writes=[b_den[r]])
                T.op("dve", lambda: nc.vector.tensor_tensor(out=OT[:, c, t * 128:(t + 1) * 128], in0=P.banks[ob][:, 0:128], in1=den[r][:], op=ALU.mult),
                     reads=[P.bb[ob], b_den[r]], writes=[b_OT])

        LAG = 2
        for i in range(len(items) + LAG):
            if i < len(items):
                front(i)
            if i >= LAG:
                back(i - LAG)
    T.barrier()
    if "nomB" in os.environ.get("K_DBG", ""):
        return
    phase_B_even_mixB(P, OT, b_OT, QTb_d, KTb_d, Vb_d, lamv_d, lami_d, subg_d)


def phase_B_even_mixB(P, OT, b_OT, QTb_d, KTb_d, Vb_d, lamv_d, lami_d, subg_d):
    nc, T = P.nc, P.T
    sc = 64.0 ** -0.5
    with ExitStack() as es:
        S = SweepCtx(P, es)
        ktbs = [P.sb(es, [128, NKB * 128], BF16, "ktb") for _ in range(2)]
        vtbs = [P.sb(es, [128, NKB, 128], BF16, "vtb") for _ in range(2)]
        b_kvs = [Buf("kvb%d" % i) for i in range(2)]
        qtb = [P.sb(es, [128, 2, TOK], BF16, "qtb") for _ in range(2)]
        b_qtb = [Buf("qtb%d" % i) for i in range(2)]
        lamv = P.sb(es, [128, 4, 64], F32, "lamv")
        b_lamv = Buf("lamv")
        for i in range(4):
            T.dma("sp", lamv[:, i, :], lamv_d[i:i + 1, :].partition_broadcast(128), writes=[b_lamv])
        lami = P.sb(es, [128, 2], F32, "lami")
        b_lami = Buf("lami")
        T.dma("sp", lami[:], lami_d[:, :], writes=[b_lami])
        lp = P.sb(es, [128, 2, 64], F32, "lp")
        b_lp = Buf("lp")
        ls = P.sb(es, [128, 4], F32, "ls")
        b_ls = Buf("ls")
        T.op("dve", lambda: nc.vector.tensor_tensor(out=lp[:, 0, :], in0=lamv[:, 0, :], in1=lamv[:, 1, :], op=ALU.mult), reads=[b_lamv], writes=[b_lp])
        T.op("dve", lambda: nc.vector.tensor_tensor(out=lp[:, 1, :], in0=lamv[:, 2, :], in1=lamv[:, 3, :], op=ALU.mult), reads=[b_lamv, b_lp], writes=[b_lp])
        T.op("dve", lambda: nc.vector.tensor_reduce(out=ls[:, 0:2], in_=lp[:], axis=AX.X, op=ALU.add), reads=[b_lp], writes=[b_ls])
        T.op("act", lambda: nc.scalar.activation(out=ls[:, 0:2], in_=ls[:, 0:2], func=AF.Exp), reads=[b_ls], writes=[b_ls])
        T.op("dve", lambda: nc.vector.tensor_tensor(out=ls[:, 2:3], in0=ls[:, 1:2], in1=ls[:, 0:1], op=ALU.subtract), reads=[b_ls], writes=[b_ls])
        T.op("dve", lambda: nc.vector.tensor_tensor(out=ls[:, 2:3], in0=ls[:, 2:3], in1=lami[:, 0:1], op=ALU.subtract), reads=[b_ls, b_lami], writes=[b_ls])
        gsc = P.sb(es, [128, 1], F32, "gsc")
        b_gsc = Buf("gsc")
        T.dma("sp", gsc[:], subg_d[:, :], writes=[b_gsc])
        T.op("dve", lambda: nc.vector.tensor_tensor(out=gsc[:], in0=gsc[:], in1=lami[:, 1:2], op=ALU.mult), reads=[b_gsc, b_lami], writes=[b_gsc])
        epss = P.sb(es, [128, 1], F32, "epss")
        b_epss = Buf("epss")
        T.op("pool", lambda: nc.gpsimd.memset(epss[:], SUBLN_EPS), writes=[b_epss])
        am = [P.sb(es, [128, 512], F32, "am") for _ in range(2)]
        b_am = [Buf("am%d" % i) for i in range(2)]
        dm = P.sb(es, [128, 512], F32, "dm")
        b_dm = Buf("dm")
        sq = P.sb(es, [128, 512], F32, "sqb")
        b_sq = Buf("sqb")
        rstd, b_rstd = sq, b_sq
        def load_head(h):
            T.dma("sp", ktbs[h % 2][:], KTb_d[h, :, :], writes=[b_kvs[h % 2]])
            T.dma("sp", vtbs[h % 2][:], Vb_d[h, :, :, :], writes=[b_kvs[h % 2]])
            T.dma("sp", qtb[h % 2][:], QTb_d[h, :, :, :], writes=[b_qtb[h % 2]])
        load_head(0)
        for h in range(4):
            ktb, vtb, b_kv = ktbs[h % 2], vtbs[h % 2], b_kvs[h % 2]
            qi = h % 2
            if h + 1 < 4:
                load_head(h + 1)
            for (c0, N) in CHUNKS:
                blocks = ALL_BLOCKS if N == 512 else CTX_BLOCKS
                for mm in range(2):
                    ob, sbk = sweep(P, S, N, blocks, qtb[qi][:, mm, c0:c0 + N], b_qtb[qi],
                                    lambda b: ktb[:, b * 128:(b + 1) * 128], lambda b: vtb[:, b, :], b_kv, 128, sc)
                    T.op("dve", lambda: nc.vector.reciprocal(out=am[mm][:, 0:N], in_=P.banks[sbk][:, 0:N]), reads=[P.bb[sbk]], writes=[b_am[mm]])
                    T.op("dve", lambda: nc.vector.tensor_tensor(out=am[mm][:, 0:N], in0=P.banks[ob][:, 0:N], in1=am[mm][:, 0:N], op=ALU.mult),
                         reads=[P.bb[ob], b_am[mm]], writes=[b_am[mm]])
                T.op("dve", lambda: nc.vector.scalar_tensor_tensor(out=dm[:, 0:N], in0=am[1][:, 0:N], scalar=ls[:, 2:3], in1=am[0][:, 0:N],
                                                                   op0=ALU.mult, op1=ALU.add), reads=[b_am[0], b_am[1], b_ls], writes=[b_dm])
                T.op("act", lambda: nc.scalar.activation(out=sq[:, 0:N], in_=dm[:, 0:N], func=AF.Square), reads=[b_dm], writes=[b_sq])
                T.op("pe", lambda: nc.tensor.matmul(P.banks[sbk][:, 0:N], lhsT=P.onesd[:], rhs=sq[:, 0:N], start=True, stop=True),
                     reads=[P.b_onesd, b_sq], writes=[P.bb[sbk]])
                T.op("act", lambda: nc.scalar.activation(out=rstd[:, 0:N], in_=P.banks[sbk][:, 0:N], func=AF.Sqrt, bias=epss[:, 0:1], scale=1.0),
                     reads=[P.bb[sbk], b_epss], writes=[b_rstd])
                T.op("dve", lambda: nc.vector.reciprocal(out=rstd[:, 0:N], in_=rstd[:, 0:N]), reads=[b_rstd], writes=[b_rstd])
                T.op("dve", lambda: nc.vector.scalar_tensor_tensor(out=OT[:, 4 + h, c0:c0 + N], in0=dm[:, 0:N], scalar=gsc[:, 0:1], in1=rstd[:, 0:N],
                                                                   op0=ALU.mult, op1=ALU.mult), reads=[b_dm, b_gsc, b_rstd], writes=[b_OT])
    T.barrier()


def layer_norm_tile(P, L, src, b_src, dst, b_dst, gam, bet, b_gb):
    nc, T = P.nc, P.T
    st, b_st, mv, b_mv, xn, b_xn = L
    T.op("dve", lambda: nc.vector.bn_stats(out=st[:, 0, :], in_=src[:, 0:512]), reads=[b_src], writes=[b_st])
    T.op("dve", lambda: nc.vector.bn_stats(out=st[:, 1, :], in_=src[:, 512:1024]), reads=[b_src, b_st], writes=[b_st])
    T.op("dve", lambda: nc.vector.bn_aggr(out=mv[:, 0:2], in_=st[:].rearrange("p a s -> p (a s)")), reads=[b_st], writes=[b_mv])
    T.op("act", lambda: nc.scalar.activation(out=mv[:, 2:3], in_=mv[:, 1:2], func=AF.Sqrt, bias=mv[:, 3:4], scale=1.0), reads=[b_mv], writes=[b_mv])
    T.op("dve", lambda: nc.vector.reciprocal(out=mv[:, 2:3], in_=mv[:, 2:3]), reads=[b_mv], writes=[b_mv])
    T.op("dve", lambda: nc.vector.tensor_scalar(out=xn[:], in0=src, scalar1=mv[:, 0:1], scalar2=mv[:, 2:3], op0=ALU.subtract, op1=ALU.mult),
         reads=[b_src, b_mv], writes=[b_xn])
    T.op("pool", lambda: nc.gpsimd.tensor_tensor(out=xn[:], in0=xn[:], in1=gam, op=ALU.mult), reads=[b_xn, b_gb], writes=[b_xn])
    T.op("pool", lambda: nc.gpsimd.tensor_tensor(out=dst, in0=xn[:], in1=bet, op=ALU.add), reads=[b_xn, b_gb], writes=[b_dst])


def ln_scratch(P, es, tag):
    nc, T = P.nc, P.T
    sets = []
    for i in range(2):
        st = P.sb(es, [128, 2, 6], F32, "lnst")
        mv = P.sb(es, [128, 4], F32, "lnmv")
        xn = P.sb(es, [128, D], F32, "lnxn")
        b_mv = Buf("lnmv%s%d" % (tag, i))
        T.op("pool", lambda: nc.gpsimd.memset(mv[:, 3:4], LN_EPS), writes=[b_mv])
        sets.append((st, Buf("lnst%s%d" % (tag, i)), mv, b_mv, xn, Buf("lnxn%s%d" % (tag, i))))
    return sets


def phase_C1(P, OT, b_OT, x_d, wo_d, gbc_d, lng_d, lnb_d, x1_d, x1b):
    nc, T = P.nc, P.T
    with ExitStack() as es:
        wo, wob = load_weight_bf16(P, es, wo_d, 8, D, "wo")
        gb = P.sb(es, [128, 2, D], F32, "g1bc")
        b_gb = Buf("g1bc")
        for who in range(2):
            T.dma("sp", gb[:, who, :], gbc_d[who, 0:1, :].partition_broadcast(128), writes=[b_gb])
        ln = P.sb(es, [128, 2, D], F32, "ln1")
        b_ln = Buf("ln1")
        T.dma("sp", ln[:, 0, :], lng_d[0:1, :].partition_broadcast(128), writes=[b_ln])
        T.dma("sp", ln[:, 1, :], lnb_d[0:1, :].partition_broadcast(128), writes=[b_ln])
        L = ln_scratch(P, es, "1")
        xt = [P.sb(es, [128, D], F32, "cxt") for _ in range(2)]
        b_xt = [Buf("cxt%d" % i) for i in range(2)]
        rr = [P.sb(es, [128, D], F32, "crr") for _ in range(2)]
        b_rr = [Buf("crr%d" % i) for i in range(2)]
        x1t = [P.sb(es, [128, D], F32, "x1t") for _ in range(2)]
        b_x1t = [Buf("x1t%d" % i) for i in range(2)]
        for t in range(NT):
            i = t % 2
            who = 1 if t == NT - 1 else 0
            yb = (0, 1) if i == 0 else (2, 3)
            T.dma("sp", xt[i][:], x_d[t * 128:(t + 1) * 128, :], writes=[b_xt[i]])
            for half in range(2):
                for k in range(8):
                    T.op("pe", lambda: nc.tensor.matmul(P.banks[yb[half]][:, :], lhsT=OT[:, k, t * 128:(t + 1) * 128],
                                                        rhs=wo[:, k, half * 512:(half + 1) * 512], start=(k == 0), stop=(k == 7)),
                         reads=[b_OT, wob[half * 4]], writes=[P.bb[yb[half]]])
                T.op("dve", lambda: nc.vector.tensor_tensor(out=rr[i][:, half * 512:(half + 1) * 512], in0=P.banks[yb[half]][:, :],
                                                            in1=gb[:, who, half * 512:(half + 1) * 512], op=ALU.mult),
                     reads=[P.bb[yb[half]], b_gb], writes=[b_rr[i]])
            T.op("dve", lambda: nc.vector.scalar_tensor_tensor(out=rr[i][:], in0=xt[i][:], scalar=ALPHA, in1=rr[i][:], op0=ALU.mult, op1=ALU.add),
                 reads=[b_xt[i], b_rr[i]], writes=[b_rr[i]])
            layer_norm_tile(P, L[i], rr[i][:], b_rr[i], x1t[i][:], b_x1t[i], ln[:, 0, :], ln[:, 1, :], b_ln)
            T.dma("pool", x1_d[t * 128:(t + 1) * 128, :], x1t[i][:], reads=[b_x1t[i]], writes=[x1b[t]])
    T.barrier()


def phase_C2(P, x1_d, x1b, mC_d, gbc_d, lng_d, lnb_d, wr_d, br_d, wg_d, wu_d, wd_d, xo_d, xob):
    nc, T = P.nc, P.T
    with ExitStack() as es:
        acc = P.sb(es, [128, NT, D], F32, "acc")
        accb = [Buf("acc%d" % t) for t in range(NT)]
        h2T = P.sb(es, [128, 8, TOK], BF16, "h2T")
        h2b = [Buf("h2T%d" % c) for c in range(len(CHUNKS))]
        m, b_m = load_modT(P, es, mC_d, "mC")
        gb = P.sb(es, [128, 2, D], F32, "g2bc")
        b_gb = Buf("g2bc")
        for who in range(2):
            T.dma("sp", gb[:, who, :], gbc_d[who, 1:2, :].partition_broadcast(128), writes=[b_gb])
        wr = P.sb(es, [128, 8, NEXP], F32, "wr")
        b_wr = Buf("wr")
        T.dma("sp", wr[:], wr_d.rearrange("(k p) e -> p k e", p=128), writes=[b_wr])
        brt = P.sb(es, [128, NEXP], F32, "brt")
        b_brt = Buf("brt")
        T.dma("sp", brt[:], br_d[0:1, :].partition_broadcast(128), writes=[b_brt])
        scs = P.sb(es, [128, NT, NEXP], F32, "scs")
        b_scs = Buf("scs")
        gates = P.sb(es, [128, NT, NEXP], F32, "gates")
        b_gates = Buf("gates")
        with ExitStack() as es2:
            hTf = [P.sb(es2, [128, 8, 128], F32, "hTf") for _ in range(2)]
            b_hTf = [Buf("hTf%d" % i) for i in range(2)]
            def pro1(t):
                i = t % 2
                who = 1 if t == NT - 1 else 0
                ch = min(t // 4, 4)
                T.dma("sp", acc[:, t, :], x1_d[t * 128:(t + 1) * 128, :], reads=[x1b[t]], writes=[accb[t]])
                transpose_mod(P, acc[:, t, :], accb[t], m[:, who, 0, :], m[:, who, 1, :], b_m, h2T[:, :, t * 128:(t + 1) * 128], h2b[ch],
                              (0, 1) if i == 0 else (2, 3), hTf[i], b_hTf[i])

            pro1(0)
            for t in range(NT):
                i = t % 2
                if t + 1 < NT:
                    pro1(t + 1)
                lb = 4 + i
                DBG = os.environ.get("K_DBG", "")
                if "nologit" not in DBG:
                    for k in range(8):
                        T.op("pe", lambda: nc.tensor.matmul(P.banks[lb][:, 0:NEXP], lhsT=hTf[i][:, k, :], rhs=wr[:, k, :], start=(k == 0), stop=(k == 7)),
                             reads=[b_hTf[i], b_wr], writes=[P.bb[lb]])
                    if "nosig" not in DBG:
                        T.op("act", lambda: nc.scalar.activation(out=scs[:, t, :], in_=P.banks[lb][:, 0:NEXP], func=AF.Sigmoid), reads=[P.bb[lb]], writes=[b_scs])
                if "nopool" not in DBG:
                    T.op("pool", lambda: nc.gpsimd.tensor_scalar(out=acc[:, t, :], in0=acc[:, t, :], scalar1=ALPHA, scalar2=None, op0=ALU.mult),
                         reads=[accb[t]], writes=[accb[t]])
            if "noroute" in os.environ.get("K_DBG", ""):
                return
            G = NT * 4
            sel = P.sb(es2, [128, NT, NEXP], F32, "sel")
            sel2 = P.sb(es2, [128, NT, NEXP], F32, "sel2")
            eq = P.sb(es2, [128, NT, NEXP], F32, "eq")
            m1 = P.sb(es2, [128, G], F32, "m1")
            m2 = P.sb(es2, [128, G], F32, "m2")
            gs = P.sb(es2, [128, G], F32, "gs")
            gmx = P.sb(es2, [128, NT], F32, "gmx")
            b_r = Buf("route")
            g4 = lambda a: a[:].rearrange("p t (g j) -> p (t g) j", j=4)
            bc4 = lambda a: a[:].unsqueeze(2).to_broadcast([128, G, 4])
            R = dict(reads=[b_r, b_scs, b_brt], writes=[b_r])
            T.op("dve", lambda: nc.vector.tensor_tensor(out=sel[:], in0=scs[:], in1=brt[:].unsqueeze(1).to_broadcast([128, NT, NEXP]), op=ALU.add), **R)
            T.op("dve", lambda: nc.vector.tensor_reduce(out=m1[:], in_=g4(sel), axis=AX.X, op=ALU.max), **R)
            T.op("dve", lambda: nc.vector.tensor_tensor(out=g4(eq), in0=g4(sel), in1=bc4(m1), op=ALU.is_equal), **R)
            T.op("dve", lambda: nc.vector.scalar_tensor_tensor(out=sel2[:], in0=eq[:], scalar=-1.0e9, in1=sel[:], op0=ALU.mult, op1=ALU.add), **R)
            T.op("dve", lambda: nc.vector.tensor_reduce(out=m2[:], in_=g4(sel2), axis=AX.X, op=ALU.max), **R)
            T.op("dve", lambda: nc.vector.tensor_tensor(out=gs[:], in0=m1[:], in1=m2[:], op=ALU.add), **R)
            T.op("dve", lambda: nc.vector.tensor_reduce(out=gmx[:], in_=gs[:].rearrange("p (t g) -> p t g", g=4), axis=AX.X, op=ALU.max), **R)
            T.op("dve", lambda: nc.vector.tensor_tensor(out=gs[:].rearrange("p (t g) -> p t g", g=4), in0=gs[:].rearrange("p (t g) -> p t g", g=4),
                                                        in1=gmx[:].unsqueeze(2).to_broadcast([128, NT, 4]), op=ALU.is_equal), **R)
            T.op("dve", lambda: nc.vector.tensor_tensor(out=g4(eq), in0=g4(sel), in1=bc4(m2), op=ALU.is_ge), **R)
            T.op("dve", lambda: nc.vector.tensor_tensor(out=g4(eq), in0=g4(eq), in1=bc4(gs), op=ALU.mult), **R)
            T.op("dve", lambda: nc.vector.tensor_tensor(out=sel[:], in0=scs[:], in1=eq[:], op=ALU.mult), **R)
            T.op("dve", lambda: nc.vector.tensor_reduce(out=gmx[:], in_=sel[:], axis=AX.X, op=ALU.add), **R)
            T.op("dve", lambda: nc.vector.reciprocal(out=gmx[:], in_=gmx[:]), **R)
            T.op("dve", lambda: nc.vector.tensor_tensor(out=gates[:], in0=sel[:], in1=gmx[:].unsqueeze(2).to_broadcast([128, NT, NEXP]), op=ALU.mult),
                 reads=[b_r], writes=[b_gates])
        T.barrier()
        DBG = os.environ.get("K_DBG", "")
        if "noexp" in DBG:
            return
        with ExitStack() as es3:
            wg = [P.sb(es3, [128, 8, DEXP], BF16, "wg") for _ in range(2)]
            wu = [P.sb(es3, [128, 8, DEXP], BF16, "wu") for _ in range(2)]
            wd = [P.sb(es3, [128, 4, D], BF16, "wd") for _ in range(2)]
            wbuf = [[[Buf("w%d_%d_%d" % (s_, m_, h_)) for h_ in range(2)] for m_ in range(3)] for s_ in range(2)]
            NSTG = 2
            stg = [P.sb(es3, [128, 2048], F32, "wstg") for _ in range(NSTG)]
            b_stg = [Buf("wstg%d" % i) for i in range(NSTG)]
            sg = [P.sb(es3, [128, 512], F32, "sg") for _ in range(2)]
            b_sg = [Buf("sg%d" % i) for i in range(2)]
            aT = [P.sb(es3, [128, 4, 512], BF16, "aT") for _ in range(2)]
            b_aT = [Buf("aT%d" % i) for i in range(2)]
            tmp = [P.sb(es3, [128, D], F32, "mtmp") for _ in range(2)]
            b_tmp = [Buf("mtmp%d" % i) for i in range(2)]
            si = 0
            fi = 0
            ti = 0

            def load_expert(e):
                nonlocal si
                s_ = e % 2
                for m_, (src, dst, K) in enumerate(((wg_d, wg[s_], 8), (wu_d, wu[s_], 8), (wd_d, wd[s_], 4))):
                    for h_ in range(2):
                        st = stg[si % NSTG]
                        bs = b_stg[si % NSTG]
                        si += 1
                        kh = K // 2
                        T.dma("sp", st[:].rearrange("p (k n) -> p k n", k=kh),
                              src[e, h_ * kh * 128:(h_ + 1) * kh * 128, :].rearrange("(k p) n -> p k n", p=128), writes=[bs])
                        T.op("act", lambda: nc.scalar.copy(out=dst[:, h_ * kh:(h_ + 1) * kh, :], in_=st[:].rearrange("p (k n) -> p k n", k=kh)),
                             reads=[bs], writes=[wbuf[s_][m_][h_]])

            def gate_up(e, chn, a):
                nonlocal fi
                s_ = e % 2
                c0, N = CHUNKS[chn]
                for f in range(4):
                    gbk = fi % 2
                    ubk = 2 + fi % 2
                    fi += 1
                    for k in range(8):
                        T.op("pe", lambda: nc.tensor.matmul(P.banks[gbk][:, 0:N], lhsT=wg[s_][:, k, f * 128:(f + 1) * 128], rhs=h2T[:, k, c0:c0 + N],
                                                            start=(k == 0), stop=(k == 7)), reads=[wbuf[s_][0][k // 4], h2b[chn]], writes=[P.bb[gbk]])
                    for k in range(8):
                        T.op("pe", lambda: nc.tensor.matmul(P.banks[ubk][:, 0:N], lhsT=wu[s_][:, k, f * 128:(f + 1) * 128], rhs=h2T[:, k, c0:c0 + N],
                                                            start=(k == 0), stop=(k == 7)), reads=[wbuf[s_][1][k // 4], h2b[chn]], writes=[P.bb[ubk]])
                    T.op("act", lambda: nc.scalar.activation(out=sg[gbk][:, 0:N], in_=P.banks[gbk][:, 0:N], func=AF.Silu),
                         reads=[P.bb[gbk]], writes=[b_sg[gbk]])
                    T.op("dve", lambda: nc.vector.tensor_tensor(out=aT[a][:, f, 0:N], in0=sg[gbk][:, 0:N], in1=P.banks[ubk][:, 0:N], op=ALU.mult),
                         reads=[b_sg[gbk], P.bb[ubk]], writes=[b_aT[a]])

            def down(e, chn, a):
                nonlocal ti
                s_ = e % 2
                c0, N = CHUNKS[chn]
                for tt in range(N // 128):
                    t = c0 // 128 + tt
                    who = 1 if t == NT - 1 else 0
                    yb = (4, 5) if ti % 2 == 0 else (6, 7)
                    tm = ti % 2
                    ti += 1
                    for half in range(2):
                        for f in range(4):
                            T.op("pe", lambda: nc.tensor.matmul(P.banks[yb[half]][:, :], lhsT=aT[a][:, f, tt * 128:(tt + 1) * 128],
                                                                rhs=wd[s_][:, f, half * 512:(half + 1) * 512], start=(f == 0), stop=(f == 3)),
                                 reads=[b_aT[a], wbuf[s_][2][f // 2]], writes=[P.bb[yb[half]]])
                        T.op("dve", lambda: nc.vector.scalar_tensor_tensor(out=tmp[tm][:, half * 512:(half + 1) * 512], in0=P.banks[yb[half]][:, :],
                                                                           scalar=gates[:, t, e:e + 1], in1=gb[:, who, half * 512:(half + 1) * 512],
                                                                           op0=ALU.mult, op1=ALU.mult),
                             reads=[P.bb[yb[half]], b_gates, b_gb], writes=[b_tmp[tm]])
                    T.op("pool", lambda: nc.gpsimd.tensor_tensor(out=acc[:, t, :], in0=acc[:, t, :], in1=tmp[tm][:], op=ALU.add),
                         reads=[accb[t], b_tmp[tm]], writes=[accb[t]])

            items = [(e, chn) for e in range(NEXP) for chn in range(len(CHUNKS))]
            load_expert(0)
            for i in range(len(items) + 1):
                if i < len(items):
                    e, chn = items[i]
                    gate_up(e, chn, i % 2)
                if i >= 1:
                    e0, chn0 = items[i - 1]
                    down(e0, chn0, (i - 1) % 2)
                if i < len(items):
                    e, chn = items[i]
                    if chn == 0 and e + 1 < NEXP:
                        load_expert(e + 1)
        T.barrier()
        with ExitStack() as es4:
            L = ln_scratch(P, es4, "2")
            ln = P.sb(es4, [128, 2, D], F32, "ln2")
            b_ln = Buf("ln2")
            T.dma("sp", ln[:, 0, :], lng_d[1:2, :].partition_broadcast(128), writes=[b_ln])
            T.dma("sp", ln[:, 1, :], lnb_d[1:2, :].partition_broadcast(128), writes=[b_ln])
            xo = [P.sb(es4, [128, D], F32, "xo") for _ in range(2)]
            b_xo = [Buf("xo%d" % i) for i in range(2)]
            for t in range(NT):
                i = t % 2
                layer_norm_tile(P, L[i], acc[:, t, :], accb[t], xo[i][:], b_xo[i], ln[:, 0, :], ln[:, 1, :], b_ln)
                T.dma("pool", xo_d[t * 128:(t + 1) * 128, :], xo[i][:], reads=[b_xo[i]], writes=[xob[t]])
    T.barrier()


def rope_tables(hd):
    q = hd // 4
    inv = (10000.0 ** (-np.arange(q, dtype=np.float32) / np.float32(q))).astype(np.float32)
    tpos = np.arange(SEQ)
    ang_r = (tpos // 64).astype(np.float32)[:, None] * inv[None, :]
    ang_c = (tpos % 64).astype(np.float32)[:, None] * inv[None, :]
    cos = np.concatenate([np.cos(ang_r), np.cos(ang_c)], axis=1).astype(np.float32)
    sin = np.concatenate([np.sin(ang_r), np.sin(ang_c)], axis=1).astype(np.float32)
    cos_c = np.ones((NCORES, TOK, 2 * q), np.float32)
    sin_c = np.zeros((NCORES, TOK, 2 * q), np.float32)
    for r in range(NCORES):
        cos_c[r, :2048] = cos[r * 2048:(r + 1) * 2048]
        sin_c[r, :2048] = sin[r * 2048:(r + 1) * 2048]
    return cos_c, sin_c


IDENT = np.eye(128, dtype=np.float32)
_PROG_CACHE = {}


def run(nc, in_maps):
    return run_bass_kernel_spmd(nc, in_maps, core_ids=list(range(NCORES))).results


def modT_layout(modv, l, cols):
    out = np.empty((128, 2, len(cols), 8), np.float32)
    for who in range(2):
        for j, c in enumerate(cols):
            out[:, who, j, :] = modv[l, who, c * 1024:(c + 1) * 1024].reshape(8, 128).T
    return out


def build_mod():
    if "mod" not in _PROG_CACHE:
        P = Prog()
        cc = P.inp("cc", [128, 8, 2], F32)
        wm = P.inp("wm", [DEPTH, D, 768], F32)
        bm = P.inp("bm", [DEPTH, 2, 768], F32)
        out = P.outp("modp", [DEPTH, 2, 768], F32)
        phase_mod(P, cc, wm, bm, out, 768)
        _PROG_CACHE["mod"] = P.close()
    return _PROG_CACHE["mod"]


def run_mod(c, c_ctx, w_mod, b_mod):
    nc = build_mod()
    cc = np.stack([c.reshape(128, 8), c_ctx.reshape(128, 8)], axis=-1).astype(np.float32)
    maps = []
    for r in range(NCORES):
        sl = slice(r * 768, (r + 1) * 768)
        maps.append({"ident": IDENT, "cc": cc, "wm": np.ascontiguousarray(w_mod[:, :, sl]),
                     "bm": np.ascontiguousarray(np.repeat(b_mod[:, None, sl], 2, axis=1))})
    res = run(nc, maps)
    return np.concatenate([res[r]["modp"] for r in range(NCORES)], axis=2)


def build_A(even):
    key = "A%d" % even
    if key not in _PROG_CACHE:
        P = Prog()
        ncols = E_COLS if even else O_COLS
        hq = 32 if even else 64
        x = P.inp("x", [TOK, D], F32)
        w = P.inp("w_in", [D, ncols], F32)
        mA = P.inp("mA", [128, 2, 2, 8], F32)
        cos = P.inp("cos", [TOK, hq], F32)
        sin = P.inp("sin", [TOK, hq], F32)
        qng = None if even else P.inp("qng", [2, 128], F32)
        qkv = P.outp("qkv", [TOK, ncols], BF16)
        phase_A(P, even, x, None, w, mA, cos, sin, qng, qkv)
        _PROG_CACHE[key] = P.close()
    return _PROG_CACHE[key]


def gather_tokens(parts, c0, c1):
    ctx = np.concatenate([parts[0][2048:, c0:c1], parts[1][2048:, c0:c1]], axis=0)
    lat = np.concatenate([p[:2048, c0:c1] for p in parts], axis=0)
    return np.concatenate([ctx, lat], axis=0)


def layout_odd(qkv):
    k_all = gather_tokens(qkv, 1024, 1280)
    v_all = gather_tokens(qkv, 1280, 1536)
    KT = np.ascontiguousarray(k_all.reshape(NKB * 128, 2, 128).transpose(1, 2, 0))
    V = np.ascontiguousarray(v_all.reshape(NKB, 128, 2, 128).transpose(2, 1, 0, 3))
    maps = []
    for r in range(NCORES):
        QT = np.ascontiguousarray(qkv[r][:, 0:1024].reshape(TOK, 8, 128).transpose(1, 2, 0))
        maps.append({"QT": QT, "KT": KT, "V": V})
    return maps


def window_masks():
    m = np.zeros((NCORES, 128, NT, 384), np.float32)
    jj = np.arange(128)[:, None]
    ii = np.arange(128)[None, :]
    left = (ii <= jj).astype(np.float32)
    right = (jj <= ii).astype(np.float32)
    for r in range(NCORES):
        for t in range(16):
            n = 16 * r + t
            if n - 1 >= 0:
                m[r, :, t, 0:128] = left
            m[r, :, t, 128:256] = 1.0
            if n + 1 < SEQ // 128:
                m[r, :, t, 256:384] = right
    return m.astype(NPBF)


def layout_even(qkv, sink, lam4, lam_init, subln_g):
    ak = gather_tokens(qkv, 512, 640)
    av = gather_tokens(qkv, 640, 768)
    bk = gather_tokens(qkv, 1280, 1792)
    bv = gather_tokens(qkv, 1792, 2304)
    KTb = np.ascontiguousarray(bk.reshape(NKB * 128, 4, 128).transpose(1, 2, 0))
    Vb = np.ascontiguousarray(bv.reshape(NKB, 128, 4, 128).transpose(2, 1, 0, 3))
    masks = window_masks()
    sinkp = np.empty((128, 4), np.float32)
    for c in range(4):
        sinkp[:64, c] = sink[2 * c]
        sinkp[64:, c] = sink[2 * c + 1]
    lami = np.empty((128, 2), np.float32)
    lami[:, 0] = lam_init
    lami[:, 1] = 1.0 - lam_init
    akb = ak.reshape(NKB, 128, 128)
    avb = av.reshape(NKB, 128, 2, 64)
    maps = []
    for r in range(NCORES):
        q = qkv[r]
        qb = q[:, 768:1280].reshape(TOK, 4, 2, 64).transpose(1, 2, 3, 0)
        QTb = np.zeros((4, 128, 2, TOK), q.dtype)
        QTb[:, 0:64, 0, :] = qb[:, 0]
        QTb[:, 64:128, 1, :] = qb[:, 1]
        QTa = np.ascontiguousarray(q[:, 0:512].reshape(TOK, 2, 4, 64).transpose(1, 3, 2, 0).reshape(128, 4, TOK))
        kw = np.zeros((20, 128, 128), ak.dtype)
        vw = np.zeros((20, 128, 2, 64), av.dtype)
        kw[0:2] = akb[0:2]
        vw[0:2] = avb[0:2]
        for w in range(2, 20):
            n = 16 * r - 1 + (w - 2)
            if 0 <= n < SEQ // 128:
                kw[w] = akb[2 + n]
                vw[w] = avb[2 + n]
        KTa = np.ascontiguousarray(kw.transpose(2, 0, 1).reshape(128, 20 * 128))
        Va = np.zeros((128, 4, 20, 128), av.dtype)
        for kvh in range(2):
            for lohi in range(2):
                Va[:, kvh * 2 + lohi, :, lohi * 64:lohi * 64 + 64] = vw[:, :, kvh, :].transpose(1, 0, 2)
        maps.append({"QTa": QTa, "KTa": KTa, "Va": Va, "mask": masks[r], "sinkp": sinkp, "QTb": QTb, "KTb": KTb, "Vb": Vb,
                     "lamv": np.ascontiguousarray(lam4.astype(np.float32)), "lami": lami,
                     "subg": np.ascontiguousarray(subln_g.reshape(128, 1).astype(np.float32))})
    return maps


def decl_B(P, even):
    if even:
        return dict(QTa=P.inp("QTa", [128, 4, TOK], BF16), KTa=P.inp("KTa", [128, 20 * 128], BF16), Va=P.inp("Va", [128, 4, 20, 128], BF16),
                    mask=P.inp("mask", [128, NT, 384], BF16), sinkp=P.inp("sinkp", [128, 4], F32), QTb=P.inp("QTb", [4, 128, 2, TOK], BF16),
                    KTb=P.inp("KTb", [4, 128, NKB * 128], BF16), Vb=P.inp("Vb", [4, 128, NKB, 128], BF16), lamv=P.inp("lamv", [4, 64], F32),
                    lami=P.inp("lami", [128, 2], F32), subg=P.inp("subg", [128, 1], F32))
    return dict(QT=P.inp("QT", [8, 128, TOK], BF16), KT=P.inp("KT", [2, 128, NKB * 128], BF16), V=P.inp("V", [2, 128, NKB, 128], BF16))


def emit_B(P, even, OT, b_OT, d):
    if even:
        phase_B_even(P, OT, b_OT, d["QTa"], d["KTa"], d["Va"], d["mask"], d["sinkp"], d["QTb"], d["KTb"], d["Vb"], d["lamv"], d["lami"], d["subg"])
    else:
        phase_B_odd(P, OT, b_OT, d["QT"], d["KT"], d["V"])


def build_Btest(even):
    key = "Bt%d" % even
    if key not in _PROG_CACHE:
        P = Prog()
        d = decl_B(P, even)
        out = P.outp("OT", [128, 8, TOK], BF16)
        OT = P.sb(P.es, [128, 8, TOK], BF16, "OT")
        b_OT = Buf("OT")
        emit_B(P, even, OT, b_OT, d)
        P.T.dma("pool", out[:, :, :], OT[:], reads=[b_OT])
        _PROG_CACHE[key] = P.close()
    return _PROG_CACHE[key]


def build_BCA(even, has_A):
    key = "BCA%d%d" % (even, has_A)
    if key in _PROG_CACHE:
        return _PROG_CACHE[key]
    P = Prog()
    nc = P.nc
    dB = decl_B(P, even)
    x = P.inp("x", [TOK, D], F32)
    wo = P.inp("w_out", [D, D], F32)
    gbc = P.inp("gbc", [2, 2, D], F32)
    mC = P.inp("mC", [128, 2, 2, 8], F32)
    lng = P.inp("lng", [2, D], F32)
    lnb = P.inp("lnb", [2, D], F32)
    wr = P.inp("wr", [D, NEXP], F32)
    br = P.inp("br", [1, NEXP], F32)
    wg = P.inp("wg", [NEXP, D, DEXP], F32)
    wu = P.inp("wu", [NEXP, D, DEXP], F32)
    wd = P.inp("wd", [NEXP, DEXP, D], F32)
    x1s = nc.dram_tensor("x1s", [TOK, D], F32, kind="Internal").ap()
    xo = P.outp("xo", [TOK, D], F32)
    x1b = [Buf("x1s%d" % t) for t in range(NT)]
    xob = [Buf("xo%d" % t) for t in range(NT)]
    if has_A:
        a_even = not even
        ncols = E_COLS if a_even else O_COLS
        hq = 32 if a_even else 64
        w_in = P.inp("w_in", [D, ncols], F32)
        mA = P.inp("mA", [128, 2, 2, 8], F32)
        cos = P.inp("cos", [TOK, hq], F32)
        sin = P.inp("sin", [TOK, hq], F32)
        qng = None if a_even else P.inp("qng", [2, 128], F32)
        qkv = P.outp("qkv", [TOK, ncols], BF16)
    with ExitStack() as es:
        OT = P.sb(es, [128, 8, TOK], BF16, "OT")
        b_OT = Buf("OT")
        emit_B(P, even, OT, b_OT, dB)
        phase_C1(P, OT, b_OT, x, wo, gbc, lng, lnb, x1s, x1b)
    if "noC2" not in os.environ.get("K_DBG", ""):
        phase_C2(P, x1s, x1b, mC, gbc, lng, lnb, wr, br, wg, wu, wd, xo, xob)
    if has_A and "noA" not in os.environ.get("K_DBG", ""):
        phase_A(P, a_even, xo, xob, w_in, mA, cos, sin, qng, qkv)
    print("BCA program: %d instructions, %d waits, %d dma sems" % (P.T.n_ins, P.T.n_wait, P.T.nsem), flush=True)
    _PROG_CACHE[key] = P.close()
    return _PROG_CACHE[key]


def a_inputs(inputs, modv, l, r, tabs):
    even = (l % 2 == 0)
    i = l // 2
    cos_c, sin_c = tabs[64 if even else 128]
    m = {"w_in": inputs["w_in_even"][i] if even else inputs["w_in_odd"][i], "mA": modT_layout(modv, l, [1, 0]),
         "cos": cos_c[r], "sin": sin_c[r]}
    if not even:
        m["qng"] = np.stack([inputs["q_norm_g"][i], inputs["k_norm_g"][i]]).astype(np.float32)
    return m


def kernel(**inputs):
    inputs = {k: np.asarray(v) for k, v in inputs.items()}
    x = inputs["x"][0]
    ctx = inputs["ctx"][0]
    tabs = {64: rope_tables(64), 128: rope_tables(128)}
    modv = run_mod(inputs["c"][0], inputs["c_ctx"], inputs["w_mod"], inputs["b_mod"])
    xres = [np.ascontiguousarray(np.concatenate([x[r * 2048:(r + 1) * 2048], ctx[(r % 2) * 128:(r % 2) * 128 + 128]], 0)) for r in range(NCORES)]
    maps = []
    for r in range(NCORES):
        m = a_inputs(inputs, modv, 0, r, tabs)
        m.update({"ident": IDENT, "x": xres[r]})
        maps.append(m)
    res = run(build_A(True), maps)
    qkv = [res[r]["qkv"] for r in range(NCORES)]
    for l in range(DEPTH):
        even = (l % 2 == 0)
        i = l // 2
        has_A = l < DEPTH - 1
        if even:
            lam_init = 0.8 - 0.6 * float(np.exp(-0.3 * l))
            lam4 = np.stack([inputs["lam_q1"][i], inputs["lam_k1"][i], inputs["lam_q2"][i], inputs["lam_k2"][i]])
            maps = layout_even(qkv, inputs["sink_logits"][i], lam4, lam_init, inputs["subln_g"][i])
        else:
            maps = layout_odd(qkv)
        gbc = np.ascontiguousarray(np.stack([np.stack([modv[l, who, 2048:3072], modv[l, who, 5120:6144]]) for who in range(2)]))
        mC = modT_layout(modv, l, [4, 3])
        for r in range(NCORES):
            m = maps[r]
            m.update({"ident": IDENT, "x": xres[r], "w_out": inputs["w_out_even"][i] if even else inputs["w_out_odd"][i], "gbc": gbc, "mC": mC,
                      "lng": inputs["ln_g"][l], "lnb": inputs["ln_b"][l], "wr": inputs["w_router"], "br": inputs["b_router"].reshape(1, NEXP),
                      "wg": inputs["w_gate"][l], "wu": inputs["w_up"][l], "wd": inputs["w_down"][l]})
            if has_A:
                m.update(a_inputs(inputs, modv, l + 1, r, tabs))
        res = run(build_BCA(even, has_A), maps)
        xres = [res[r]["xo"] for r in range(NCORES)]
        if has_A:
            qkv = [res[r]["qkv"] for r in range(NCORES)]
    out = np.concatenate([xres[r][:2048] for r in range(NCORES)], axis=0)
    return out.reshape(1, SEQ, D).astype(np.float32)
```
